# Optimizing a Trainium2 kernel written in Bass

```python
import math
import jax, jax.numpy as jnp
from jax import lax
import numpy as np

D_MODEL = 1024
BATCH = 2
SEQ = 8192
DEPTH = 2

CHUNK = 64
N_EVEN = (DEPTH + 1) // 2
N_ODD = DEPTH // 2
HEAD_DIM = 64
POOL_WINDOWS = (2, 4, 8, 16)
POOL_GROUPS = 4
POOL_WIDTH = D_MODEL // 2
POOL_GROUP_DIM = POOL_WIDTH // POOL_GROUPS
ATT_HEADS = (D_MODEL // 2) // HEAD_DIM
ATT_WIDTH = ATT_HEADS * HEAD_DIM
LEFT_CHUNKS = 8
BAND = (LEFT_CHUNKS + 1) * CHUNK
REL_CLIP = 128
N_REL = 2 * REL_CLIP + 1
EVEN_IN = POOL_WIDTH + 3 * ATT_WIDTH
EVEN_OUT = POOL_WIDTH + ATT_WIDTH
SSM_WIDTH = D_MODEL // 2
SSM_GROUP_DIM = 16
SSM_GROUPS = SSM_WIDTH // SSM_GROUP_DIM
SSM_STATE = 64
DT_MIN = 1e-3
DT_MAX = 1e-1
FOX_HEADS = (D_MODEL // 2) // HEAD_DIM
FOX_WIDTH = FOX_HEADS * HEAD_DIM
ODD_IN = SSM_WIDTH + 3 * FOX_WIDTH + FOX_HEADS
ODD_OUT = SSM_WIDTH + FOX_WIDTH
Q_BLOCK = 128
N_EXPERTS = 32
TOP_K = 4
D_EXPERT = D_MODEL
SWIGLU_LIMIT = 7.0
SWIGLU_ALPHA = 1.702
ROW_BLOCK = 128
DN_ALPHA = (2.0 * DEPTH) ** 0.25
DN_BETA = (8.0 * DEPTH) ** -0.25
LN_EPS = 1e-5
NEG_INF = -1e30

kernel_name = "hybrid_pool_chunkattn_s5_fox_moe"


def layer_norm(x, g, b):
    xf = x.astype(jnp.float32)
    mu = jnp.mean(xf, axis=-1, keepdims=True)
    var = jnp.mean(jnp.square(xf - mu), axis=-1, keepdims=True)
    return ((xf - mu) * lax.rsqrt(var + LN_EPS) * g.astype(jnp.float32) + b.astype(jnp.float32)).astype(x.dtype)


def ada_mod(c, w, b):
    m = jax.nn.silu(c) @ w + b
    shift, scale, gate = jnp.split(m[:, None, :], 3, axis=-1)
    return shift, scale, gate


def pool_mixer(u, w_pool, pool_scale):
    Bn, S, _ = u.shape
    ug = u.reshape(Bn, S, POOL_GROUPS, POOL_GROUP_DIM).astype(jnp.float32)
    cs = jnp.concatenate([jnp.zeros_like(ug[:, :1]), jnp.cumsum(ug, axis=1)], axis=1)
    t = jnp.arange(S)[:, None]
    win = jnp.array(POOL_WINDOWS, dtype=jnp.int32)[None, :]
    lo = jnp.maximum(t + 1 - win, 0)
    cnt = (t + 1 - lo).astype(jnp.float32)[None, :, :, None]
    window_sum = cs[:, 1:] - cs[:, lo, jnp.arange(POOL_GROUPS)[None, :]]
    d = (window_sum / cnt - ug).astype(u.dtype)
    y = jnp.einsum('bsgc,gcd->bsgd', d, w_pool)
    return y.reshape(Bn, S, POOL_WIDTH) * pool_scale


def chunk_attention(q, k, v, rel_bias):
    Bn, S, H, Dh = q.shape
    nc = S // CHUNK
    qc = q.reshape(Bn, nc, CHUNK, H, Dh)

    def band(t):
        tp = jnp.pad(t.reshape(Bn, nc, CHUNK, H, Dh), ((0, 0), (LEFT_CHUNKS, 0), (0, 0), (0, 0), (0, 0)))
        return jnp.concatenate([tp[:, j:j + nc] for j in range(LEFT_CHUNKS + 1)], axis=2)

    kb, vb = band(k), band(v)
    scores = jnp.einsum('bnqhd,bnkhd->bnhqk', qc, kb).astype(jnp.float32) * (Dh ** -0.5)
    q_pos = jnp.arange(CHUNK)[:, None]
    k_pos = jnp.arange(BAND)[None, :] - LEFT_CHUNKS * CHUNK
    rel = jnp.clip(q_pos - k_pos, -REL_CLIP, REL_CLIP) + REL_CLIP
    bias = rel_bias.astype(jnp.float32)[:, rel]
    key_chunk = jnp.arange(nc)[:, None] - LEFT_CHUNKS + jnp.arange(BAND)[None, :] // CHUNK
    scores = jnp.where((key_chunk >= 0)[None, :, None, None, :], scores + bias[None, None], NEG_INF)
    p = jax.nn.softmax(scores, axis=-1).astype(v.dtype)
    o = jnp.einsum('bnhqk,bnkhd->bnqhd', p, vb)
    return o.reshape(Bn, S, H * Dh)


def s5_mixer(u, lam_re, lam_im, log_dt, b_re, b_im, c_re, c_im, d_skip, glu_w, glu_b):
    Bn, S, _ = u.shape
    ug = u.reshape(Bn, S, SSM_GROUPS, SSM_GROUP_DIM).astype(jnp.float32)
    dt = jnp.exp(log_dt.astype(jnp.float32))[:, None]
    lre = jnp.minimum(lam_re.astype(jnp.float32), -1e-4)
    lim = lam_im.astype(jnp.float32)
    mag = jnp.exp(lre * dt)
    abar_re, abar_im = mag * jnp.cos(lim * dt), mag * jnp.sin(lim * dt)
    den = lre * lre + lim * lim
    nre, nim = abar_re - 1.0, abar_im
    g_re = (nre * lre + nim * lim) / den
    g_im = (nim * lre - nre * lim) / den
    bre, bim = b_re.astype(jnp.float32), b_im.astype(jnp.float32)
    bb_re = g_re[..., None] * bre - g_im[..., None] * bim
    bb_im = g_re[..., None] * bim + g_im[..., None] * bre
    bu_re = jnp.einsum('bsgc,gpc->bsgp', ug, bb_re)
    bu_im = jnp.einsum('bsgc,gpc->bsgp', ug, bb_im)
    a_re = jnp.broadcast_to(abar_re, bu_re.shape)
    a_im = jnp.broadcast_to(abar_im, bu_im.shape)

    def combine(e1, e2):
        a1r, a1i, b1r, b1i = e1
        a2r, a2i, b2r, b2i = e2
        return (a2r * a1r - a2i * a1i,
                a2r * a1i + a2i * a1r,
                a2r * b1r - a2i * b1i + b2r,
                a2r * b1i + a2i * b1r + b2i)

    _, _, xr, xi = lax.associative_scan(combine, (a_re, a_im, bu_re, bu_im), axis=1)
    y = (jnp.einsum('bsgp,gcp->bsgc', xr, c_re.astype(jnp.float32))
         - jnp.einsum('bsgp,gcp->bsgc', xi, c_im.astype(jnp.float32))
         + d_skip.astype(jnp.float32) * ug)
    y = jax.nn.gelu(y.reshape(Bn, S, SSM_WIDTH))
    y = y * jax.nn.sigmoid(y @ glu_w.astype(jnp.float32) + glu_b.astype(jnp.float32))
    return y.astype(u.dtype)


def forgetting_attention(q, k, v, f_logit):
    Bn, S, H, Dh = q.shape
    F = jnp.cumsum(jax.nn.log_sigmoid(f_logit.astype(jnp.float32)), axis=1)
    Fh = jnp.transpose(F, (0, 2, 1))
    k_pos = jnp.arange(S)
    q_off = jnp.arange(Q_BLOCK)

    def block(i):
        q0 = i * Q_BLOCK
        qb = lax.dynamic_slice_in_dim(q, q0, Q_BLOCK, axis=1)
        fq = lax.dynamic_slice_in_dim(Fh, q0, Q_BLOCK, axis=2)
        s = jnp.einsum('bqhd,bkhd->bhqk', qb, k).astype(jnp.float32) * (Dh ** -0.5)
        s = s + fq[..., None] - Fh[:, :, None, :]
        s = jnp.where(k_pos[None, :] <= (q0 + q_off)[:, None], s, NEG_INF)
        p = jax.nn.softmax(s, axis=-1).astype(v.dtype)
        return jnp.einsum('bhqk,bkhd->bqhd', p, v)

    o = lax.map(block, jnp.arange(S // Q_BLOCK))
    return jnp.transpose(o, (1, 0, 2, 3, 4)).reshape(Bn, S, H * Dh)


def even_mixer(h, w_in, w_pool, pool_scale, rel_bias, w_out):
    Bn, S, _ = h.shape
    z = h @ w_in
    o1, o2, o3 = POOL_WIDTH, POOL_WIDTH + ATT_WIDTH, POOL_WIDTH + 2 * ATT_WIDTH
    heads = lambda t: t.reshape(Bn, S, ATT_HEADS, HEAD_DIM)
    ya = pool_mixer(z[..., :o1], w_pool, pool_scale)
    yb = chunk_attention(heads(z[..., o1:o2]), heads(z[..., o2:o3]), heads(z[..., o3:]), rel_bias)
    return jnp.concatenate([ya, yb], axis=-1) @ w_out


def odd_mixer(h, w_in, forget_b, lam_re, lam_im, log_dt, b_re, b_im, c_re, c_im, d_skip, glu_w, glu_b, w_out):
    Bn, S, _ = h.shape
    z = h @ w_in
    o1 = SSM_WIDTH
    o2, o3, o4 = o1 + FOX_WIDTH, o1 + 2 * FOX_WIDTH, o1 + 3 * FOX_WIDTH
    heads = lambda t: t.reshape(Bn, S, FOX_HEADS, HEAD_DIM)
    yc = s5_mixer(z[..., :o1], lam_re, lam_im, log_dt, b_re, b_im, c_re, c_im, d_skip, glu_w, glu_b)
    yd = forgetting_attention(heads(z[..., o1:o2]), heads(z[..., o2:o3]), heads(z[..., o3:o4]),
                              z[..., o4:] + forget_b)
    return jnp.concatenate([yc, yd], axis=-1) @ w_out


def moe_ffn(h, router_w, router_b, w_gu, b_gu, w_down, b_down):
    Bn, S, D = h.shape
    xt = h.reshape(-1, D)
    T = xt.shape[0]
    logits = (xt @ router_w + router_b).astype(jnp.float32)
    top_v, top_e = lax.top_k(logits, TOP_K)
    gates = jax.nn.softmax(top_v, axis=-1)
    e_flat = top_e.reshape(-1)
    tok_flat = jnp.arange(T * TOP_K) // TOP_K
    order = jnp.argsort(e_flat)
    e_sorted, tok_sorted = e_flat[order], tok_flat[order]
    g_sorted = gates.reshape(-1)[order].astype(h.dtype)
    counts = jnp.zeros((N_EXPERTS,), jnp.int32).at[e_flat].add(1)
    padded = (counts + ROW_BLOCK - 1) // ROW_BLOCK * ROW_BLOCK
    start = jnp.cumsum(counts) - counts
    pend = jnp.cumsum(padded)
    pstart = pend - padded
    dest = pstart[e_sorted] + (jnp.arange(T * TOP_K) - start[e_sorted])
    n_blocks = (T * TOP_K + N_EXPERTS * (ROW_BLOCK - 1) + ROW_BLOCK - 1) // ROW_BLOCK
    n_rows = n_blocks * ROW_BLOCK
    x_disp = jnp.zeros((n_rows, D), h.dtype).at[dest].set(xt[tok_sorted])
    blk_expert = jnp.minimum(jnp.searchsorted(pend, jnp.arange(n_blocks) * ROW_BLOCK, side='right'), N_EXPERTS - 1)

    def expert_block(args):
        xb, e = args
        gu = xb @ w_gu[e] + b_gu[e]
        gate = jnp.minimum(gu[:, :D_EXPERT], SWIGLU_LIMIT)
        up = jnp.clip(gu[:, D_EXPERT:], -SWIGLU_LIMIT, SWIGLU_LIMIT)
        glu = gate * jax.nn.sigmoid(SWIGLU_ALPHA * gate)
        return ((up + 1.0) * glu) @ w_down[e] + b_down[e]

    y_disp = lax.map(expert_block, (x_disp.reshape(n_blocks, ROW_BLOCK, D), blk_expert)).reshape(n_rows, D)
    y = jnp.zeros((T, D), h.dtype).at[tok_sorted].add(y_disp[dest] * g_sorted[:, None])
    return y.reshape(Bn, S, D)


def setup_inputs(seed: int = 0) -> dict:
    key = jax.random.key(seed)
    ks = jax.random.split(key, 30)
    nrm = lambda k, shape, s: jax.random.normal(k, shape, jnp.float32) * s
    D = D_MODEL
    return {
        "x": nrm(ks[0], (BATCH, SEQ, D), 1.0),
        "c": nrm(ks[1], (BATCH, D), 1.0),
        "mod_w": nrm(ks[2], (DEPTH, 2, D, 3 * D), 0.3 * D ** -0.5),
        "mod_b": nrm(ks[3], (DEPTH, 2, 3 * D), 0.02),
        "ln_g": 1.0 + nrm(ks[4], (DEPTH, 2, D), 0.02),
        "ln_b": nrm(ks[5], (DEPTH, 2, D), 0.02),
        "even_w_in": nrm(ks[6], (N_EVEN, D, EVEN_IN), D ** -0.5),
        "pool_w": nrm(ks[7], (N_EVEN, POOL_GROUPS, POOL_GROUP_DIM, POOL_GROUP_DIM), POOL_GROUP_DIM ** -0.5),
        "pool_scale": 1.0 + nrm(ks[8], (N_EVEN, POOL_WIDTH), 0.02),
        "rel_bias": nrm(ks[9], (N_EVEN, ATT_HEADS, N_REL), 0.5),
        "even_w_out": nrm(ks[10], (N_EVEN, EVEN_OUT, D), DN_BETA * EVEN_OUT ** -0.5),
        "odd_w_in": nrm(ks[11], (N_ODD, D, ODD_IN), D ** -0.5),
        "forget_b": jax.random.uniform(ks[12], (N_ODD, FOX_HEADS), jnp.float32, 1.0, 4.0),
        "ssm_lam_re": -0.5 + nrm(ks[13], (N_ODD, SSM_GROUPS, SSM_STATE), 0.01),
        "ssm_lam_im": math.pi * jnp.arange(SSM_STATE, dtype=jnp.float32) + nrm(ks[14], (N_ODD, SSM_GROUPS, SSM_STATE), 0.01),
        "ssm_log_dt": jax.random.uniform(ks[15], (N_ODD, SSM_GROUPS), jnp.float32, math.log(DT_MIN), math.log(DT_MAX)),
        "ssm_b_re": nrm(ks[16], (N_ODD, SSM_GROUPS, SSM_STATE, SSM_GROUP_DIM), (2 * SSM_GROUP_DIM) ** -0.5),
        "ssm_b_im": nrm(ks[17], (N_ODD, SSM_GROUPS, SSM_STATE, SSM_GROUP_DIM), (2 * SSM_GROUP_DIM) ** -0.5),
        "ssm_c_re": nrm(ks[18], (N_ODD, SSM_GROUPS, SSM_GROUP_DIM, SSM_STATE), SSM_STATE ** -0.5),
        "ssm_c_im": nrm(ks[19], (N_ODD, SSM_GROUPS, SSM_GROUP_DIM, SSM_STATE), SSM_STATE ** -0.5),
        "ssm_d": nrm(ks[20], (N_ODD, SSM_GROUPS, SSM_GROUP_DIM), 1.0),
        "ssm_glu_w": nrm(ks[21], (N_ODD, SSM_WIDTH, SSM_WIDTH), SSM_WIDTH ** -0.5),
        "ssm_glu_b": nrm(ks[22], (N_ODD, SSM_WIDTH), 0.02),
        "odd_w_out": nrm(ks[23], (N_ODD, ODD_OUT, D), DN_BETA * ODD_OUT ** -0.5),
        "router_w": nrm(ks[24], (DEPTH, D, N_EXPERTS), D ** -0.5),
        "router_b": nrm(ks[25], (DEPTH, N_EXPERTS), 0.01),
        "exp_w_gu": nrm(ks[26], (DEPTH, N_EXPERTS, D, 2 * D_EXPERT), D ** -0.5),
        "exp_b_gu": nrm(ks[27], (DEPTH, N_EXPERTS, 2 * D_EXPERT), 0.02),
        "exp_w_down": nrm(ks[28], (DEPTH, N_EXPERTS, D_EXPERT, D), DN_BETA * D_EXPERT ** -0.5),
        "exp_b_down": nrm(ks[29], (DEPTH, N_EXPERTS, D), 0.02),
    }


def reference(x, c, mod_w, mod_b, ln_g, ln_b, even_w_in, pool_w, pool_scale, rel_bias, even_w_out,
              odd_w_in, forget_b, ssm_lam_re, ssm_lam_im, ssm_log_dt, ssm_b_re, ssm_b_im, ssm_c_re, ssm_c_im,
              ssm_d, ssm_glu_w, ssm_glu_b, odd_w_out, router_w, router_b, exp_w_gu, exp_b_gu, exp_w_down,
              exp_b_down):
    for layer in range(DEPTH):
        i = layer // 2
        shift, scale, gate = ada_mod(c, mod_w[layer, 0], mod_b[layer, 0])
        h = x * (1.0 + scale) + shift
        if layer % 2 == 0:
            y = even_mixer(h, even_w_in[i], pool_w[i], pool_scale[i], rel_bias[i], even_w_out[i])
        else:
            y = odd_mixer(h, odd_w_in[i], forget_b[i], ssm_lam_re[i], ssm_lam_im[i], ssm_log_dt[i],
                          ssm_b_re[i], ssm_b_im[i], ssm_c_re[i], ssm_c_im[i], ssm_d[i], ssm_glu_w[i],
                          ssm_glu_b[i], odd_w_out[i])
        x = layer_norm(DN_ALPHA * x + (1.0 + gate) * y, ln_g[layer, 0], ln_b[layer, 0])
        shift, scale, gate = ada_mod(c, mod_w[layer, 1], mod_b[layer, 1])
        h = x * (1.0 + scale) + shift
        y = moe_ffn(h, router_w[layer], router_b[layer], exp_w_gu[layer], exp_b_gu[layer],
                    exp_w_down[layer], exp_b_down[layer])
        x = layer_norm(DN_ALPHA * x + (1.0 + gate) * y, ln_g[layer, 1], ln_b[layer, 1])
    return x
```

```python
import contextlib
import math
import numpy as np
import ml_dtypes
import concourse.bass as bass
import concourse.mybir as mybir
from concourse.bass_utils import run_bass_kernel_spmd

F32 = mybir.dt.float32
BF16 = mybir.dt.bfloat16
AF = mybir.ActivationFunctionType
ALU = mybir.AluOpType
AX = mybir.AxisListType

SAME_ENGINE_SYNC = True
DEBUG = False
NEG = -30000.0
D = 1024
NE = 32
ALPHA = (2.0 * 2) ** 0.25
LN_EPS = 1e-5


class Buf:
    __slots__ = ("name", "w", "r")

    def __init__(self, name=""):
        self.name = name
        self.w = None
        self.r = []


class Op:
    __slots__ = ("eng", "idx", "fn", "deps", "signal", "count", "is_dma", "sem_key", "snap")


class Prog:
    ENGS = ("pe", "act", "dve", "pool", "sp")

    def __init__(self, nc, sbuf_bytes=184 * 1024):
        self.nc = nc
        self.ops = {e: [] for e in self.ENGS}
        self.known = {e: {} for e in self.ENGS}
        self.stack = contextlib.ExitStack()
        self._cnt = {}
        self.last = {}
        self.streams = {}
        self.free_slots = []
        self.nslots = 0
        self.all_ops = []
        self._regs = {}
        self.arena_words = sbuf_bytes // 4
        self.arena = self.stack.enter_context(nc.sbuf_tensor("arena", [128, self.arena_words], F32))
        self.ps = self.stack.enter_context(nc.psum_tensor("psall", [128, 4096], F32))
        self.top = 0
        self.nb = 0

    def alloc(self, shape, dtype=F32):
        free = 1
        for s in shape[1:]:
            free *= s
        esz = 4 if dtype == F32 else 2
        words = (free * esz + 3) // 4
        words = (words + 15) // 16 * 16
        assert self.top + words <= self.arena_words, f"SBUF arena overflow {self.top}+{words}"
        v = self.arena[:, self.top:self.top + words]
        self.top += words
        if dtype != F32:
            v = v.bitcast(dtype)
        v = v[:, :free]
        if len(shape) == 3:
            v = v.rearrange("p (a b) -> p a b", b=shape[2])
        elif len(shape) == 4:
            v = v.rearrange("p (a b c) -> p a b c", b=shape[2], c=shape[3])
        if shape[0] < 128:
            v = v[:shape[0]]
        return v

    def bank(self, i, n=1):
        return self.ps[:, i * 512:(i + n) * 512]

    def buf(self, name=""):
        self.nb += 1
        return Buf(name or f"b{self.nb}")

    def bufs(self, n):
        return [self.buf() for _ in range(n)]

    def _add(self, eng, fn, reads, writes, is_dma, extra=()):
        op = Op()
        op.eng = eng
        op.fn = fn
        op.is_dma = is_dma
        op.signal = bool(is_dma)
        op.count = None
        if is_dma:
            sb_ = writes[0] if writes else reads[0]
            key = self.streams.get(sb_)
            if key is None:
                if self.free_slots:
                    key = self.free_slots.pop()
                else:
                    key = "dma:%d" % self.nslots
                    self.nslots += 1
                self.streams[sb_] = key
            op.sem_key = key
        else:
            op.sem_key = eng
        op.idx = self._cnt.get(op.sem_key, 0)
        if fn is not None:
            self._cnt[op.sem_key] = op.idx + 1
        deps = list(extra)
        for b in reads:
            if b.w is not None:
                deps.append(b.w)
        for b in writes:
            if b.w is not None:
                deps.append(b.w)
            deps.extend(b.r)
        kn = self.known[eng]
        need = {}
        for d in deps:
            if d.sem_key == eng and (eng == "pe" or not SAME_ENGINE_SYNC):
                continue
            if kn.get(d.sem_key, -1) >= d.idx:
                continue
            cur = need.get(d.sem_key)
            if cur is None or cur.idx < d.idx:
                need[d.sem_key] = d
        op.deps = list(need.values())
        for d in op.deps:
            d.signal = True
            if kn.get(d.sem_key, -1) < d.idx:
                kn[d.sem_key] = d.idx
            for k, v in d.snap.items():
                if kn.get(k, -1) < v:
                    kn[k] = v
        op.snap = dict(kn)
        if fn is not None:
            for b in reads:
                b.r.append(op)
            for b in writes:
                b.w = op
                b.r = []
            self.last[op.sem_key] = op
        self.ops[eng].append(op)
        self.all_ops.append(op)
        return op

    def op(self, eng, fn, reads=(), writes=()):
        return self._add(eng, fn, reads, writes, False)

    def dma(self, eng, out, in_, reads=(), writes=(), **kw):
        return self._add(eng, lambda e: e.dma_start(out=out, in_=in_, **kw), reads, writes, True)

    def dump(self, name, ap, b, shape, dtype=F32):
        if not DEBUG:
            return
        d = self.nc.dram_tensor("dbg_" + name, list(shape), dtype, kind="ExternalOutput").ap()
        self.dma("sp", d, ap, reads=[b])

    def const_reg(self, e, value):
        if value not in self._regs:
            r = e.alloc_register("c%d" % value)
            e.reg_mov(r, value)
            self._regs[value] = r
        return self._regs[value]

    def barrier(self):
        lasts = list(self.last.values())
        for e in self.ENGS:
            self._add(e, None, (), (), False, extra=lasts)
        self.streams = {}
        self.free_slots = ["dma:%d" % i for i in range(self.nslots)]

    def emit(self, final_waits=()):
        nc = self.nc
        for d in final_waits:
            d.signal = True
        keys = set()
        for e in self.ENGS:
            for o in self.ops[e]:
                if o.fn is not None:
                    keys.add(o.sem_key)
        counts = {k: 0 for k in keys}
        for o in self.all_ops:
            if o.fn is not None and o.signal:
                counts[o.sem_key] += 16 if o.is_dma else 1
                o.count = counts[o.sem_key]
        self.maxcounts = counts
        sems = {}
        for k in sorted(keys):
            sems[k] = self.stack.enter_context(nc.semaphore(k.replace(":", "_")))
        block = self.stack.enter_context(nc.Block())

        def run(eng_name, e):
            for o in self.ops[eng_name]:
                for d in o.deps:
                    e.wait_ge(sems[d.sem_key], d.count)
                if o.fn is None:
                    continue
                ins = o.fn(e)
                if o.signal:
                    ins.then_inc(sems[o.sem_key], 16 if o.is_dma else 1)
            if eng_name == "sp":
                for d in final_waits:
                    e.wait_ge(sems[d.sem_key], d.count)

        @block.sync
        def _(e):
            run("sp", e)

        @block.scalar
        def _(e):
            run("act", e)

        @block.vector
        def _(e):
            run("dve", e)

        @block.gpsimd
        def _(e):
            run("pool", e)

        @block.tensor
        def _(e):
            run("pe", e)

    def close(self):
        self.stack.close()


def mm(P, out, lhsT, rhs, start, stop, reads, writes):
    return P.op("pe", lambda e: e.matmul(out, lhsT=lhsT, rhs=rhs, start=start, stop=stop), reads, writes)


def tr(P, out, in_, ident, reads, writes):
    return P.op("pe", lambda e: e.transpose(out, in_, ident), reads, writes)


def act(P, out, in_, func, reads, writes, bias=0.0, scale=1.0, eng="act"):
    return P.op(eng, lambda e: e.activation(out=out, in_=in_, func=func, bias=bias, scale=scale), reads, writes)


def tt(P, eng, out, in0, in1, op, reads, writes):
    return P.op(eng, lambda e: e.tensor_tensor(out=out, in0=in0, in1=in1, op=op), reads, writes)


def ts(P, eng, out, in0, s1, s2, op0, op1, reads, writes):
    if s2 is None:
        return P.op(eng, lambda e: e.tensor_scalar(out=out, in0=in0, scalar1=s1, scalar2=None, op0=op0), reads, writes)
    return P.op(eng, lambda e: e.tensor_scalar(out=out, in0=in0, scalar1=s1, scalar2=s2, op0=op0, op1=op1), reads, writes)


def stt(P, eng, out, in0, scalar, in1, op0, op1, reads, writes):
    return P.op(eng, lambda e: e.scalar_tensor_tensor(out=out, in0=in0, scalar=scalar, in1=in1, op0=op0, op1=op1),
                reads, writes)


def cp(P, eng, out, in_, reads, writes):
    if eng == "act":
        return P.op("act", lambda e: e.copy(out=out, in_=in_), reads, writes)
    return P.op(eng, lambda e: e.tensor_copy(out=out, in_=in_), reads, writes)


def memset(P, eng, ap, val, writes):
    return P.op(eng, lambda e: e.memset(ap, val), (), writes)


class Ctx:
    pass


def phase_consts(P, io):
    C = Ctx()
    C.ident = P.alloc([128, 128], F32)
    C.identb = P.alloc([128, 128], BF16)
    C.b_ident = P.buf()
    P.dma("sp", C.ident, io["ident"], writes=[C.b_ident])
    C.b_identb = P.buf()
    cp(P, "dve", C.identb, C.ident, [C.b_ident], [C.b_identb])
    C.ccol = P.alloc([128, 8], F32)
    C.b_ccol = P.buf()
    P.dma("sp", C.ccol, io["ccol"], writes=[C.b_ccol])
    act(P, C.ccol, C.ccol, AF.Silu, [C.b_ccol], [C.b_ccol])
    return C


def phase_mod(P, C, mod_w, mod_b, scr_gate, lng, lnb):
    M = Ctx()
    M.modT = P.alloc([128, 24], F32)
    M.gate_bc = P.alloc([128, 1024], F32)
    M.g_bc = P.alloc([128, 1024], F32)
    M.b_bc = P.alloc([128, 1024], F32)
    M.b_mod = P.buf()
    M.b_bc_buf = P.buf()
    mark = P.top
    wt = [P.alloc([128, 8, 512], F32) for _ in range(2)]
    wb = P.bufs(2)
    mb = P.alloc([128, 24], F32)
    b_mb = P.buf()
    P.dma("sp", mb, mod_b, writes=[b_mb])
    ps = P.bank(0)
    b_ps = P.buf()
    wv = mod_w.rearrange("(k p) n -> p k n", p=128)
    for cb in range(6):
        w = wt[cb % 2]
        P.dma("sp", w, wv[:, :, cb * 512:(cb + 1) * 512], writes=[wb[cb % 2]])
        for c4 in range(4):
            ct = cb * 4 + c4
            for k in range(8):
                mm(P, ps[:, ct:ct + 1], w[:, k, c4 * 128:(c4 + 1) * 128], C.ccol[:, k:k + 1], k == 0, k == 7,
                   [wb[cb % 2], C.b_ccol], [b_ps])
    tt(P, "dve", M.modT, ps[:, 0:24], mb, ALU.add, [b_ps, b_mb], [M.b_mod])
    ts(P, "dve", M.modT[:, 8:24], M.modT[:, 8:24], 1.0, None, ALU.add, None, [M.b_mod], [M.b_mod])
    b_scr = P.buf()
    gsb = P.alloc([8, 128], F32)
    b_gsb = P.buf()
    ps2 = P.bank(1)
    b_ps2 = P.buf()
    tr(P, ps2[:8, 0:128], M.modT[:, 16:24], C.ident, [M.b_mod, C.b_ident], [b_ps2])
    cp(P, "act", gsb, ps2[:8, 0:128], [b_ps2], [b_gsb])
    P.dma("sp", scr_gate.rearrange("(c p) -> c p", p=128), gsb, reads=[b_gsb], writes=[b_scr])
    P.dma("sp", M.gate_bc, scr_gate.partition_broadcast(128), reads=[b_scr], writes=[M.b_bc_buf])
    P.dma("sp", M.g_bc, lng.partition_broadcast(128), writes=[M.b_bc_buf])
    P.dma("sp", M.b_bc, lnb.partition_broadcast(128), writes=[M.b_bc_buf])
    P.dump("modT%d" % P.nb, M.modT, M.b_mod, [128, 24])
    P.dump("gbc%d" % P.nb, M.gate_bc, M.b_bc_buf, [128, 1024])
    P.barrier()
    P.top = mark
    return M


def build_hT(P, C, M, x_dram, ntiles, hT, hT_bufs, ps_banks=(0, 4)):
    mark = P.top
    xt = [P.alloc([128, 1024], F32) for _ in range(2)]
    xb = P.bufs(2)
    pss = [P.bank(ps_banks[0], 2), P.bank(ps_banks[1], 2)]
    psb = P.bufs(2)
    for t in range(ntiles):
        x_ = xt[t % 2]
        P.dma("sp", x_, x_dram[t * 128:(t + 1) * 128, :], writes=[xb[t % 2]])
        ps = pss[t % 2]
        for k in range(8):
            tr(P, ps[:, k * 128:(k + 1) * 128], x_[:, k * 128:(k + 1) * 128], C.ident, [xb[t % 2], C.b_ident],
               [psb[t % 2]])
        for k in range(8):
            eng = "act"
            act(P, hT[:, k, t * 128:(t + 1) * 128], ps[:, k * 128:(k + 1) * 128], AF.Identity,
                [psb[t % 2], M.b_mod], [hT_bufs[t]], bias=M.modT[:, k:k + 1], scale=M.modT[:, 8 + k:9 + k])
    P.barrier()
    P.top = mark


def ln_tile(P, M, tmp, b_tmp, out, b_out, sm, b_sm):
    P.op("dve", lambda e: e.bn_stats(out=sm[:, 0:6], in_=tmp[:, 0:512]), [b_tmp], [b_sm])
    P.op("dve", lambda e: e.bn_stats(out=sm[:, 6:12], in_=tmp[:, 512:1024]), [b_tmp], [b_sm])
    P.op("dve", lambda e: e.bn_aggr(out=sm[:, 12:14], in_=sm[:, 0:12].rearrange("p (a b) -> p a b", b=6)),
         [b_sm], [b_sm])
    ts(P, "dve", sm[:, 15:16], sm[:, 13:14], LN_EPS, None, ALU.add, None, [b_sm], [b_sm])
    act(P, sm[:, 15:16], sm[:, 15:16], AF.Sqrt, [b_sm], [b_sm])
    P.op("dve", lambda e: e.reciprocal(out=sm[:, 14:15], in_=sm[:, 15:16]), [b_sm], [b_sm])
    ts(P, "dve", tmp, tmp, sm[:, 12:13], sm[:, 14:15], ALU.subtract, ALU.mult, [b_tmp, b_sm], [b_tmp])
    tt(P, "pool", tmp, tmp, M.g_bc, ALU.mult, [b_tmp, M.b_bc_buf], [b_tmp])
    tt(P, "pool", out, tmp, M.b_bc, ALU.add, [b_tmp, M.b_bc_buf], [b_out])


def phase_outproj_ln(P, C, M, yT, b_yT, w_out, x_dram, xo_dram, b_xo, ntiles=16):
    mark = P.top
    wo = P.alloc([128, 8, 1024], BF16)
    b_wo = P.buf()
    wv = w_out.rearrange("(k p) n -> p k n", p=128)
    for h in range(2):
        P.dma("pool", wo[:, :, h * 512:(h + 1) * 512], wv[:, :, h * 512:(h + 1) * 512], writes=[b_wo])
    xt = [P.alloc([128, 1024], F32) for _ in range(2)]
    xb = P.bufs(2)
    tm = [P.alloc([128, 1024], F32) for _ in range(2)]
    tb = P.bufs(2)
    ot = [P.alloc([128, 1024], F32) for _ in range(2)]
    ob = P.bufs(2)
    sm = [P.alloc([128, 16], F32) for _ in range(2)]
    sb = P.bufs(2)
    pss = [P.bank(0, 2), P.bank(2, 2)]
    psb = P.bufs(2)
    for t in range(ntiles):
        i = t % 2
        P.dma("sp", xt[i], x_dram[t * 128:(t + 1) * 128, :], writes=[xb[i]])
        for cb in range(2):
            for k in range(8):
                mm(P, pss[i][:, cb * 512:(cb + 1) * 512], yT[:, k, t * 128:(t + 1) * 128],
                   wo[:, k, cb * 512:(cb + 1) * 512], k == 0, k == 7, [b_yT, b_wo], [psb[i]])
        tt(P, "dve", tm[i], pss[i], M.gate_bc, ALU.mult, [psb[i], M.b_bc_buf], [tb[i]])
        stt(P, "dve", tm[i], xt[i], ALPHA, tm[i], ALU.mult, ALU.add, [xb[i], tb[i]], [tb[i]])
        ln_tile(P, M, tm[i], tb[i], ot[i], ob[i], sm[i], sb[i])
        P.dma("sp", xo_dram[t * 128:(t + 1) * 128, :], ot[i], reads=[ob[i]], writes=[b_xo])
    P.barrier()
    P.top = mark


def phase_moe(P, C, M, x_dram, out_dram, b_out, io, layer):
    if SPARSE_MOE:
        if not hasattr(P, "moe_scr"):
            P.moe_scr = (P.nc.dram_tensor("moe_xd", [NE * CAP, 1024], F32, kind="Internal").ap(),
                         P.nc.dram_tensor("moe_yd", [NE * CAP, 1024], F32, kind="Internal").ap())
        return phase_moe_sparse(P, C, M, x_dram, out_dram, b_out, io, layer, P.moe_scr[0], P.moe_scr[1])
    rw, rb = io["router_w"][layer], io["router_b"][layer]
    wgu, bguT = io["exp_w_gu"][layer], io["b_guT"][layer]
    wdn, bdn = io["exp_w_down"][layer], io["exp_b_down"][layer]
    mark0 = P.top
    rwt = P.alloc([128, 8, NE], BF16)
    b_rw = P.buf()
    P.dma("pool", rwt, rw.rearrange("(k p) n -> p k n", p=128), writes=[b_rw])
    rbc = P.alloc([128, NE], F32)
    b_rb = P.buf()
    P.dma("sp", rbc, rb.partition_broadcast(128), writes=[b_rb])
    bdt = P.alloc([32, 1024], F32)
    b_bd = P.buf()
    P.dma("sp", bdt, bdn, writes=[b_bd])
    bgt = P.alloc([128, 16, NE], F32)
    b_bg = P.buf()
    P.dma("sp", bgt, bguT, writes=[b_bg])
    ts(P, "dve", bgt[:, 8:16, :], bgt[:, 8:16, :], 1.0, None, ALU.add, None, [b_bg], [b_bg])
    wguv = wgu.rearrange("e (k p) n -> e p k n", p=128)
    wdnv = wdn.rearrange("e (k p) n -> e p k n", p=128)
    markh = P.top
    for half in range(2):
        P.top = markh
        t0 = half * 8
        hT = P.alloc([128, 8, 1024], BF16)
        hTb = P.bufs(8)
        build_hT(P, C, M, x_dram[half * 1024:(half + 1) * 1024, :], 8, hT, hTb)
        G = P.alloc([128, 8, NE], F32)
        b_G = P.buf()
        GT = P.alloc([32, 1024], F32)
        b_GT = P.buf()
        yacc = P.alloc([128, 8, 1024], F32)
        yb = P.bufs(8)
        mark1 = P.top
        lg = [P.alloc([128, NE], F32) for _ in range(2)]
        lgb = P.bufs(2)
        m8 = [P.alloc([128, 8], F32) for _ in range(2)]
        m8b = P.bufs(2)
        ex = [P.alloc([128, NE], F32) for _ in range(2)]
        exb = P.bufs(2)
        pss = [P.bank(0), P.bank(1)]
        psb = P.bufs(2)
        pst = [P.bank(2), P.bank(3)]
        pstb = P.bufs(2)
        for t in range(8):
            i = t % 2
            for k in range(8):
                mm(P, pss[i][:, 0:NE], hT[:, k, t * 128:(t + 1) * 128], rwt[:, k, :], k == 0, k == 7,
                   [hTb[t], b_rw], [psb[i]])
            tt(P, "dve", lg[i], pss[i][:, 0:NE], rbc, ALU.add, [psb[i], b_rb], [lgb[i]])
            P.op("dve", lambda e, i=i: e.max(out=m8[i], in_=lg[i]), [lgb[i]], [m8b[i]])
            ts(P, "dve", ex[i], lg[i], m8[i][:, 0:1], None, ALU.subtract, None, [lgb[i], m8b[i]], [exb[i]])
            act(P, ex[i], ex[i], AF.Exp, [exb[i]], [exb[i]])
            stt(P, "dve", ex[i], lg[i], m8[i][:, 3:4], ex[i], ALU.is_ge, ALU.mult, [lgb[i], m8b[i], exb[i]],
                [exb[i]])
            P.op("dve", lambda e, i=i: e.reduce_sum(out=m8[i][:, 4:5], in_=ex[i], axis=AX.X), [exb[i]], [m8b[i]])
            P.op("dve", lambda e, i=i: e.reciprocal(out=m8[i][:, 5:6], in_=m8[i][:, 4:5]), [m8b[i]], [m8b[i]])
            ts(P, "dve", G[:, t, :], ex[i], m8[i][:, 5:6], None, ALU.mult, None, [exb[i], m8b[i]], [b_G])
            tr(P, pst[i][:32, 0:128], G[:, t, :], C.ident, [b_G, C.b_ident], [pstb[i]])
            cp(P, "act", GT[:, t * 128:(t + 1) * 128], pst[i][:32, 0:128], [pstb[i]], [b_GT])
        P.barrier()
        P.top = mark1
        wg = P.alloc([128, 8, 2048], BF16)
        wgb = P.bufs(2)
        wd = P.alloc([128, 8, 1024], BF16)
        wdb = P.buf()
        aT = P.alloc([128, 8, 1024], BF16)
        aTb = P.bufs(2)
        gt = [P.alloc([128, 512], F32) for _ in range(2)]
        gtb = P.bufs(2)
        sg = [P.alloc([128, 512], F32) for _ in range(2)]
        sgb = P.bufs(2)
        up = [P.alloc([128, 512], F32) for _ in range(2)]
        upb = P.bufs(2)
        psg = [P.bank(0), P.bank(1)]
        psgb = P.bufs(2)
        psu = [P.bank(2), P.bank(3)]
        psub = P.bufs(2)
        psd = [P.bank(4, 2), P.bank(6, 2)]
        psdb = P.bufs(2)
        it = 0
        for e in range(NE):
            for c in range(2):
                P.dma("pool", wg[:, :, c * 512:(c + 1) * 512], wguv[e][:, :, c * 512:(c + 1) * 512], writes=[wgb[c]])
                P.dma("pool", wg[:, :, 1024 + c * 512:1024 + (c + 1) * 512],
                      wguv[e][:, :, 1024 + c * 512:1024 + (c + 1) * 512], writes=[wgb[c]])
            for ct in range(8):
                c = ct // 4
                for tb_ in range(2):
                    i = it % 2
                    it += 1
                    tok = slice(tb_ * 512, (tb_ + 1) * 512)
                    hb = hTb[tb_ * 4: tb_ * 4 + 4]
                    for k in range(8):
                        mm(P, psg[i], wg[:, k, ct * 128:(ct + 1) * 128], hT[:, k, tok], k == 0, k == 7,
                           [wgb[c]] + hb, [psgb[i]])
                    for k in range(8):
                        mm(P, psu[i], wg[:, k, 1024 + ct * 128:1024 + (ct + 1) * 128], hT[:, k, tok], k == 0, k == 7,
                           [wgb[c]] + hb, [psub[i]])
                    ts(P, "dve", gt[i], psg[i], bgt[:, ct, e:e + 1], 7.0, ALU.add, ALU.min, [psgb[i], b_bg], [gtb[i]])
                    act(P, sg[i], gt[i], AF.Sigmoid, [gtb[i]], [sgb[i]], scale=1.702)
                    ts(P, "dve", up[i], psu[i], bgt[:, 8 + ct, e:e + 1], -6.0, ALU.add, ALU.max, [psub[i], b_bg],
                       [upb[i]])
                    tt(P, "pool", sg[i], sg[i], gt[i], ALU.mult, [sgb[i], gtb[i]], [sgb[i]])
                    stt(P, "dve", aT[:, ct, tok], up[i], 8.0, sg[i], ALU.min, ALU.mult, [sgb[i], upb[i]], [aTb[tb_]])
            for h2 in range(2):
                P.dma("pool", wd[:, :, h2 * 512:(h2 + 1) * 512], wdnv[e][:, :, h2 * 512:(h2 + 1) * 512],
                      writes=[wdb])
            for t in range(8):
                i = t % 2
                for cb in range(2):
                    for k in range(8):
                        mm(P, psd[i][:, cb * 512:(cb + 1) * 512], aT[:, k, t * 128:(t + 1) * 128],
                           wd[:, k, cb * 512:(cb + 1) * 512], k == 0, k == 7, [aTb[t // 4], wdb], [psdb[i]])
                if e == 0:
                    ts(P, "dve", yacc[:, t, :], psd[i], G[:, t, e:e + 1], None, ALU.mult, None,
                       [psdb[i], b_G], [yb[t]])
                else:
                    stt(P, "dve", yacc[:, t, :], psd[i], G[:, t, e:e + 1], yacc[:, t, :], ALU.mult, ALU.add,
                        [psdb[i], b_G, yb[t]], [yb[t]])
        P.barrier()
        P.top = mark1
        xt = [P.alloc([128, 1024], F32) for _ in range(2)]
        xb = P.bufs(2)
        ot = [P.alloc([128, 1024], F32) for _ in range(2)]
        ob = P.bufs(2)
        sm = [P.alloc([128, 16], F32) for _ in range(2)]
        sb = P.bufs(2)
        for t in range(8):
            i = t % 2
            tg = t0 + t
            P.dma("sp", xt[i], x_dram[tg * 128:(tg + 1) * 128, :], writes=[xb[i]])
            for cb in range(2):
                mm(P, psd[i][:, cb * 512:(cb + 1) * 512], GT[:, t * 128:(t + 1) * 128],
                   bdt[:, cb * 512:(cb + 1) * 512], True, True, [b_GT, b_bd], [psdb[i]])
            tt(P, "dve", yacc[:, t, :], yacc[:, t, :], psd[i], ALU.add, [psdb[i], yb[t]], [yb[t]])
            tt(P, "dve", yacc[:, t, :], yacc[:, t, :], M.gate_bc, ALU.mult, [yb[t], M.b_bc_buf], [yb[t]])
            stt(P, "dve", yacc[:, t, :], xt[i], ALPHA, yacc[:, t, :], ALU.mult, ALU.add, [xb[i], yb[t]], [yb[t]])
            ln_tile(P, M, yacc[:, t, :], yb[t], ot[i], ob[i], sm[i], sb[i])
            P.dma("sp", out_dram[tg * 128:(tg + 1) * 128, :], ot[i], reads=[ob[i]], writes=[b_out])
        P.barrier()
    P.top = mark0


def phase_even_mixer(P, C, M, io, x_dram_halo, yT, b_yT):
    w_in = io["even_w_in"]
    mark0 = P.top
    NU = 2176
    uT = P.alloc([128, 4, NU], BF16)
    ub = P.bufs(4)
    qT = P.alloc([128, 4, 2048], BF16)
    qb = P.bufs(4)
    kT = P.alloc([128, 4, 2560], BF16)
    kb = P.bufs(4)
    V = P.alloc([128, 20, 8 * 65], BF16)
    vb = P.bufs(20)
    b_vones = P.buf()
    memset(P, "pool", V, 1.0, [b_vones])
    flag = P.alloc([128, 2], F32)
    b_flag = P.buf()
    P.dma("sp", flag, io["flag"], writes=[b_flag])
    icnt = P.alloc([128, 4, 16], F32)
    b_icnt = P.buf()
    P.dma("sp", icnt, io["icnt"], writes=[b_icnt])
    psc = P.alloc([128, 4], F32)
    b_psc = P.buf()
    P.dma("sp", psc, io["pool_scaleT"], writes=[b_psc])
    wp = P.alloc([128, 4, 128], BF16)
    b_wp = P.buf()
    P.dma("pool", wp, io["pool_w"].rearrange("g c d -> c g d"), writes=[b_wp])
    mark_tmp = P.top
    hT = P.alloc([128, 8, 2560], BF16)
    hTb = P.bufs(20)
    build_hT(P, C, M, x_dram_halo, 20, hT, hTb)
    wi2 = [P.alloc([128, 8, 512], BF16) for _ in range(2)]
    wi2b = P.bufs(2)
    wv = w_in.rearrange("(k p) n -> p k n", p=128)

    class WI:
        loaded = -1

        def __getitem__(self, key):
            _, k, cols = key
            c = cols.start // 512
            if c > WI.loaded:
                assert c == WI.loaded + 1
                P.dma("pool", wi2[c % 2], wv[:, :, c * 512:(c + 1) * 512], writes=[wi2b[c % 2]])
                WI.loaded = c
            return wi2[c % 2][:, k, cols.start - c * 512:cols.stop - c * 512]
    wi = WI()
    wib = [wi2b[0], wi2b[1], wi2b[0], wi2b[1]]
    pss = [P.bank(i) for i in range(4)]
    psb = P.bufs(4)
    it = 0
    allh = hTb

    def proj(cols, tok0, ntok, evac):
        nonlocal it
        i = it % 4
        it += 1
        t_lo, t_hi = tok0 // 128, (tok0 + ntok + 127) // 128
        for k in range(8):
            mm(P, pss[i][:, 0:ntok], wi[:, k, cols], hT[:, k, tok0:tok0 + ntok], k == 0, k == 7,
               [wib[cols.start // 512]] + allh[t_lo:t_hi], [psb[i]])
        evac(pss[i][:, 0:ntok], psb[i])

    for g in range(4):
        for blk in range(5):
            tok0 = 384 + blk * 512
            ntok = min(512, 2560 - tok0)
            proj(slice(g * 128, (g + 1) * 128), tok0, ntok,
                 lambda ps, b, g=g, blk=blk, ntok=ntok: cp(P, "act", uT[:, g, blk * 512:blk * 512 + ntok], ps, [b],
                                                          [ub[g]]))
    for c in range(4):
        for blk in range(4):
            proj(slice(512 + c * 128, 512 + (c + 1) * 128), 512 + blk * 512, 512,
                 lambda ps, b, c=c, blk=blk: act(P, qT[:, c, blk * 512:(blk + 1) * 512], ps, AF.Copy, [b], [qb[c]],
                                                 scale=0.125))
        for blk in range(5):
            proj(slice(1024 + c * 128, 1024 + (c + 1) * 128), blk * 512, 512,
                 lambda ps, b, c=c, blk=blk: cp(P, "dve", kT[:, c, blk * 512:(blk + 1) * 512], ps, [b], [kb[c]]))
    for t in range(20):
        i = it % 4
        it += 1
        for k in range(8):
            mm(P, pss[i], hT[:, k, t * 128:(t + 1) * 128], wi[:, k, 1536:2048], k == 0, k == 7, [wib[3], hTb[t]],
               [psb[i]])
        cp(P, "dve" if t % 2 else "act", V[:, t, :].rearrange("p (h d) -> p h d", d=65)[:, :, 0:64],
           pss[i].rearrange("p (h d) -> p h d", d=64), [psb[i], b_vones], [vb[t]])
    P.dump("hT", hT[:, :, 512:640], hTb[4], [128, 8, 128], BF16)
    P.dump("ident", C.ident, C.b_ident, [128, 128])
    P.dump("uT", uT, ub[0], [128, 4, NU], BF16)
    P.dump("qT", qT, qb[0], [128, 4, 2048], BF16)
    P.dump("kT", kT, kb[0], [128, 4, 2560], BF16)
    P.dump("V", V, vb[0], [128, 20, 520], BF16)
    P.barrier()
    P.top = mark_tmp
    for g in range(4):
        w = 2 ** (g + 1)
        ts(P, "dve", uT[:, g, 0:128], uT[:, g, 0:128], flag[:, 0:1], None, ALU.mult, None, [ub[g], b_flag], [ub[g]])
    sA = P.alloc([128, NU], F32)
    sB = P.alloc([128, NU], F32)
    b_sA, b_sB = P.buf(), P.buf()
    dT = P.alloc([128, 4, 2048], BF16)
    db = P.bufs(4)
    for g in range(4):
        w = 2 ** (g + 1)
        eng = "dve" if g % 2 == 0 else "pool"
        src, b_src = uT[:, g, :], ub[g]
        m = 1
        cur, b_cur = sA, b_sA
        lo = 0
        while m < w:
            lo_new = lo + m
            tt(P, eng, cur[:, lo_new:NU], src[:, lo_new:NU], src[:, lo_new - m:NU - m], ALU.add, [b_src], [b_cur])
            src, b_src = cur, b_cur
            cur, b_cur = (sB, b_sB) if cur is sA else (sA, b_sA)
            lo = lo_new
            m *= 2
        stt(P, "dve", dT[:, g, 16:2048], src[:, 144:NU], 1.0 / w, uT[:, g, 144:NU], ALU.mult, ALU.subtract,
            [b_src, ub[g]], [db[g]])
        tt(P, eng, cur[:, 0:16], src[:, 128:144], icnt[:, g, :], ALU.mult, [b_src, b_icnt], [b_cur])
        tt(P, eng, dT[:, g, 0:16], cur[:, 0:16], uT[:, g, 128:144], ALU.subtract, [b_cur, ub[g]], [db[g]])
    it = 0
    for g in range(4):
        for blk in range(4):
            i = it % 4
            it += 1
            mm(P, pss[i], wp[:, g, :], dT[:, g, blk * 512:(blk + 1) * 512], True, True, [b_wp, db[g]], [psb[i]])
            act(P, yT[:, g, blk * 512:(blk + 1) * 512], pss[i], AF.Copy, [psb[i], b_psc], [b_yT],
                scale=psc[:, g:g + 1])
    P.barrier()
    P.top = P.top
    BT = P.alloc([128, 8, 5, 256], BF16)
    b_BT = P.buf()
    P.dma("sp", BT, io["biasT"], writes=[b_BT])
    zero = P.alloc([128, 1], F32)
    b_zero = P.buf()
    memset(P, "dve", zero, 0.0, [b_zero])
    pT = [P.alloc([128, 5, 128], BF16) for _ in range(2)]
    pTb = P.bufs(2)
    rs = [P.alloc([128, 8], F32) for _ in range(2)]
    rsb = P.bufs(2)
    ya = [P.alloc([128, 512], BF16) for _ in range(2)]
    yab = P.bufs(2)
    psS = [P.bank(0, 2), P.bank(2, 2)]
    psSb = P.bufs(2)
    psO = [P.bank(4, 2), P.bank(6, 2)]
    psOb = P.bufs(2)
    psT = P.bank(4, 2)
    def emit_S(qt, h, si):
        c, r0 = h // 2, (h % 2) * 64
        for j in range(5):
            kt = qt + j
            mm(P, psS[si][:, j * 128:(j + 1) * 128], kT[r0:r0 + 64, c, kt * 128:(kt + 1) * 128],
               qT[r0:r0 + 64, c, qt * 128:(qt + 1) * 128], True, False, [kb[c], qb[c]], [psSb[si]])
            mm(P, psS[si][:, j * 128:(j + 1) * 128], C.identb, BT[:, h, j, 0:128], False, False,
               [C.b_identb, b_BT], [psSb[si]])
            mm(P, psS[si][:, j * 128:(j + 1) * 128], C.identb, BT[:, h, j, 128:256], False, True,
               [C.b_identb, b_BT], [psSb[si]])
        for j in range(5):
            kt = qt + j
            bias = flag[:, 1:2] if kt < 4 else zero[:, 0:1]
            act(P, pT[si][:, j, :], psS[si][:, j * 128:(j + 1) * 128], AF.Exp, [psSb[si], b_flag, b_zero],
                [pTb[si]], bias=bias)

    def emit_PV(qt, h, si):
        oi = qt % 2
        ob = psO[oi][:, (h // 4) * 512 + (h % 4) * 65:(h // 4) * 512 + (h % 4) * 65 + 65]
        for j in range(5):
            kt = qt + j
            mm(P, ob, pT[si][:, j, :], V[:, kt, h * 65:(h + 1) * 65], j == 0, j == 4, [pTb[si], vb[kt]],
               [psOb[oi]])
        if h != 7:
            return
        for hb_ in range(2):
            ov = psO[oi][:, hb_ * 512:hb_ * 512 + 260].rearrange("p (h d) -> p h d", d=65)
            P.op("dve", lambda e, ov=ov, oi=oi, hb_=hb_: e.reciprocal(out=rs[oi][:, hb_ * 4:(hb_ + 1) * 4],
                                                                        in_=ov[:, :, 64]), [psOb[oi]], [rsb[oi]])
            for hh in range(4):
                h2 = hb_ * 4 + hh
                ts(P, "dve", ya[oi][:, h2 * 64:(h2 + 1) * 64], ov[:, hh, 0:64], rs[oi][:, h2:h2 + 1], None, ALU.mult,
                   None, [psOb[oi], rsb[oi]], [yab[oi]])
        sT = (si + 1) % 2
        psTv = psS[si].bitcast(BF16)
        for c in range(4):
            tr(P, psTv[:, c * 128:(c + 1) * 128], ya[oi][:, c * 128:(c + 1) * 128], C.identb,
               [yab[oi], C.b_identb], [psSb[si]])
        cp(P, "act", yT[:, 4:8, qt * 128:(qt + 1) * 128],
           psTv[:, 0:512].rearrange("p (c q) -> p c q", q=128), [psSb[si]], [b_yT])

    items = [(qt, h) for qt in range(16) for h in range(8)]
    emit_S(items[0][0], items[0][1], 0)
    for n_ in range(len(items)):
        if n_ + 1 < len(items):
            emit_S(items[n_ + 1][0], items[n_ + 1][1], (n_ + 1) % 2)
        emit_PV(items[n_][0], items[n_][1], n_ % 2)
    P.dump("yT", yT, b_yT, [128, 8, 2048], BF16)
    P.barrier()
    P.top = mark0


def dram_in(nc, name, shape, dtype=F32):
    return nc.dram_tensor(name, list(shape), dtype, kind="ExternalInput").ap()


def build_stage_a():
    nc = bass.Bass("TRN2", target_bir_lowering=False)
    io = {}
    io["x"] = dram_in(nc, "x", [2560, 1024])
    io["ustrict"] = dram_in(nc, "ustrict", [128, 128], BF16)
    io["iotaC"] = dram_in(nc, "iotaC", [128, NE])
    io["ident"] = dram_in(nc, "ident", [128, 128])
    io["ccol"] = dram_in(nc, "ccol", [128, 8])
    io["mod_w"] = dram_in(nc, "mod_w", [3, 1024, 3072])
    io["mod_bT"] = dram_in(nc, "mod_bT", [3, 128, 24])
    io["ln_g"] = dram_in(nc, "ln_g", [2, 1024])
    io["ln_b"] = dram_in(nc, "ln_b", [2, 1024])
    io["even_w_in"] = dram_in(nc, "even_w_in", [1024, 2048])
    io["flag"] = dram_in(nc, "flag", [128, 2])
    io["icnt"] = dram_in(nc, "icnt", [128, 4, 16])
    io["pool_scaleT"] = dram_in(nc, "pool_scaleT", [128, 4])
    io["pool_w"] = dram_in(nc, "pool_w", [4, 128, 128])
    io["biasT"] = dram_in(nc, "biasT", [128, 8, 5, 256], BF16)
    io["even_w_out"] = dram_in(nc, "even_w_out", [1024, 1024])
    io["router_w"] = dram_in(nc, "router_w", [1, 1024, 32])
    io["router_b"] = dram_in(nc, "router_b", [1, 32])
    if not STAGE_A_MOE:
        NEX = 1
    else:
        NEX = 32
    io["exp_w_gu"] = dram_in(nc, "exp_w_gu", [1, NEX, 1024, 2048])
    io["b_guT"] = dram_in(nc, "b_guT", [1, 128, 16, 32])
    io["exp_w_down"] = dram_in(nc, "exp_w_down", [1, NEX, 1024, 1024])
    io["exp_b_down"] = dram_in(nc, "exp_b_down", [1, 32, 1024])
    xmid = nc.dram_tensor("xmid", [2048, 1024], F32, kind="ExternalOutput").ap()
    xout = nc.dram_tensor("xout", [2048, 1024], F32, kind="ExternalOutput").ap()
    scr = nc.dram_tensor("scr_gate", [3, 1024], F32, kind="ExternalOutput").ap()
    h1T_out = nc.dram_tensor("h1T", [128, 8, 2048], BF16, kind="ExternalOutput").ap()
    P = Prog(nc)
    C = phase_consts(P, io)
    P.barrier()
    stage_a_body(P, C, io, scr, xmid, xout, h1T_out)
    P.emit(final_waits=[P.last[k] for k in P.last if k.startswith("dma:")])
    P.close()
    return nc


def stage_a_body(P, C, io, scr, xmid, xout, h1T_out):
    base = P.top
    M = phase_mod(P, C, io["mod_w"][0], io["mod_bT"][0], scr[0], io["ln_g"][0], io["ln_b"][0])
    yT = P.alloc([128, 8, 2048], BF16)
    b_yT = P.buf()
    phase_even_mixer(P, C, M, io, io["x"], yT, b_yT)
    b_xmid = P.buf()
    phase_outproj_ln(P, C, M, yT, b_yT, io["even_w_out"], io["x"][512:2560, :], xmid, b_xmid)
    P.barrier()
    P.top = base
    M2 = phase_mod(P, C, io["mod_w"][1], io["mod_bT"][1], scr[1], io["ln_g"][1], io["ln_b"][1])
    b_xout = P.buf()
    if STAGE_A_MOE:
        phase_moe(P, C, M2, xmid, xout, b_xout, io, 0)
        P.barrier()
        P.top = base
        M3 = phase_mod(P, C, io["mod_w"][2], io["mod_bT"][2], scr[2], io["ln_g"][0], io["ln_b"][0])
        h1 = P.alloc([128, 8, 2048], BF16)
        h1b = P.bufs(16)
        build_hT(P, C, M3, xout, 16, h1, h1b)
        P.dma("sp", h1T_out, h1, reads=h1b, writes=[P.buf()])
    P.barrier()
    P.top = base


STAGE_A_MOE = True


def _f32(a):
    return np.ascontiguousarray(np.asarray(a, dtype=np.float32))


def _colT(v, ncol):
    return _f32(np.asarray(v).reshape(ncol, 128).T)


def _bias_tables(rel_bias):
    j = np.arange(640)[:, None]
    i = np.arange(128)[None, :]
    d = i + 512 - j
    idx = np.clip(d, -128, 128) + 128
    ck, cq = j // 64, i // 64
    allowed = (ck >= cq) & (ck <= 8 + cq)
    full = np.where(allowed[None], np.asarray(rel_bias, np.float32)[:, idx], np.float32(NEG))
    hi = full.astype(ml_dtypes.bfloat16)
    lo = (full - hi.astype(np.float32)).astype(ml_dtypes.bfloat16)
    out = np.zeros((128, 8, 5, 256), ml_dtypes.bfloat16)
    hi5 = hi.reshape(8, 5, 128, 128).transpose(2, 0, 1, 3)
    lo5 = lo.reshape(8, 5, 128, 128).transpose(2, 0, 1, 3)
    out[..., 0:128] = hi5
    out[..., 128:256] = lo5
    return out


def _moe_consts():
    tp = np.arange(128)[:, None]
    tq = np.arange(128)[None, :]
    return {"ustrict": (tp < tq).astype(np.float32).astype(ml_dtypes.bfloat16),
            "iotaC": _f32(np.broadcast_to((np.arange(NE) * CAP).astype(np.float32)[None], (128, NE)))}


def prep_stage_a(inp):
    x = _f32(inp["x"])
    c = _f32(inp["c"])
    common = {
        **_moe_consts(),
        "ident": np.eye(128, dtype=np.float32),
        "mod_w": _f32(np.concatenate([inp["mod_w"][0], inp["mod_w"][1, 0:1]])),
        "mod_bT": _f32(np.stack([_colT(inp["mod_b"][0, 0], 24), _colT(inp["mod_b"][0, 1], 24),
                                 _colT(inp["mod_b"][1, 0], 24)])),
        "ln_g": _f32(inp["ln_g"][0]),
        "ln_b": _f32(inp["ln_b"][0]),
        "even_w_in": _f32(inp["even_w_in"][0]),
        "pool_scaleT": _colT(inp["pool_scale"][0], 4),
        "pool_w": _f32(inp["pool_w"][0]),
        "biasT": _bias_tables(inp["rel_bias"][0]),
        "even_w_out": _f32(inp["even_w_out"][0]),
        "router_w": _f32(inp["router_w"][0:1]),
        "router_b": _f32(inp["router_b"][0:1]),
        "exp_w_gu": _f32(inp["exp_w_gu"][0:1, 0:(32 if STAGE_A_MOE else 1)]),
        "b_guT": _f32(np.asarray(inp["exp_b_gu"][0]).reshape(32, 16, 128).transpose(2, 1, 0)[None]),
        "exp_w_down": _f32(inp["exp_w_down"][0:1, 0:(32 if STAGE_A_MOE else 1)]),
        "exp_b_down": _f32(inp["exp_b_down"][0:1]),
    }
    wins = np.array([2, 4, 8, 16])
    maps = []
    for core in range(8):
        b, seg = core // 4, core % 4
        t0 = seg * 2048
        xs = np.zeros((2560, 1024), np.float32)
        if seg > 0:
            xs[:] = x[b, t0 - 512:t0 + 2048]
        else:
            xs[512:] = x[b, 0:2048]
        flag = np.zeros((128, 2), np.float32)
        flag[:, 0] = 1.0 if seg > 0 else 0.0
        flag[:, 1] = 0.0 if seg > 0 else NEG
        tpos = np.arange(16)
        if seg > 0:
            cnt = np.broadcast_to(wins[:, None], (4, 16))
        else:
            cnt = np.minimum(tpos[None, :] + 1, wins[:, None])
        icnt = np.broadcast_to((1.0 / cnt).astype(np.float32)[None], (128, 4, 16))
        m = dict(common)
        m["x"] = xs
        m["ccol"] = _colT(c[b], 8)
        m["flag"] = flag
        m["icnt"] = _f32(icnt)
        maps.append(m)
    return maps


I32 = mybir.dt.int32
TWO_PI = 2.0 * math.pi
SB_T = 1024
B_STOP = 0
B_SKIP_FOX = False


def sin_table(P, out, b_out, turns, b_turns, tmpf, tmpi, b_tmp):
    ts(P, "dve", turns, turns, 0.5, None, ALU.add, None, [b_turns], [b_turns])
    cp(P, "dve", tmpi, turns, [b_turns], [b_tmp])
    cp(P, "dve", tmpf, tmpi, [b_tmp], [b_tmp])
    tt(P, "dve", turns, turns, tmpf, ALU.subtract, [b_turns, b_tmp], [b_turns])
    ts(P, "dve", tmpf, turns, 0.0, None, ALU.is_lt, None, [b_turns], [b_tmp])
    tt(P, "dve", turns, turns, tmpf, ALU.add, [b_turns, b_tmp], [b_turns])
    act(P, out, turns, AF.Sin, [b_turns], [b_out], scale=TWO_PI, bias=-math.pi)


def stage_b_body(P, C, io, ygT_out, ydT_out):
    hT_blk = io["hT_blk"]
    NTOK = 8192
    base = P.top
    W = P.alloc([128, 8, 514], BF16)
    b_W = P.buf()
    P.dma("pool", W, io["w"].rearrange("(k p) n -> p k n", p=128), writes=[b_W])
    fb = P.alloc([2, 1], F32)
    b_fb = P.buf()
    P.dma("sp", fb, io["fb"], writes=[b_fb])
    maskT = P.alloc([128, 128], BF16)
    b_mask = P.buf()
    P.dma("sp", maskT, io["maskT"], writes=[b_mask])
    mark_fox = P.top
    QK = [[P.alloc([68, NTOK], BF16) for _ in range(2)] for _ in range(2)]
    qkb = [[P.buf() for _ in range(2)] for _ in range(2)]
    V = P.alloc([128, 64, 130], BF16)
    b_vones = P.buf()
    vb = P.bufs(64)
    memset(P, "pool", V, 1.0, [b_vones])
    FL = P.alloc([2, NTOK], F32)
    b_FL = P.buf()
    ydT = P.alloc([128, NTOK], BF16)
    b_ydT = P.buf()
    for hd in range(2):
        memset(P, "pool", QK[0][hd][64:68], -1.0, [qkb[0][hd]])
        memset(P, "pool", QK[1][hd][64:68], 1.0, [qkb[1][hd]])
    mark_in = P.top
    hblk = [P.alloc([128, 8, 512], BF16) for _ in range(2)]
    hbb = P.bufs(2)
    pss = [P.bank(i) for i in range(8)]
    psb = P.bufs(8)
    it = 0
    for blk in range(16):
        i = blk % 2
        tok = slice(blk * 512, (blk + 1) * 512)
        P.dma("sp", hblk[i], hT_blk(blk), writes=[hbb[i]])
        for which in range(2):
            for hd in range(2):
                cols = slice(128 + which * 128 + hd * 64, 128 + which * 128 + hd * 64 + 64)
                pi = it % 8
                it += 1
                for k in range(8):
                    mm(P, pss[pi][:64, :], W[:, k, cols], hblk[i][:, k, :], k == 0, k == 7, [b_W, hbb[i]], [psb[pi]])
                if which == 0:
                    act(P, QK[0][hd][0:64, tok], pss[pi][:64, :], AF.Copy, [psb[pi]], [qkb[0][hd]], scale=0.125)
                else:
                    cp(P, "dve", QK[1][hd][0:64, tok], pss[pi][:64, :], [psb[pi]], [qkb[1][hd]])
        pi = it % 8
        it += 1
        for k in range(8):
            mm(P, pss[pi][:2, :], W[:, k, 512:514], hblk[i][:, k, :], k == 0, k == 7, [b_W, hbb[i]], [psb[pi]])
        act(P, FL[:, tok], pss[pi][:2, :], AF.Identity, [psb[pi], b_fb], [b_FL], bias=fb[:, 0:1])
        for t4 in range(4):
            t = blk * 4 + t4
            pi = it % 8
            it += 1
            for k in range(8):
                mm(P, pss[pi][:, 0:128], hblk[i][:, k, t4 * 128:(t4 + 1) * 128], W[:, k, 384:512], k == 0, k == 7,
                   [b_W, hbb[i]], [psb[pi]])
            cp(P, "dve" if t4 % 2 else "act", V[:, t, :].rearrange("p (h d) -> p h d", d=65)[:, :, 0:64],
               pss[pi][:, 0:128].rearrange("p (h d) -> p h d", d=64), [psb[pi], b_vones], [vb[t]])
    P.barrier()
    if B_STOP == 1:
        return
    P.top = mark_in
    one2 = P.alloc([2, 1], F32)
    b_one2 = P.buf()
    memset(P, "dve", one2, 1.0, [b_one2])
    act(P, FL, FL, AF.Exp, [b_FL], [b_FL], scale=-1.0)
    act(P, FL, FL, AF.Ln, [b_FL], [b_FL], bias=1.0)
    ts(P, "dve", FL, FL, -1.0, None, ALU.mult, None, [b_FL], [b_FL])
    P.op("dve", lambda e: e.tensor_tensor_scan(out=FL, data0=one2.to_broadcast([2, NTOK]), data1=FL, initial=0.0,
                                               op0=ALU.mult, op1=ALU.add), [b_FL, b_one2], [b_FL])
    CH = 2048
    hrow = [[P.alloc([2, CH], BF16) for _ in range(3)] for _ in range(2)]
    hrb = [[P.buf() for _ in range(3)] for _ in range(2)]
    rres = [P.alloc([2, CH], F32) for _ in range(2)]
    rrb = P.bufs(2)
    FKb = P.alloc([128, 2, 64], F32)
    for cidx in range(NTOK // CH):
        i = cidx % 2
        sl = slice(cidx * CH, (cidx + 1) * CH)
        cp(P, "dve", hrow[i][0], FL[:, sl], [b_FL], [hrb[i][0]])
        tt(P, "dve", rres[i], FL[:, sl], hrow[i][0], ALU.subtract, [b_FL, hrb[i][0]], [rrb[i]])
        cp(P, "dve", hrow[i][1], rres[i], [rrb[i]], [hrb[i][1]])
        tt(P, "dve", rres[i], rres[i], hrow[i][1], ALU.subtract, [rrb[i], hrb[i][1]], [rrb[i]])
        cp(P, "dve", hrow[i][2], rres[i], [rrb[i]], [hrb[i][2]])
        for hd in range(2):
            P.dma("sp", QK[0][hd][64:65, sl], hrow[i][0][hd:hd + 1, :], reads=[hrb[i][0]], writes=[qkb[0][hd]])
            for j in range(3):
                P.dma("sp", QK[1][hd][65 + j:66 + j, sl], hrow[i][j][hd:hd + 1, :], reads=[hrb[i][j]],
                      writes=[qkb[1][hd]])
    P.barrier()
    if B_STOP == 2:
        return
    P.top = mark_in
    pT = [P.alloc([128, 512], BF16) for _ in range(4)]
    pTb = P.bufs(4)
    rs = [P.alloc([128, 2], F32) for _ in range(2)]
    rsb = P.bufs(2)
    yd = [P.alloc([128, 128], BF16) for _ in range(2)]
    ydb = P.bufs(2)
    psS = [P.bank(i) for i in range(4)]
    psSb = P.bufs(4)
    psO = [[P.bank(4), P.bank(5)], [P.bank(6), P.bank(7)]]
    psOb = [[P.buf(), P.buf()], [P.buf(), P.buf()]]
    items = []
    for qt in range(0 if not B_SKIP_FOX else 64, 64):
        for hd in range(2):
            for g0 in range(0, qt + 1, 4):
                items.append((qt, hd, list(range(g0, min(g0 + 4, qt + 1)))))
    ctr = {"n": 0}

    def emit_S(item):
        qt, hd, kts = item
        Q, K = QK[0][hd], QK[1][hd]
        qs = slice(qt * 128, (qt + 1) * 128)
        si = ctr["n"] % 4
        ctr["n"] += 1
        for jj, kt in enumerate(kts):
            mm(P, psS[si][:, jj * 128:(jj + 1) * 128], K[:, kt * 128:(kt + 1) * 128], Q[:, qs], True, kt != qt,
               [qkb[1][hd], qkb[0][hd]], [psSb[si]])
            if kt == qt:
                mm(P, psS[si][:, jj * 128:(jj + 1) * 128], C.identb, maskT, False, True,
                   [C.b_identb, b_mask], [psSb[si]])
        w = len(kts) * 128
        act(P, pT[si][:, 0:w], psS[si][:, 0:w], AF.Exp, [psSb[si]], [pTb[si]])
        return si

    def emit_PV(item, si):
        qt, hd, kts = item
        oi = qt % 2
        qs = slice(qt * 128, (qt + 1) * 128)
        ob = psO[oi][hd][:, 0:65]
        for jj, kt in enumerate(kts):
            mm(P, ob, pT[si][:, jj * 128:(jj + 1) * 128], V[:, kt, hd * 65:(hd + 1) * 65], kt == 0, kt == qt,
               [pTb[si], vb[kt]], [psOb[oi][hd]])
        if kts[-1] != qt:
            return
        P.op("dve", lambda e, oi=oi, hd=hd: e.reciprocal(out=rs[oi][:, hd:hd + 1], in_=psO[oi][hd][:, 64:65]),
             [psOb[oi][hd]], [rsb[oi]])
        ts(P, "dve", yd[oi][:, hd * 64:(hd + 1) * 64], psO[oi][hd][:, 0:64], rs[oi][:, hd:hd + 1], None, ALU.mult,
           None, [psOb[oi][hd], rsb[oi]], [ydb[oi]])
        if hd == 1:
            s2 = ctr["n"] % 4
            ctr["n"] += 1
            pv = psS[s2].bitcast(BF16)
            tr(P, pv[:, 0:128], yd[oi], C.identb, [ydb[oi], C.b_identb], [psSb[s2]])
            cp(P, "act", ydT[:, qs], pv[:, 0:128], [psSb[s2]], [b_ydT])

    if items:
        cur = emit_S(items[0])
        for n_ in range(len(items)):
            nxt = emit_S(items[n_ + 1]) if n_ + 1 < len(items) else None
            emit_PV(items[n_], cur)
            cur = nxt
    P.dma("sp", ydT_out, ydT, reads=[b_ydT], writes=[P.buf()])
    P.barrier()
    if B_STOP == 3:
        return
    P.top = mark_fox
    lam = P.alloc([128, 4, 3], F32)
    b_lam = P.buf()
    P.dma("sp", lam, io["lam"], writes=[b_lam])
    bS = P.alloc([128, 4, 2, 128], F32)
    b_bS = P.buf()
    P.dma("sp", bS, io["bS"], writes=[b_bS])
    cS = P.alloc([128, 4, 2, 128], F32)
    b_cS = P.buf()
    P.dma("sp", cS, io["cS"], writes=[b_cS])
    dcol = P.alloc([128, 1], F32)
    b_dcol = P.buf()
    P.dma("sp", dcol, io["dcol"], writes=[b_dcol])
    jrow = P.alloc([128, SB_T], F32)
    b_jrow = P.buf()
    P.dma("sp", jrow, io["jrow"], writes=[b_jrow])
    sp_ = P.alloc([128, 16, 4], F32)
    b_sp = P.buf()
    DT, LRE, R, TH, KRE, KIM, GRE, GIM, T0, T1, T2, T3, DEN, KT = [sp_[:, i, :] for i in range(14)]
    lre_in, lim_in, ldt_in = lam[:, :, 0], lam[:, :, 1], lam[:, :, 2]
    act(P, DT, ldt_in, AF.Exp, [b_lam], [b_sp])
    ts(P, "dve", LRE, lre_in, -1e-4, None, ALU.min, None, [b_lam], [b_sp])
    tt(P, "dve", T0, LRE, DT, ALU.mult, [b_sp], [b_sp])
    act(P, R, T0, AF.Exp, [b_sp], [b_sp])
    tt(P, "dve", TH, lim_in, DT, ALU.mult, [b_lam, b_sp], [b_sp])
    ts(P, "dve", TH, TH, 1.0 / TWO_PI, None, ALU.mult, None, [b_sp], [b_sp])
    if B_STOP == 5:
        P.barrier()
        return
    cosT = P.alloc([128, 4, SB_T], F32)
    sinT = P.alloc([128, 4, SB_T], F32)
    b_tab = P.buf()
    BD = P.alloc([128, 4, 2, 128], BF16)
    b_BD = P.buf()
    CB = P.alloc([128, 4, 2, 128], BF16)
    b_CB = P.buf()
    Y = P.alloc([128, 2, 4], F32)
    b_Y = P.buf()
    mark_tmp5 = P.top
    tmpa = P.alloc([128, SB_T], F32)
    tmpb = P.alloc([128, SB_T], F32)
    tmpi = P.alloc([128, SB_T], F32).bitcast(I32)
    b_ta, b_tb = P.buf(), P.buf()
    for st in range(4):
        ts(P, "dve", tmpa, jrow, TH[:, st:st + 1], None, ALU.mult, None, [b_jrow, b_sp], [b_ta])
        sin_table(P, sinT[:, st, :], b_tab, tmpa, b_ta, tmpb, tmpi, b_tb)
        ts(P, "dve", tmpa, jrow, TH[:, st:st + 1], 0.25, ALU.mult, ALU.add, [b_jrow, b_sp], [b_ta])
        sin_table(P, cosT[:, st, :], b_tab, tmpa, b_ta, tmpb, tmpi, b_tb)
    if B_STOP == 6:
        P.barrier()
        return
    ts(P, "dve", KT, TH, float(SB_T), None, ALU.mult, None, [b_sp], [b_sp])
    sin_table(P, KIM, b_sp, KT, b_sp, T1, T2.bitcast(I32), b_sp)
    ts(P, "dve", KT, TH, float(SB_T), 0.25, ALU.mult, ALU.add, [b_sp], [b_sp])
    sin_table(P, KRE, b_sp, KT, b_sp, T1, T2.bitcast(I32), b_sp)
    tt(P, "dve", KRE, KRE, R, ALU.mult, [b_sp], [b_sp])
    tt(P, "dve", KIM, KIM, R, ALU.mult, [b_sp], [b_sp])
    if B_STOP == 7:
        P.barrier()
        return
    tt(P, "dve", T0, cosT[:, :, 1], R, ALU.mult, [b_tab, b_sp], [b_sp])
    ts(P, "dve", T0, T0, 1.0, None, ALU.subtract, None, [b_sp], [b_sp])
    tt(P, "dve", T1, sinT[:, :, 1], R, ALU.mult, [b_tab, b_sp], [b_sp])
    tt(P, "dve", DEN, LRE, LRE, ALU.mult, [b_sp], [b_sp])
    tt(P, "dve", T2, lim_in, lim_in, ALU.mult, [b_lam], [b_sp])
    tt(P, "dve", DEN, DEN, T2, ALU.add, [b_sp], [b_sp])
    P.op("dve", lambda e: e.reciprocal(out=DEN, in_=DEN), [b_sp], [b_sp])
    tt(P, "dve", T2, T0, LRE, ALU.mult, [b_sp], [b_sp])
    tt(P, "dve", T3, T1, lim_in, ALU.mult, [b_sp, b_lam], [b_sp])
    tt(P, "dve", T2, T2, T3, ALU.add, [b_sp], [b_sp])
    tt(P, "dve", GRE, T2, DEN, ALU.mult, [b_sp], [b_sp])
    tt(P, "dve", T2, T1, LRE, ALU.mult, [b_sp], [b_sp])
    tt(P, "dve", T3, T0, lim_in, ALU.mult, [b_sp, b_lam], [b_sp])
    tt(P, "dve", T2, T2, T3, ALU.subtract, [b_sp], [b_sp])
    tt(P, "dve", GIM, T2, DEN, ALU.mult, [b_sp], [b_sp])
    if B_STOP == 8:
        P.barrier()
        return
    bbf = P.alloc([128, 128], F32)
    b_bbf = P.buf()
    bbb = P.alloc([128, 128], BF16)
    b_bbb = P.buf()
    pst = P.bank(0)
    b_pst = P.buf()
    pstv = pst.bitcast(BF16)
    for st in range(4):
        for ri in range(2):
            a_, b__ = (bS[:, st, 0, :], bS[:, st, 1, :]) if ri == 0 else (bS[:, st, 1, :], bS[:, st, 0, :])
            ts(P, "dve", bbf, b__, GIM[:, st:st + 1], None, ALU.mult, None, [b_bS, b_sp], [b_bbf])
            stt(P, "dve", bbb, a_, GRE[:, st:st + 1], bbf, ALU.mult, ALU.subtract if ri == 0 else ALU.add,
                [b_bS, b_sp, b_bbf], [b_bbb])
            tr(P, pstv[:, 0:128], bbb, C.identb, [b_bbb, C.b_identb], [b_pst])
            cp(P, "act", BD[:, st, ri, :], pstv[:, 0:128], [b_pst], [b_BD])
        cp(P, "dve", CB[:, st, 0, :], cS[:, st, 0, :], [b_cS], [b_CB])
        ts(P, "dve", CB[:, st, 1, :], cS[:, st, 1, :], -1.0, None, ALU.mult, None, [b_cS], [b_CB])
    memset(P, "dve", Y, 0.0, [b_Y])
    P.barrier()
    if B_STOP == 4:
        return
    P.top = mark_tmp5
    hblk = [P.alloc([128, 8, 512], BF16) for _ in range(2)]
    hbb = P.bufs(2)
    uB = [P.alloc([128, SB_T], BF16) for _ in range(2)]
    uBb = P.bufs(2)
    uF = [P.alloc([128, SB_T], F32) for _ in range(2)]
    uFb = P.bufs(2)
    NB = 2
    BuR = [P.alloc([128, SB_T], F32) for _ in range(NB)]
    BuI = [P.alloc([128, SB_T], F32) for _ in range(NB)]
    t1 = [P.alloc([128, SB_T], F32) for _ in range(NB)]
    t2 = [P.alloc([128, SB_T], F32) for _ in range(NB)]
    t3 = [P.alloc([128, SB_T], F32) for _ in range(NB)]
    t4 = [P.alloc([128, SB_T], F32) for _ in range(NB)]
    bub, t1b, t2b, t3b, t4b = P.bufs(NB), P.bufs(NB), P.bufs(NB), P.bufs(NB), P.bufs(NB)
    xr = [P.alloc([128, 4, SB_T], BF16) for _ in range(2)]
    xi = [P.alloc([128, 4, SB_T], BF16) for _ in range(2)]
    xrb = [P.bufs(4) for _ in range(2)]
    yg = [P.alloc([128, SB_T], F32) for _ in range(2)]
    ygb = P.bufs(2)
    yo = [P.alloc([128, SB_T], BF16) for _ in range(2)]
    yob = P.bufs(2)
    sm4 = P.alloc([128, 8], F32)
    b_sm4 = P.buf()
    psu = [P.bank(0), P.bank(1)]
    psub = P.bufs(2)
    psB = [P.bank(2), P.bank(3), P.bank(4), P.bank(5)]
    psBb = P.bufs(4)
    psy = [P.bank(6), P.bank(7)]
    psyb = P.bufs(2)
    nb = 0
    nbu = 0
    b_ygout = P.buf()
    for sbk in range(NTOK // SB_T):
        si = sbk % 2
        for half in range(2):
            blk = sbk * 2 + half
            i = blk % 2
            P.dma("sp", hblk[i], hT_blk(blk), writes=[hbb[i]])
            for k in range(8):
                mm(P, psu[i], W[:, k, 0:128], hblk[i][:, k, :], k == 0, k == 7, [b_W, hbb[i]], [psub[i]])
            cp(P, "dve", uF[si][:, half * 512:(half + 1) * 512], psu[i], [psub[i]], [uFb[si]])
            cp(P, "pool", uB[si][:, half * 512:(half + 1) * 512], uF[si][:, half * 512:(half + 1) * 512],
               [uFb[si]], [uBb[si]])
        if B_STOP == 9:
            P.barrier()
            return
        for st in range(4):
            bi_ = nb % NB
            nb += 1
            for ri, dst in ((0, BuR[bi_]), (1, BuI[bi_])):
                for half in range(2):
                    pi = nbu % 4
                    nbu += 1
                    mm(P, psB[pi], BD[:, st, ri, :], uB[si][:, half * 512:(half + 1) * 512], True, True,
                       [b_BD, uBb[si]], [psBb[pi]])
                    cp(P, "act", dst[:, half * 512:(half + 1) * 512], psB[pi], [psBb[pi]], [bub[bi_]])
            cs, sn = cosT[:, st, :], sinT[:, st, :]
            tt(P, "pool", t1[bi_], cs, BuR[bi_], ALU.mult, [b_tab, bub[bi_]], [t1b[bi_]])
            tt(P, "dve", t2[bi_], sn, BuI[bi_], ALU.mult, [b_tab, bub[bi_]], [t2b[bi_]])
            tt(P, "dve", t1[bi_], t1[bi_], t2[bi_], ALU.add, [t1b[bi_], t2b[bi_]], [t1b[bi_]])
            tt(P, "pool", t3[bi_], cs, BuI[bi_], ALU.mult, [b_tab, bub[bi_]], [t3b[bi_]])
            tt(P, "pool", t4[bi_], sn, BuR[bi_], ALU.mult, [b_tab, bub[bi_]], [t4b[bi_]])
            tt(P, "dve", t3[bi_], t3[bi_], t4[bi_], ALU.subtract, [t3b[bi_], t4b[bi_]], [t3b[bi_]])
            tt(P, "dve", t1[bi_][:, 0:1], t1[bi_][:, 0:1], Y[:, 0, st:st + 1], ALU.add, [t1b[bi_], b_Y], [t1b[bi_]])
            tt(P, "dve", t3[bi_][:, 0:1], t3[bi_][:, 0:1], Y[:, 1, st:st + 1], ALU.add, [t3b[bi_], b_Y], [t3b[bi_]])
            P.op("dve", lambda e, bi_=bi_, st=st: e.tensor_tensor_scan(out=t1[bi_], data0=R[:, st:st + 1].to_broadcast([128, SB_T]), data1=t1[bi_],
                                                                         initial=0.0, op0=ALU.mult, op1=ALU.add),
                 [t1b[bi_], b_sp], [t1b[bi_]])
            P.op("dve", lambda e, bi_=bi_, st=st: e.tensor_tensor_scan(out=t3[bi_], data0=R[:, st:st + 1].to_broadcast([128, SB_T]), data1=t3[bi_],
                                                                         initial=0.0, op0=ALU.mult, op1=ALU.add),
                 [t3b[bi_], b_sp], [t3b[bi_]])
            er, ei = t1[bi_][:, SB_T - 1:SB_T], t3[bi_][:, SB_T - 1:SB_T]
            ts(P, "dve", sm4[:, 0:1], ei, KIM[:, st:st + 1], None, ALU.mult, None, [t3b[bi_], b_sp], [b_sm4])
            stt(P, "dve", Y[:, 0, st:st + 1], er, KRE[:, st:st + 1], sm4[:, 0:1], ALU.mult, ALU.subtract,
                [t1b[bi_], b_sp, b_sm4], [b_Y])
            ts(P, "dve", sm4[:, 1:2], er, KIM[:, st:st + 1], None, ALU.mult, None, [t1b[bi_], b_sp], [b_sm4])
            stt(P, "dve", Y[:, 1, st:st + 1], ei, KRE[:, st:st + 1], sm4[:, 1:2], ALU.mult, ALU.add,
                [t3b[bi_], b_sp, b_sm4], [b_Y])
            tt(P, "pool", t2[bi_], cs, t1[bi_], ALU.mult, [b_tab, t1b[bi_]], [t2b[bi_]])
            tt(P, "dve", t4[bi_], sn, t3[bi_], ALU.mult, [b_tab, t3b[bi_]], [t4b[bi_]])
            tt(P, "dve", xr[si][:, st, :], t2[bi_], t4[bi_], ALU.subtract, [t2b[bi_], t4b[bi_]], [xrb[si][st]])
            tt(P, "pool", t2[bi_], sn, t1[bi_], ALU.mult, [b_tab, t1b[bi_]], [t2b[bi_]])
            tt(P, "dve", t4[bi_], cs, t3[bi_], ALU.mult, [b_tab, t3b[bi_]], [t4b[bi_]])
            tt(P, "dve", xi[si][:, st, :], t2[bi_], t4[bi_], ALU.add, [t2b[bi_], t4b[bi_]], [xrb[si][st]])
            if B_STOP == 10:
                P.barrier()
                return
        for half in range(2):
            hs = slice(half * 512, (half + 1) * 512)
            pi = half
            for st in range(4):
                mm(P, psy[pi], CB[:, st, 0, :], xr[si][:, st, hs], st == 0, False, [b_CB, xrb[si][st]], [psyb[pi]])
                mm(P, psy[pi], CB[:, st, 1, :], xi[si][:, st, hs], False, st == 3, [b_CB, xrb[si][st]], [psyb[pi]])
            stt(P, "dve", yg[si][:, hs], uF[si][:, hs], dcol[:, 0:1], psy[pi], ALU.mult, ALU.add,
                [uFb[si], b_dcol, psyb[pi]], [ygb[si]])
        g_ = t2[0]
        act(P, g_, yg[si], AF.Square, [ygb[si]], [t2b[0]])
        ts(P, "dve", g_, g_, 0.044715, 1.0, ALU.mult, ALU.add, [t2b[0]], [t2b[0]])
        tt(P, "dve", g_, g_, yg[si], ALU.mult, [t2b[0], ygb[si]], [t2b[0]])
        act(P, g_, g_, AF.Sigmoid, [t2b[0]], [t2b[0]], scale=1.5957691216057308)
        tt(P, "pool", yo[si], g_, yg[si], ALU.mult, [t2b[0], ygb[si]], [yob[si]])
        P.dma("sp", ygT_out[:, sbk * SB_T:(sbk + 1) * SB_T], yo[si], reads=[yob[si]], writes=[b_ygout])
        if B_STOP == 11:
            P.barrier()
            return
    P.barrier()
    P.top = base


def build_stage_b():
    nc = bass.Bass("TRN2", target_bir_lowering=False)
    io = {}
    io["ident"] = dram_in(nc, "ident", [128, 128])
    io["ccol"] = dram_in(nc, "ccol", [128, 8])
    io["hT"] = dram_in(nc, "hT", [128, 8, 8192], BF16)
    io["hT_blk"] = lambda blk: io["hT"][:, :, blk * 512:(blk + 1) * 512]
    io["w"] = dram_in(nc, "w", [1024, 514])
    io["fb"] = dram_in(nc, "fb", [2, 1])
    io["maskT"] = dram_in(nc, "maskT", [128, 128], BF16)
    io["lam"] = dram_in(nc, "lam", [128, 4, 3])
    io["bS"] = dram_in(nc, "bS", [128, 4, 2, 128])
    io["cS"] = dram_in(nc, "cS", [128, 4, 2, 128])
    io["dcol"] = dram_in(nc, "dcol", [128, 1])
    io["jrow"] = dram_in(nc, "jrow", [128, SB_T])
    ygT = nc.dram_tensor("ygT", [128, 8192], BF16, kind="ExternalOutput").ap()
    ydT = nc.dram_tensor("ydT", [128, 8192], BF16, kind="ExternalOutput").ap()
    P = Prog(nc)
    C = phase_consts(P, io)
    P.barrier()
    stage_b_body(P, C, io, ygT, ydT)
    P.emit(final_waits=[P.last[k] for k in P.last if k.startswith("dma:")])
    P.close()
    return nc


def prep_stage_b(inp, h1T):
    c = _f32(inp["c"])
    W = np.asarray(inp["odd_w_in"][0], np.float32)
    k_ = np.arange(128)[:, None]
    q_ = np.arange(128)[None, :]
    maskT = np.where(k_ <= q_, 0.0, NEG).astype(ml_dtypes.bfloat16)
    maps = []
    for core in range(8):
        b, j = core // 4, core % 4
        hT = np.concatenate(h1T[b * 4:(b + 1) * 4], axis=2)
        cols = np.concatenate([np.arange(128 * j, 128 * j + 128), 512 + np.arange(128 * j, 128 * j + 128),
                               1024 + np.arange(128 * j, 128 * j + 128), 1536 + np.arange(128 * j, 128 * j + 128),
                               2048 + np.arange(2 * j, 2 * j + 2)])
        gs = np.arange(8 * j, 8 * j + 8)
        lam = np.zeros((128, 4, 3), np.float32)
        bS = np.zeros((128, 4, 2, 128), np.float32)
        cS = np.zeros((128, 4, 2, 128), np.float32)
        for st in range(4):
            for hh in range(2):
                gl = 2 * st + hh
                g = gs[gl]
                ps_ = slice(hh * 64, (hh + 1) * 64)
                lam[ps_, st, 0] = inp["ssm_lam_re"][0, g]
                lam[ps_, st, 1] = inp["ssm_lam_im"][0, g]
                lam[ps_, st, 2] = inp["ssm_log_dt"][0, g]
                bS[ps_, st, 0, 16 * gl:16 * gl + 16] = inp["ssm_b_re"][0, g]
                bS[ps_, st, 1, 16 * gl:16 * gl + 16] = inp["ssm_b_im"][0, g]
                cS[ps_, st, 0, 16 * gl:16 * gl + 16] = np.asarray(inp["ssm_c_re"][0, g]).T
                cS[ps_, st, 1, 16 * gl:16 * gl + 16] = np.asarray(inp["ssm_c_im"][0, g]).T
        maps.append({
            "ident": np.eye(128, dtype=np.float32),
            "ccol": _colT(c[b], 8),
            "hT": np.ascontiguousarray(hT),
            "w": _f32(W[:, cols]),
            "fb": _f32(np.asarray(inp["forget_b"][0, 2 * j:2 * j + 2]).reshape(2, 1)),
            "maskT": maskT,
            "lam": lam, "bS": bS, "cS": cS,
            "dcol": _f32(np.asarray(inp["ssm_d"][0, gs]).reshape(128, 1)),
            "jrow": _f32(np.broadcast_to(np.arange(SB_T, dtype=np.float32)[None], (128, SB_T))),
        })
    return maps


def build_stage_c():
    nc = bass.Bass("TRN2", target_bir_lowering=False)
    io = {}
    io["ustrict"] = dram_in(nc, "ustrict", [128, 128], BF16)
    io["iotaC"] = dram_in(nc, "iotaC", [128, NE])
    io["ident"] = dram_in(nc, "ident", [128, 128])
    io["ccol"] = dram_in(nc, "ccol", [128, 8])
    io["x1"] = dram_in(nc, "x1", [2048, 1024])
    io["ygT"] = dram_in(nc, "ygT", [128, 4, 2048], BF16)
    io["ydT"] = dram_in(nc, "ydT", [128, 4, 2048], BF16)
    io["mod_w"] = dram_in(nc, "mod_w", [2, 1024, 3072])
    io["mod_bT"] = dram_in(nc, "mod_bT", [2, 128, 24])
    io["ln_g"] = dram_in(nc, "ln_g", [2, 1024])
    io["ln_b"] = dram_in(nc, "ln_b", [2, 1024])
    io["glu_w"] = dram_in(nc, "glu_w", [512, 512])
    io["glu_bT"] = dram_in(nc, "glu_bT", [128, 4])
    io["odd_w_out"] = dram_in(nc, "odd_w_out", [1024, 1024])
    io["router_w"] = dram_in(nc, "router_w", [1, 1024, 32])
    io["router_b"] = dram_in(nc, "router_b", [1, 32])
    io["exp_w_gu"] = dram_in(nc, "exp_w_gu", [1, 32, 1024, 2048])
    io["b_guT"] = dram_in(nc, "b_guT", [1, 128, 16, 32])
    io["exp_w_down"] = dram_in(nc, "exp_w_down", [1, 32, 1024, 1024])
    io["exp_b_down"] = dram_in(nc, "exp_b_down", [1, 32, 1024])
    xmid = nc.dram_tensor("xmid", [2048, 1024], F32, kind="ExternalOutput").ap()
    xout = nc.dram_tensor("xout", [2048, 1024], F32, kind="ExternalOutput").ap()
    scr = nc.dram_tensor("scr_gate", [2, 1024], F32, kind="ExternalOutput").ap()
    P = Prog(nc)
    C = phase_consts(P, io)
    P.barrier()
    stage_c_body(P, C, io, scr, xmid, xout, 0, 0)
    P.emit(final_waits=[P.last[k] for k in P.last if k.startswith("dma:")])
    P.close()
    return nc


def stage_c_body(P, C, io, scr, xmid, xout, mi, layer):
    base = P.top
    M = phase_mod(P, C, io["mod_w"][mi], io["mod_bT"][mi], scr[0], io["ln_g"][mi], io["ln_b"][mi])
    yT = P.alloc([128, 8, 2048], BF16)
    b_yT = P.buf()
    mark = P.top
    yg = P.alloc([128, 4, 2048], BF16)
    b_yg = P.buf()
    P.dma("sp", yg, io["ygT"], writes=[b_yg])
    P.dma("sp", yT[:, 4:8, :], io["ydT"], writes=[b_yT])
    gw = P.alloc([128, 4, 512], BF16)
    b_gw = P.buf()
    P.dma("pool", gw, io["glu_w"].rearrange("(k p) n -> p k n", p=128), writes=[b_gw])
    gb = P.alloc([128, 4], F32)
    b_gb = P.buf()
    P.dma("sp", gb, io["glu_bT"], writes=[b_gb])
    sg = [P.alloc([128, 512], F32) for _ in range(2)]
    sgb = P.bufs(2)
    pss = [P.bank(0), P.bank(1)]
    psb = P.bufs(2)
    n = 0
    for ct in range(4):
        for blk in range(4):
            i = n % 2
            n += 1
            tok = slice(blk * 512, (blk + 1) * 512)
            for ci in range(4):
                mm(P, pss[i], gw[:, ci, ct * 128:(ct + 1) * 128], yg[:, ci, tok], ci == 0, ci == 3, [b_gw, b_yg],
                   [psb[i]])
            act(P, sg[i], pss[i], AF.Sigmoid, [psb[i], b_gb], [sgb[i]], bias=gb[:, ct:ct + 1])
            tt(P, "dve", yT[:, ct, tok], yg[:, ct, tok], sg[i], ALU.mult, [b_yg, sgb[i]], [b_yT])
    P.barrier()
    P.top = mark
    b_xmid = P.buf()
    phase_outproj_ln(P, C, M, yT, b_yT, io["odd_w_out"], io["x1"], xmid, b_xmid)
    P.barrier()
    P.top = base
    M2 = phase_mod(P, C, io["mod_w"][mi + 1], io["mod_bT"][mi + 1], scr[1], io["ln_g"][mi + 1], io["ln_b"][mi + 1])
    b_xout = P.buf()
    phase_moe(P, C, M2, xmid, xout, b_xout, io, layer)
    P.barrier()
    P.top = base


def prep_stage_c(inp, x1, ygT, ydT):
    c = _f32(inp["c"])
    common = {
        **_moe_consts(),
        "ident": np.eye(128, dtype=np.float32),
        "mod_w": _f32(inp["mod_w"][1]),
        "mod_bT": _f32(np.stack([_colT(inp["mod_b"][1, j], 24) for j in range(2)])),
        "ln_g": _f32(inp["ln_g"][1]),
        "ln_b": _f32(inp["ln_b"][1]),
        "glu_w": _f32(inp["ssm_glu_w"][0]),
        "glu_bT": _colT(inp["ssm_glu_b"][0], 4),
        "odd_w_out": _f32(inp["odd_w_out"][0]),
        "router_w": _f32(inp["router_w"][1:2]),
        "router_b": _f32(inp["router_b"][1:2]),
        "exp_w_gu": _f32(inp["exp_w_gu"][1:2]),
        "b_guT": _f32(np.asarray(inp["exp_b_gu"][1]).reshape(32, 16, 128).transpose(2, 1, 0)[None]),
        "exp_w_down": _f32(inp["exp_w_down"][1:2]),
        "exp_b_down": _f32(inp["exp_b_down"][1:2]),
    }
    maps = []
    for core in range(8):
        b, seg = core // 4, core % 4
        tok = slice(seg * 2048, (seg + 1) * 2048)
        m = dict(common)
        m["ccol"] = _colT(c[b], 8)
        m["x1"] = _f32(x1[core])
        m["ygT"] = np.ascontiguousarray(np.stack([ygT[b * 4 + j][:, tok] for j in range(4)], axis=1))
        m["ydT"] = np.ascontiguousarray(np.stack([ydT[b * 4 + j][:, tok] for j in range(4)], axis=1))
        maps.append(m)
    return maps


_NC_CACHE = {}


def _get(name, fn):
    if name not in _NC_CACHE:
        _NC_CACHE[name] = fn()
    return _NC_CACHE[name]


def kernel(**inputs):
    if FUSED:
        return kernel_fused(**inputs)
    inp = {k: np.asarray(v) for k, v in inputs.items()}
    cores = list(range(8))
    ra = run_bass_kernel_spmd(_get("a", build_stage_a), prep_stage_a(inp), core_ids=cores).results
    x1 = [r["xout"] for r in ra]
    h1T = [r["h1T"] for r in ra]
    rb = run_bass_kernel_spmd(_get("b", build_stage_b), prep_stage_b(inp, h1T), core_ids=cores).results
    ygT = [r["ygT"] for r in rb]
    ydT = [r["ydT"] for r in rb]
    rc = run_bass_kernel_spmd(_get("c", build_stage_c), prep_stage_c(inp, x1, ygT, ydT), core_ids=cores).results
    out = np.stack([r["xout"] for r in rc]).reshape(2, 8192, 1024).astype(np.float32)
    return out


def build_fused():
    nc = bass.Bass("TRN2", target_bir_lowering=False)
    io = {}
    io["ustrict"] = dram_in(nc, "ustrict", [128, 128], BF16)
    io["iotaC"] = dram_in(nc, "iotaC", [128, NE])
    io["ident"] = dram_in(nc, "ident", [128, 128])
    io["ccol"] = dram_in(nc, "ccol", [128, 8])
    io["xs"] = dram_in(nc, "xs", [4, 2560, 1024])
    io["flags"] = dram_in(nc, "flags", [4, 128, 2])
    io["icnts"] = dram_in(nc, "icnts", [4, 128, 4, 16])
    io["sel"] = dram_in(nc, "sel", [128, 4])
    io["mod_w"] = dram_in(nc, "mod_w", [4, 1024, 3072])
    io["mod_bT"] = dram_in(nc, "mod_bT", [4, 128, 24])
    io["ln_g"] = dram_in(nc, "ln_g", [4, 1024])
    io["ln_b"] = dram_in(nc, "ln_b", [4, 1024])
    io["even_w_in"] = dram_in(nc, "even_w_in", [1024, 2048])
    io["pool_scaleT"] = dram_in(nc, "pool_scaleT", [128, 4])
    io["pool_w"] = dram_in(nc, "pool_w", [4, 128, 128])
    io["biasT"] = dram_in(nc, "biasT", [128, 8, 5, 256], BF16)
    io["even_w_out"] = dram_in(nc, "even_w_out", [1024, 1024])
    io["router_w"] = dram_in(nc, "router_w", [2, 1024, 32])
    io["router_b"] = dram_in(nc, "router_b", [2, 32])
    io["exp_w_gu"] = dram_in(nc, "exp_w_gu", [2, 32, 1024, 2048])
    io["b_guT"] = dram_in(nc, "b_guT", [2, 128, 16, 32])
    io["exp_w_down"] = dram_in(nc, "exp_w_down", [2, 32, 1024, 1024])
    io["exp_b_down"] = dram_in(nc, "exp_b_down", [2, 32, 1024])
    io["wB"] = dram_in(nc, "wB", [4, 1024, 514])
    io["fbB"] = dram_in(nc, "fbB", [4, 2, 1])
    io["maskT"] = dram_in(nc, "maskT", [128, 128], BF16)
    io["lamB"] = dram_in(nc, "lamB", [4, 128, 4, 3])
    io["bSB"] = dram_in(nc, "bSB", [4, 128, 4, 2, 128])
    io["cSB"] = dram_in(nc, "cSB", [4, 128, 4, 2, 128])
    io["dcolB"] = dram_in(nc, "dcolB", [4, 128, 1])
    io["jrow"] = dram_in(nc, "jrow", [128, SB_T])
    io["glu_w"] = dram_in(nc, "glu_w", [512, 512])
    io["glu_bT"] = dram_in(nc, "glu_bT", [128, 4])
    io["odd_w_out"] = dram_in(nc, "odd_w_out", [1024, 1024])
    out = nc.dram_tensor("out", [2048, 1024], F32, kind="ExternalOutput").ap()

    def scratch(name, shape, dt=F32):
        return nc.dram_tensor(name, list(shape), dt, kind="Internal").ap()
    scr = scratch("scr_gate", [3, 1024])
    xmid = scratch("xmid", [2048, 1024])
    x1_all = scratch("x1_all", [4, 2048, 1024])
    h1T_all = scratch("h1T_all", [4, 128, 8, 2048], BF16)
    ygT_all = scratch("ygT_all", [4, 128, 8192], BF16)
    ydT_all = scratch("ydT_all", [4, 128, 8192], BF16)
    x1_own = scratch("x1_own", [2048, 1024])
    ygT_own = scratch("ygT_own", [128, 4, 2048], BF16)
    ydT_own = scratch("ydT_own", [128, 4, 2048], BF16)
    P = Prog(nc)
    C = phase_consts(P, io)
    P.barrier()
    for seg in range(4):
        ioa = dict(io)
        ioa["x"] = io["xs"][seg]
        ioa["flag"] = io["flags"][seg]
        ioa["icnt"] = io["icnts"][seg]
        stage_a_body(P, C, ioa, scr, xmid, x1_all[seg], h1T_all[seg])
        P.barrier()
    for j in range(4):
        iob = dict(io)
        iob["hT_blk"] = lambda blk: h1T_all[blk // 4][:, :, (blk % 4) * 512:(blk % 4 + 1) * 512]
        iob["w"], iob["fb"], iob["lam"] = io["wB"][j], io["fbB"][j], io["lamB"][j]
        iob["bS"], iob["cS"], iob["dcol"] = io["bSB"][j], io["cSB"][j], io["dcolB"][j]
        stage_b_body(P, C, iob, ygT_all[j], ydT_all[j])
        P.barrier()
    base = P.top
    sel = P.alloc([128, 4], F32)
    b_sel = P.buf()
    P.dma("sp", sel, io["sel"], writes=[b_sel])
    tl = [[P.alloc([128, 1024], F32) for _ in range(4)] for _ in range(2)]
    tlb = [P.bufs(4) for _ in range(2)]
    acc = [P.alloc([128, 1024], F32) for _ in range(2)]
    accb = P.bufs(2)
    b_own = P.buf()
    for t in range(16):
        i = t % 2
        for sg_ in range(4):
            P.dma("sp", tl[i][sg_], x1_all[sg_][t * 128:(t + 1) * 128, :], writes=[tlb[i][sg_]])
        ts(P, "dve", acc[i], tl[i][0], sel[:, 0:1], None, ALU.mult, None, [tlb[i][0], b_sel], [accb[i]])
        for sg_ in range(1, 4):
            stt(P, "dve", acc[i], tl[i][sg_], sel[:, sg_:sg_ + 1], acc[i], ALU.mult, ALU.add,
                [tlb[i][sg_], b_sel, accb[i]], [accb[i]])
        P.dma("sp", x1_own[t * 128:(t + 1) * 128, :], acc[i], reads=[accb[i]], writes=[b_own])
    P.barrier()
    P.top = base
    sel = P.alloc([128, 4], F32)
    b_sel = P.buf()
    P.dma("sp", sel, io["sel"], writes=[b_sel])
    tb2 = [[P.alloc([128, 2048], BF16) for _ in range(4)] for _ in range(2)]
    tb2b = [P.bufs(4) for _ in range(2)]
    ac2 = [P.alloc([128, 2048], F32) for _ in range(2)]
    ac2b = P.bufs(2)
    ao2 = [P.alloc([128, 2048], BF16) for _ in range(2)]
    ao2b = P.bufs(2)
    n = 0
    for src_all, dst in ((ygT_all, ygT_own), (ydT_all, ydT_own)):
        for j in range(4):
            i = n % 2
            n += 1
            for sg_ in range(4):
                P.dma("sp", tb2[i][sg_], src_all[j][:, sg_ * 2048:(sg_ + 1) * 2048], writes=[tb2b[i][sg_]])
            ts(P, "dve", ac2[i], tb2[i][0], sel[:, 0:1], None, ALU.mult, None, [tb2b[i][0], b_sel], [ac2b[i]])
            for sg_ in range(1, 4):
                stt(P, "dve", ac2[i], tb2[i][sg_], sel[:, sg_:sg_ + 1], ac2[i], ALU.mult, ALU.add,
                    [tb2b[i][sg_], b_sel, ac2b[i]], [ac2b[i]])
            cp(P, "act", ao2[i], ac2[i], [ac2b[i]], [ao2b[i]])
            P.dma("sp", dst[:, j, :], ao2[i], reads=[ao2b[i]], writes=[b_own])
    P.barrier()
    P.top = base
    ioc = dict(io)
    ioc["x1"], ioc["ygT"], ioc["ydT"] = x1_own, ygT_own, ydT_own
    stage_c_body(P, C, ioc, scr, xmid, out, 2, 1)
    P.barrier()
    P.emit(final_waits=[P.last[k] for k in P.last if k.startswith("dma:")])
    P.close()
    return nc


def prep_fused(inp):
    x = _f32(inp["x"])
    c = _f32(inp["c"])
    pa = prep_stage_a(inp)
    dummy_h = [np.zeros((128, 8, 2048), ml_dtypes.bfloat16)] * 8
    pb = prep_stage_b(inp, dummy_h)
    mods = [(0, 0), (0, 1), (1, 0), (1, 1)]
    common = {
        **_moe_consts(),
        "ident": np.eye(128, dtype=np.float32),
        "mod_w": _f32(np.stack([inp["mod_w"][l, j] for l, j in mods])),
        "mod_bT": _f32(np.stack([_colT(inp["mod_b"][l, j], 24) for l, j in mods])),
        "ln_g": _f32(np.stack([inp["ln_g"][l, j] for l, j in mods])),
        "ln_b": _f32(np.stack([inp["ln_b"][l, j] for l, j in mods])),
        "even_w_in": pa[0]["even_w_in"], "pool_scaleT": pa[0]["pool_scaleT"], "pool_w": pa[0]["pool_w"],
        "biasT": pa[0]["biasT"], "even_w_out": pa[0]["even_w_out"],
        "router_w": _f32(inp["router_w"]), "router_b": _f32(inp["router_b"]),
        "exp_w_gu": _f32(inp["exp_w_gu"]),
        "b_guT": _f32(np.stack([np.asarray(inp["exp_b_gu"][l]).reshape(32, 16, 128).transpose(2, 1, 0)
                                for l in range(2)])),
        "exp_w_down": _f32(inp["exp_w_down"]), "exp_b_down": _f32(inp["exp_b_down"]),
        "wB": _f32(np.stack([pb[j]["w"] for j in range(4)])),
        "fbB": _f32(np.stack([pb[j]["fb"] for j in range(4)])),
        "maskT": pb[0]["maskT"],
        "lamB": _f32(np.stack([pb[j]["lam"] for j in range(4)])),
        "bSB": _f32(np.stack([pb[j]["bS"] for j in range(4)])),
        "cSB": _f32(np.stack([pb[j]["cS"] for j in range(4)])),
        "dcolB": _f32(np.stack([pb[j]["dcol"] for j in range(4)])),
        "jrow": pb[0]["jrow"],
        "glu_w": _f32(inp["ssm_glu_w"][0]), "glu_bT": _colT(inp["ssm_glu_b"][0], 4),
        "odd_w_out": _f32(inp["odd_w_out"][0]),
    }
    maps = []
    for core in range(8):
        b, seg = core // 4, core % 4
        m = dict(common)
        m["ccol"] = _colT(c[b], 8)
        m["xs"] = np.stack([pa[b * 4 + s]["x"] for s in range(4)])
        m["flags"] = np.stack([pa[b * 4 + s]["flag"] for s in range(4)])
        m["icnts"] = np.stack([pa[b * 4 + s]["icnt"] for s in range(4)])
        sel = np.zeros((128, 4), np.float32)
        sel[:, seg] = 1.0
        m["sel"] = sel
        maps.append(m)
    return maps


FUSED = True


def kernel_fused(**inputs):
    inp = {k: np.asarray(v) for k, v in inputs.items()}
    res = run_bass_kernel_spmd(_get("f", build_fused), prep_fused(inp), core_ids=list(range(8))).results
    return np.stack([r["out"] for r in res]).reshape(2, 8192, 1024).astype(np.float32)


CAP = 768
NCH = CAP // 128
U32 = mybir.dt.uint32
SPARSE_MOE = True


def phase_moe_sparse(P, C, M, x_dram, out_dram, b_out, io, layer, xd, yd):
    rw, rb = io["router_w"][layer], io["router_b"][layer]
    wgu, bguT = io["exp_w_gu"][layer], io["b_guT"][layer]
    wdn, bdn = io["exp_w_down"][layer], io["exp_b_down"][layer]
    BOUND = NE * CAP - 1
    mark0 = P.top
    rwt = P.alloc([128, 8, NE], BF16)
    b_rw = P.buf()
    P.dma("pool", rwt, rw.rearrange("(k p) n -> p k n", p=128), writes=[b_rw])
    rbc = P.alloc([128, NE], F32)
    b_rb = P.buf()
    P.dma("sp", rbc, rb.partition_broadcast(128), writes=[b_rb])
    bdt = P.alloc([32, 1024], F32)
    b_bd = P.buf()
    P.dma("sp", bdt, bdn, writes=[b_bd])
    bgt = P.alloc([128, 16, NE], F32)
    b_bg = P.buf()
    P.dma("sp", bgt, bguT, writes=[b_bg])
    ts(P, "dve", bgt[:, 8:16, :], bgt[:, 8:16, :], 1.0, None, ALU.add, None, [b_bg], [b_bg])
    ustr = P.alloc([128, 128], BF16)
    b_us = P.buf()
    P.dma("sp", ustr, io["ustrict"], writes=[b_us])
    ones = P.alloc([128, 128], BF16)
    b_ones = P.buf()
    memset(P, "dve", ones, 1.0, [b_ones])
    iotaC = P.alloc([128, NE], F32)
    b_io = P.buf()
    P.dma("sp", iotaC, io["iotaC"], writes=[b_io])
    G = P.alloc([128, 16, NE], F32)
    b_G = P.buf()
    GT = P.alloc([32, 2048], F32)
    b_GT = P.buf()
    destu = P.alloc([128, 64], F32).bitcast(U32)
    b_dest = P.bufs(16)
    gk = P.alloc([128, 16, 4], F32)
    b_gk = P.bufs(16)
    maccf = P.alloc([128, NE], F32)
    maccb = P.alloc([128, NE], BF16)
    b_macc = P.buf()
    memset(P, "dve", maccf, 0.0, [b_macc])
    memset(P, "dve", maccb, 0.0, [b_macc])
    b_xd = P.buf()
    mark1 = P.top
    xt = [P.alloc([128, 1024], F32) for _ in range(2)]
    xb = P.bufs(2)
    hTt = [P.alloc([128, 8, 128], BF16) for _ in range(2)]
    hTb = P.bufs(2)
    lg = [P.alloc([128, NE], F32) for _ in range(2)]
    lgb = P.bufs(2)
    m8 = [P.alloc([128, 8], F32) for _ in range(2)]
    m8b = P.bufs(2)
    ex = [P.alloc([128, NE], F32) for _ in range(2)]
    exb = P.bufs(2)
    mk = [P.alloc([128, NE], BF16) for _ in range(2)]
    mkb = P.bufs(2)
    Dt = [P.alloc([128, NE], F32) for _ in range(2)]
    Dtb = P.bufs(2)
    ov = [P.alloc([128, NE], F32) for _ in range(2)]
    ovb = P.bufs(2)
    oh = [P.alloc([128, NE], F32) for _ in range(2)]
    ohb = P.bufs(2)
    tm = [P.alloc([128, NE], F32) for _ in range(2)]
    tmb = P.bufs(2)
    dsf = [P.alloc([128, 8], F32) for _ in range(2)]
    dsb = P.bufs(2)
    psx = [P.bank(0, 2), P.bank(2, 2)]
    psxb = P.bufs(2)
    psl = [P.bank(4), P.bank(5)]
    pslb = P.bufs(2)
    psr = [P.bank(6), P.bank(7)]
    psrb = P.bufs(2)
    for t in range(16):
        i = t % 2
        P.dma("sp", xt[i], x_dram[t * 128:(t + 1) * 128, :], writes=[xb[i]])
        for k in range(8):
            tr(P, psx[i][:, k * 128:(k + 1) * 128], xt[i][:, k * 128:(k + 1) * 128], C.ident, [xb[i], C.b_ident],
               [psxb[i]])
        for k in range(8):
            act(P, hTt[i][:, k, :], psx[i][:, k * 128:(k + 1) * 128], AF.Identity, [psxb[i], M.b_mod], [hTb[i]],
                bias=M.modT[:, k:k + 1], scale=M.modT[:, 8 + k:9 + k])
        for k in range(8):
            mm(P, psl[i][:, 0:NE], hTt[i][:, k, :], rwt[:, k, :], k == 0, k == 7, [hTb[i], b_rw], [pslb[i]])
        tt(P, "dve", lg[i], psl[i][:, 0:NE], rbc, ALU.add, [pslb[i], b_rb], [lgb[i]])
        P.op("dve", lambda e, i=i: e.max(out=m8[i], in_=lg[i]), [lgb[i]], [m8b[i]])
        ts(P, "dve", ex[i], lg[i], m8[i][:, 0:1], None, ALU.subtract, None, [lgb[i], m8b[i]], [exb[i]])
        act(P, ex[i], ex[i], AF.Exp, [exb[i]], [exb[i]])
        ts(P, "dve", mk[i], lg[i], m8[i][:, 3:4], None, ALU.is_ge, None, [lgb[i], m8b[i]], [mkb[i]])
        tt(P, "dve", ex[i], ex[i], mk[i], ALU.mult, [exb[i], mkb[i]], [exb[i]])
        P.op("dve", lambda e, i=i: e.reduce_sum(out=m8[i][:, 4:5], in_=ex[i], axis=AX.X), [exb[i]], [m8b[i]])
        P.op("dve", lambda e, i=i: e.reciprocal(out=m8[i][:, 5:6], in_=m8[i][:, 4:5]), [m8b[i]], [m8b[i]])
        ts(P, "dve", G[:, t, :], ex[i], m8[i][:, 5:6], None, ALU.mult, None, [exb[i], m8b[i]], [b_G])
        mm(P, psr[i][:, 0:NE], ustr, mk[i], True, False, [b_us, mkb[i]], [psrb[i]])
        mm(P, psr[i][:, 0:NE], ones, maccb, False, True, [b_ones, b_macc], [psrb[i]])
        ts(P, "dve", ov[i], psr[i][:, 0:NE], float(CAP), None, ALU.is_ge, None, [psrb[i]], [ovb[i]])
        tt(P, "dve", Dt[i], psr[i][:, 0:NE], iotaC, ALU.add, [psrb[i], b_io], [Dtb[i]])
        stt(P, "dve", Dt[i], ov[i], 1.0e9, Dt[i], ALU.mult, ALU.add, [ovb[i], Dtb[i]], [Dtb[i]])
        tt(P, "dve", maccf, maccf, mk[i], ALU.add, [b_macc, mkb[i]], [b_macc])
        cp(P, "dve", maccb, maccf, [b_macc], [b_macc])
        for k in range(4):
            ts(P, "dve", oh[i], lg[i], m8[i][:, k:k + 1], None, ALU.is_equal, None, [lgb[i], m8b[i]], [ohb[i]])
            tt(P, "dve", tm[i], oh[i], Dt[i], ALU.mult, [ohb[i], Dtb[i]], [tmb[i]])
            P.op("dve", lambda e, i=i, k=k: e.reduce_sum(out=dsf[i][:, k:k + 1], in_=tm[i], axis=AX.X), [tmb[i]],
                 [dsb[i]])
            tt(P, "dve", tm[i], oh[i], G[:, t, :], ALU.mult, [ohb[i], b_G], [tmb[i]])
            P.op("dve", lambda e, i=i, k=k: e.reduce_sum(out=dsf[i][:, 4 + k:5 + k], in_=tm[i], axis=AX.X), [tmb[i]],
                 [dsb[i]])
        ts(P, "dve", tm[i][:, 0:4], dsf[i][:, 0:4], float(BOUND), None, ALU.is_le, None, [dsb[i]], [tmb[i]])
        tt(P, "dve", gk[:, t, :], dsf[i][:, 4:8], tm[i][:, 0:4], ALU.mult, [dsb[i], tmb[i]], [b_gk[t]])
        cp(P, "dve", destu[:, t * 4:t * 4 + 4], dsf[i][:, 0:4], [dsb[i]], [b_dest[t]])
        for k in range(4):
            P._add("pool", lambda e, t=t, k=k, src=xt[i]: e.indirect_dma_start(
                out=xd, out_offset=bass.IndirectOffsetOnAxis(ap=destu[:, t * 4 + k:t * 4 + k + 1], axis=0), in_=src,
                in_offset=None, bounds_check=P.const_reg(e, BOUND), oob_is_err=False), [xb[i], b_dest[t]], [b_xd],
                True)
        tr(P, psl[i][:32, 128:256], G[:, t, :], C.ident, [b_G, C.b_ident], [pslb[i]])
        cp(P, "act", GT[:, t * 128:(t + 1) * 128], psl[i][:32, 128:256], [pslb[i]], [b_GT])
    P.barrier()
    P.top = mark1
    wguv = wgu.rearrange("e (k p) n -> e p k n", p=128)
    wdnv = wdn.rearrange("e (k p) n -> e p k n", p=128)
    wg = P.alloc([128, 8, 2048], BF16)
    wgb = P.bufs(2)
    wd = [P.alloc([128, 8, 1024], BF16) for _ in range(2)]
    wdb = P.bufs(2)
    xe = P.alloc([128, NCH, 1024], F32)
    xeb = P.buf()
    xeT = [P.alloc([128, 8, CAP], BF16) for _ in range(2)]
    xeTb = P.bufs(2)
    aT = P.alloc([128, 8, CAP], BF16)
    aTb = P.buf()
    gt = [P.alloc([128, 512], F32) for _ in range(2)]
    gtb = P.bufs(2)
    sg = [P.alloc([128, 512], F32) for _ in range(2)]
    sgb = P.bufs(2)
    up = [P.alloc([128, 512], F32) for _ in range(2)]
    upb = P.bufs(2)
    yo = [P.alloc([128, 1024], F32) for _ in range(2)]
    yob = P.bufs(2)
    b_yd = P.buf()
    pst = [P.bank(0, 2), P.bank(2, 2)]
    pstb = P.bufs(2)
    psg = [P.bank(4), P.bank(5)]
    psgb = P.bufs(2)
    psu = [P.bank(6), P.bank(7)]
    psub = P.bufs(2)
    blks = [(0, 512), (512, CAP)]
    cnt = {"it": 0, "nt": 0}

    def load_wg(e):
        for c in range(2):
            P.dma("pool", wg[:, :, c * 512:(c + 1) * 512], wguv[e][:, :, c * 512:(c + 1) * 512], writes=[wgb[c]])
            P.dma("pool", wg[:, :, 1024 + c * 512:1024 + (c + 1) * 512],
                  wguv[e][:, :, 1024 + c * 512:1024 + (c + 1) * 512], writes=[wgb[c]])

    def load_wd(e):
        for h2 in range(2):
            P.dma("pool", wd[e % 2][:, :, h2 * 512:(h2 + 1) * 512], wdnv[e][:, :, h2 * 512:(h2 + 1) * 512],
                  writes=[wdb[e % 2]])

    def load_xe(e):
        P.dma("sp", xe, xd[e * CAP:(e + 1) * CAP, :].rearrange("(c p) f -> p c f", p=128), reads=[b_xd],
              writes=[xeb])

    def transposes(e):
        for k in range(8):
            i = cnt["nt"] % 2
            cnt["nt"] += 1
            for ch in range(NCH):
                tr(P, pst[i][:, ch * 128:(ch + 1) * 128], xe[:, ch, k * 128:(k + 1) * 128], C.ident,
                   [xeb, C.b_ident], [pstb[i]])
            act(P, xeT[e % 2][:, k, :], pst[i][:, 0:CAP], AF.Identity, [pstb[i], M.b_mod], [xeTb[e % 2]],
                bias=M.modT[:, k:k + 1], scale=M.modT[:, 8 + k:9 + k])

    def gu(e):
        xT, xTb = xeT[e % 2], xeTb[e % 2]
        for ct in range(8):
            c = ct // 4
            for (b0, b1) in blks:
                i = cnt["it"] % 2
                cnt["it"] += 1
                w_ = b1 - b0
                for k in range(8):
                    mm(P, psg[i][:, 0:w_], wg[:, k, ct * 128:(ct + 1) * 128], xT[:, k, b0:b1], k == 0, k == 7,
                       [wgb[c], xTb], [psgb[i]])
                for k in range(8):
                    mm(P, psu[i][:, 0:w_], wg[:, k, 1024 + ct * 128:1024 + (ct + 1) * 128], xT[:, k, b0:b1], k == 0,
                       k == 7, [wgb[c], xTb], [psub[i]])
                ts(P, "dve", gt[i][:, 0:w_], psg[i][:, 0:w_], bgt[:, ct, e:e + 1], 7.0, ALU.add, ALU.min,
                   [psgb[i], b_bg], [gtb[i]])
                act(P, sg[i][:, 0:w_], gt[i][:, 0:w_], AF.Sigmoid, [gtb[i]], [sgb[i]], scale=1.702)
                ts(P, "dve", up[i][:, 0:w_], psu[i][:, 0:w_], bgt[:, 8 + ct, e:e + 1], -6.0, ALU.add, ALU.max,
                   [psub[i], b_bg], [upb[i]])
                tt(P, "dve", sg[i][:, 0:w_], sg[i][:, 0:w_], gt[i][:, 0:w_], ALU.mult, [sgb[i], gtb[i]], [sgb[i]])
                stt(P, "dve", aT[:, ct, b0:b1], up[i][:, 0:w_], 8.0, sg[i][:, 0:w_], ALU.min, ALU.mult,
                    [sgb[i], upb[i]], [aTb])

    def down(e):
        for ch in range(NCH):
            i = cnt["nt"] % 2
            cnt["nt"] += 1
            for cb in range(2):
                for k in range(8):
                    mm(P, pst[i][:, cb * 512:(cb + 1) * 512], aT[:, k, ch * 128:(ch + 1) * 128],
                       wd[e % 2][:, k, cb * 512:(cb + 1) * 512], k == 0, k == 7, [aTb, wdb[e % 2]], [pstb[i]])
            cp(P, "act", yo[i], pst[i], [pstb[i]], [yob[i]])
            P.dma("sp", yd[e * CAP + ch * 128:e * CAP + (ch + 1) * 128, :], yo[i], reads=[yob[i]], writes=[b_yd])

    load_wg(0)
    load_wd(0)
    load_xe(0)
    transposes(0)
    for e in range(NE):
        if e + 1 < NE:
            load_xe(e + 1)
        gu(e)
        if e + 1 < NE:
            load_wg(e + 1)
            load_wd(e + 1)
            transposes(e + 1)
        down(e)
    P.barrier()
    P.top = mark1
    yk = [[P.alloc([128, 1024], F32) for _ in range(4)] for _ in range(2)]
    ykb = [P.bufs(4) for _ in range(2)]
    for i in range(2):
        for k in range(4):
            memset(P, "pool", yk[i][k], 0.0, [ykb[i][k]])
    xt = [P.alloc([128, 1024], F32) for _ in range(2)]
    xb = P.bufs(2)
    acc = [P.alloc([128, 1024], F32) for _ in range(2)]
    accb = P.bufs(2)
    ot = [P.alloc([128, 1024], F32) for _ in range(2)]
    ob = P.bufs(2)
    sm = [P.alloc([128, 16], F32) for _ in range(2)]
    sb = P.bufs(2)
    psd = [P.bank(4, 2), P.bank(6, 2)]
    psdb = P.bufs(2)
    for t in range(16):
        i = t % 2
        P.dma("sp", xt[i], x_dram[t * 128:(t + 1) * 128, :], writes=[xb[i]])
        for k in range(4):
            P._add("pool", lambda e, t=t, k=k, dst=yk[i][k]: e.indirect_dma_start(
                out=dst, out_offset=None, in_=yd,
                in_offset=bass.IndirectOffsetOnAxis(ap=destu[:, t * 4 + k:t * 4 + k + 1], axis=0),
                bounds_check=P.const_reg(e, BOUND), oob_is_err=False), [b_yd, b_dest[t]], [ykb[i][k]], True)
        for cb in range(2):
            mm(P, psd[i][:, cb * 512:(cb + 1) * 512], GT[:, t * 128:(t + 1) * 128], bdt[:, cb * 512:(cb + 1) * 512],
               True, True, [b_GT, b_bd], [psdb[i]])
        stt(P, "dve", acc[i], yk[i][0], gk[:, t, 0:1], psd[i], ALU.mult, ALU.add, [ykb[i][0], b_gk[t], psdb[i]],
            [accb[i]])
        for k in range(1, 4):
            stt(P, "dve", acc[i], yk[i][k], gk[:, t, k:k + 1], acc[i], ALU.mult, ALU.add,
                [ykb[i][k], b_gk[t], accb[i]], [accb[i]])
        tt(P, "dve", acc[i], acc[i], M.gate_bc, ALU.mult, [accb[i], M.b_bc_buf], [accb[i]])
        stt(P, "dve", acc[i], xt[i], ALPHA, acc[i], ALU.mult, ALU.add, [xb[i], accb[i]], [accb[i]])
        ln_tile(P, M, acc[i], accb[i], ot[i], ob[i], sm[i], sb[i])
        P.dma("sp", out_dram[t * 128:(t + 1) * 128, :], ot[i], reads=[ob[i]], writes=[b_out])
    P.barrier()
    P.top = mark0
```

```python
import contextlib
import math
import numpy as np
import ml_dtypes
import concourse.bass as bass
import concourse.mybir as mybir
from concourse.bass_utils import run_bass_kernel_spmd

F32 = mybir.dt.float32
BF16 = mybir.dt.bfloat16
AF = mybir.ActivationFunctionType
ALU = mybir.AluOpType
AX = mybir.AxisListType

SAME_ENGINE_SYNC = True
DEBUG = False
NEG = -30000.0
D = 1024
NE = 32
ALPHA = (2.0 * 2) ** 0.25
LN_EPS = 1e-5


class Buf:
    __slots__ = ("name", "w", "r")

    def __init__(self, name=""):
        self.name = name
        self.w = None
        self.r = []


class Op:
    __slots__ = ("eng", "idx", "fn", "deps", "signal", "count", "is_dma", "sem_key", "snap")


class Prog:
    ENGS = ("pe", "act", "dve", "pool", "sp")

    def __init__(self, nc, sbuf_bytes=184 * 1024):
        self.nc = nc
        self.ops = {e: [] for e in self.ENGS}
        self.known = {e: {} for e in self.ENGS}
        self.stack = contextlib.ExitStack()
        self._cnt = {}
        self.last = {}
        self.streams = {}
        self.free_slots = []
        self.nslots = 0
        self.all_ops = []
        self._regs = {}
        self.arena_words = sbuf_bytes // 4
        self.arena = self.stack.enter_context(nc.sbuf_tensor("arena", [128, self.arena_words], F32))
        self.ps = self.stack.enter_context(nc.psum_tensor("psall", [128, 4096], F32))
        self.top = 0
        self.nb = 0

    def alloc(self, shape, dtype=F32):
        free = 1
        for s in shape[1:]:
            free *= s
        esz = 4 if dtype == F32 else 2
        words = (free * esz + 3) // 4
        words = (words + 15) // 16 * 16
        assert self.top + words <= self.arena_words, f"SBUF arena overflow {self.top}+{words}"
        v = self.arena[:, self.top:self.top + words]
        self.top += words
        if dtype != F32:
            v = v.bitcast(dtype)
        v = v[:, :free]
        if len(shape) == 3:
            v = v.rearrange("p (a b) -> p a b", b=shape[2])
        elif len(shape) == 4:
            v = v.rearrange("p (a b c) -> p a b c", b=shape[2], c=shape[3])
        if shape[0] < 128:
            v = v[:shape[0]]
        return v

    def bank(self, i, n=1):
        return self.ps[:, i * 512:(i + n) * 512]

    def buf(self, name=""):
        self.nb += 1
        return Buf(name or f"b{self.nb}")

    def bufs(self, n):
        return [self.buf() for _ in range(n)]

    def _add(self, eng, fn, reads, writes, is_dma, extra=()):
        op = Op()
        op.eng = eng
        op.fn = fn
        op.is_dma = is_dma
        op.signal = bool(is_dma)
        op.count = None
        if is_dma:
            sb_ = writes[0] if writes else reads[0]
            key = self.streams.get(sb_)
            if key is None:
                if self.free_slots:
                    key = self.free_slots.pop()
                else:
                    key = "dma:%d" % self.nslots
                    self.nslots += 1
                self.streams[sb_] = key
            op.sem_key = key
        else:
            op.sem_key = eng
        op.idx = self._cnt.get(op.sem_key, 0)
        if fn is not None:
            self._cnt[op.sem_key] = op.idx + 1
        deps = list(extra)
        for b in reads:
            if b.w is not None:
                deps.append(b.w)
        for b in writes:
            if b.w is not None:
                deps.append(b.w)
            deps.extend(b.r)
        kn = self.known[eng]
        need = {}
        for d in deps:
            if d.sem_key == eng and (eng == "pe" or not SAME_ENGINE_SYNC):
                continue
            if kn.get(d.sem_key, -1) >= d.idx:
                continue
            cur = need.get(d.sem_key)
            if cur is None or cur.idx < d.idx:
                need[d.sem_key] = d
        op.deps = list(need.values())
        for d in op.deps:
            d.signal = True
            if kn.get(d.sem_key, -1) < d.idx:
                kn[d.sem_key] = d.idx
            for k, v in d.snap.items():
                if kn.get(k, -1) < v:
                    kn[k] = v
        op.snap = dict(kn)
        if fn is not None:
            for b in reads:
                b.r.append(op)
            for b in writes:
                b.w = op
                b.r = []
            self.last[op.sem_key] = op
        self.ops[eng].append(op)
        self.all_ops.append(op)
        return op

    def op(self, eng, fn, reads=(), writes=()):
        return self._add(eng, fn, reads, writes, False)

    def dma(self, eng, out, in_, reads=(), writes=(), **kw):
        return self._add(eng, lambda e: e.dma_start(out=out, in_=in_, **kw), reads, writes, True)

    def dump(self, name, ap, b, shape, dtype=F32):
        if not DEBUG:
            return
        d = self.nc.dram_tensor("dbg_" + name, list(shape), dtype, kind="ExternalOutput").ap()
        self.dma("sp", d, ap, reads=[b])

    def const_reg(self, e, value):
        if value not in self._regs:
            r = e.alloc_register("c%d" % value)
            e.reg_mov(r, value)
            self._regs[value] = r
        return self._regs[value]

    def barrier(self):
        lasts = list(self.last.values())
        for e in self.ENGS:
            self._add(e, None, (), (), False, extra=lasts)
        self.streams = {}
        self.free_slots = ["dma:%d" % i for i in range(self.nslots)]

    def emit(self, final_waits=()):
        nc = self.nc
        for d in final_waits:
            d.signal = True
        keys = set()
        for e in self.ENGS:
            for o in self.ops[e]:
                if o.fn is not None:
                    keys.add(o.sem_key)
        counts = {k: 0 for k in keys}
        for o in self.all_ops:
            if o.fn is not None and o.signal:
                counts[o.sem_key] += 16 if o.is_dma else 1
                o.count = counts[o.sem_key]
        self.maxcounts = counts
        sems = {}
        for k in sorted(keys):
            sems[k] = self.stack.enter_context(nc.semaphore(k.replace(":", "_")))
        block = self.stack.enter_context(nc.Block())

        def run(eng_name, e):
            for o in self.ops[eng_name]:
                for d in o.deps:
                    e.wait_ge(sems[d.sem_key], d.count)
                if o.fn is None:
                    continue
                ins = o.fn(e)
                if o.signal:
                    ins.then_inc(sems[o.sem_key], 16 if o.is_dma else 1)
            if eng_name == "sp":
                for d in final_waits:
                    e.wait_ge(sems[d.sem_key], d.count)

        @block.sync
        def _(e):
            run("sp", e)

        @block.scalar
        def _(e):
            run("act", e)

        @block.vector
        def _(e):
            run("dve", e)

        @block.gpsimd
        def _(e):
            run("pool", e)

        @block.tensor
        def _(e):
            run("pe", e)

    def close(self):
        self.stack.close()


def mm(P, out, lhsT, rhs, start, stop, reads, writes):
    return P.op("pe", lambda e: e.matmul(out, lhsT=lhsT, rhs=rhs, start=start, stop=stop), reads, writes)


def tr(P, out, in_, ident, reads, writes):
    return P.op("pe", lambda e: e.transpose(out, in_, ident), reads, writes)


def act(P, out, in_, func, reads, writes, bias=0.0, scale=1.0, eng="act"):
    return P.op(eng, lambda e: e.activation(out=out, in_=in_, func=func, bias=bias, scale=scale), reads, writes)


def tt(P, eng, out, in0, in1, op, reads, writes):
    return P.op(eng, lambda e: e.tensor_tensor(out=out, in0=in0, in1=in1, op=op), reads, writes)


def ts(P, eng, out, in0, s1, s2, op0, op1, reads, writes):
    if s2 is None:
        return P.op(eng, lambda e: e.tensor_scalar(out=out, in0=in0, scalar1=s1, scalar2=None, op0=op0), reads, writes)
    return P.op(eng, lambda e: e.tensor_scalar(out=out, in0=in0, scalar1=s1, scalar2=s2, op0=op0, op1=op1), reads, writes)


def stt(P, eng, out, in0, scalar, in1, op0, op1, reads, writes):
    return P.op(eng, lambda e: e.scalar_tensor_tensor(out=out, in0=in0, scalar=scalar, in1=in1, op0=op0, op1=op1),
                reads, writes)


def cp(P, eng, out, in_, reads, writes):
    if eng == "act":
        return P.op("act", lambda e: e.copy(out=out, in_=in_), reads, writes)
    return P.op(eng, lambda e: e.tensor_copy(out=out, in_=in_), reads, writes)


def memset(P, eng, ap, val, writes):
    return P.op(eng, lambda e: e.memset(ap, val), (), writes)


class Ctx:
    pass


def phase_consts(P, io):
    C = Ctx()
    C.ident = P.alloc([128, 128], F32)
    C.identb = P.alloc([128, 128], BF16)
    C.b_ident = P.buf()
    P.dma("sp", C.ident, io["ident"], writes=[C.b_ident])
    C.b_identb = P.buf()
    cp(P, "dve", C.identb, C.ident, [C.b_ident], [C.b_identb])
    C.ccol = P.alloc([128, 8], F32)
    C.b_ccol = P.buf()
    P.dma("sp", C.ccol, io["ccol"], writes=[C.b_ccol])
    act(P, C.ccol, C.ccol, AF.Silu, [C.b_ccol], [C.b_ccol])
    return C


def phase_mod(P, C, mod_w, mod_b, scr_gate, lng, lnb):
    M = Ctx()
    M.modT = P.alloc([128, 24], F32)
    M.gate_bc = P.alloc([128, 1024], F32)
    M.g_bc = P.alloc([128, 1024], F32)
    M.b_bc = P.alloc([128, 1024], F32)
    M.b_mod = P.buf()
    M.b_bc_buf = P.buf()
    key = int(mod_w.offset)
    cache = getattr(P, "mod_cache", None)
    if cache is not None and key in cache:
        P.dma("sp", M.modT, cache[key][0], writes=[M.b_mod])
        P.dma("sp", M.gate_bc, cache[key][1].partition_broadcast(128), writes=[M.b_bc_buf])
        P.dma("sp", M.g_bc, lng.partition_broadcast(128), writes=[M.b_bc_buf])
        P.dma("sp", M.b_bc, lnb.partition_broadcast(128), writes=[M.b_bc_buf])
        P.barrier()
        return M
    mark = P.top
    wt = [P.alloc([128, 8, 512], F32) for _ in range(2)]
    wb = P.bufs(2)
    mb = P.alloc([128, 24], F32)
    b_mb = P.buf()
    P.dma("sp", mb, mod_b, writes=[b_mb])
    ps = P.bank(0)
    b_ps = P.buf()
    wv = mod_w.rearrange("(k p) n -> p k n", p=128)
    for cb in range(6):
        w = wt[cb % 2]
        P.dma("sp", w, wv[:, :, cb * 512:(cb + 1) * 512], writes=[wb[cb % 2]])
        for c4 in range(4):
            ct = cb * 4 + c4
            for k in range(8):
                mm(P, ps[:, ct:ct + 1], w[:, k, c4 * 128:(c4 + 1) * 128], C.ccol[:, k:k + 1], k == 0, k == 7,
                   [wb[cb % 2], C.b_ccol], [b_ps])
    tt(P, "dve", M.modT, ps[:, 0:24], mb, ALU.add, [b_ps, b_mb], [M.b_mod])
    ts(P, "dve", M.modT[:, 8:24], M.modT[:, 8:24], 1.0, None, ALU.add, None, [M.b_mod], [M.b_mod])
    b_scr = P.buf()
    gsb = P.alloc([8, 128], F32)
    b_gsb = P.buf()
    ps2 = P.bank(1)
    b_ps2 = P.buf()
    tr(P, ps2[:8, 0:128], M.modT[:, 16:24], C.ident, [M.b_mod, C.b_ident], [b_ps2])
    cp(P, "act", gsb, ps2[:8, 0:128], [b_ps2], [b_gsb])
    P.dma("sp", scr_gate.rearrange("(c p) -> c p", p=128), gsb, reads=[b_gsb], writes=[b_scr])
    P.dma("sp", M.gate_bc, scr_gate.partition_broadcast(128), reads=[b_scr], writes=[M.b_bc_buf])
    P.dma("sp", M.g_bc, lng.partition_broadcast(128), writes=[M.b_bc_buf])
    P.dma("sp", M.b_bc, lnb.partition_broadcast(128), writes=[M.b_bc_buf])
    if cache is not None and key is not None:
        cache[key] = (P.nc.dram_tensor("modT_cache%d" % len(cache), [128, 24], F32, kind="Internal").ap(), scr_gate)
        P.dma("sp", cache[key][0], M.modT, reads=[M.b_mod], writes=[P.buf()])
    P.dump("modT%d" % P.nb, M.modT, M.b_mod, [128, 24])
    P.dump("gbc%d" % P.nb, M.gate_bc, M.b_bc_buf, [128, 1024])
    P.barrier()
    P.top = mark
    return M


def build_hT(P, C, M, x_dram, ntiles, hT, hT_bufs, ps_banks=(0, 4)):
    mark = P.top
    xt = [P.alloc([128, 1024], F32) for _ in range(2)]
    xb = P.bufs(2)
    pss = [P.bank(ps_banks[0], 2), P.bank(ps_banks[1], 2)]
    psb = P.bufs(2)
    for t in range(ntiles):
        x_ = xt[t % 2]
        P.dma("sp", x_, x_dram[t * 128:(t + 1) * 128, :], writes=[xb[t % 2]])
        ps = pss[t % 2]
        for k in range(8):
            tr(P, ps[:, k * 128:(k + 1) * 128], x_[:, k * 128:(k + 1) * 128], C.ident, [xb[t % 2], C.b_ident],
               [psb[t % 2]])
        for k in range(8):
            eng = "act"
            act(P, hT[:, k, t * 128:(t + 1) * 128], ps[:, k * 128:(k + 1) * 128], AF.Identity,
                [psb[t % 2], M.b_mod], [hT_bufs[t]], bias=M.modT[:, k:k + 1], scale=M.modT[:, 8 + k:9 + k])
    P.barrier()
    P.top = mark


def ln_tile(P, M, tmp, b_tmp, out, b_out, sm, b_sm):
    P.op("dve", lambda e: e.bn_stats(out=sm[:, 0:6], in_=tmp[:, 0:512]), [b_tmp], [b_sm])
    P.op("dve", lambda e: e.bn_stats(out=sm[:, 6:12], in_=tmp[:, 512:1024]), [b_tmp], [b_sm])
    P.op("dve", lambda e: e.bn_aggr(out=sm[:, 12:14], in_=sm[:, 0:12].rearrange("p (a b) -> p a b", b=6)),
         [b_sm], [b_sm])
    ts(P, "dve", sm[:, 15:16], sm[:, 13:14], LN_EPS, None, ALU.add, None, [b_sm], [b_sm])
    act(P, sm[:, 15:16], sm[:, 15:16], AF.Sqrt, [b_sm], [b_sm])
    P.op("dve", lambda e: e.reciprocal(out=sm[:, 14:15], in_=sm[:, 15:16]), [b_sm], [b_sm])
    ts(P, "dve", tmp, tmp, sm[:, 12:13], sm[:, 14:15], ALU.subtract, ALU.mult, [b_tmp, b_sm], [b_tmp])
    tt(P, "pool", tmp, tmp, M.g_bc, ALU.mult, [b_tmp, M.b_bc_buf], [b_tmp])
    tt(P, "pool", out, tmp, M.b_bc, ALU.add, [b_tmp, M.b_bc_buf], [b_out])


def phase_outproj_ln(P, C, M, yT, b_yT, w_out, x_dram, xo_dram, b_xo, ntiles=16):
    mark = P.top
    wo = P.alloc([128, 8, 1024], BF16)
    b_wo = P.buf()
    wv = w_out.rearrange("(k p) n -> p k n", p=128)
    for h in range(2):
        P.dma("pool", wo[:, :, h * 512:(h + 1) * 512], wv[:, :, h * 512:(h + 1) * 512], writes=[b_wo])
    xt = [P.alloc([128, 1024], F32) for _ in range(2)]
    xb = P.bufs(2)
    tm = [P.alloc([128, 1024], F32) for _ in range(2)]
    tb = P.bufs(2)
    ot = [P.alloc([128, 1024], F32) for _ in range(2)]
    ob = P.bufs(2)
    sm = [P.alloc([128, 16], F32) for _ in range(2)]
    sb = P.bufs(2)
    pss = [P.bank(0, 2), P.bank(2, 2)]
    psb = P.bufs(2)
    for t in range(ntiles):
        i = t % 2
        P.dma("sp", xt[i], x_dram[t * 128:(t + 1) * 128, :], writes=[xb[i]])
        for cb in range(2):
            for k in range(8):
                mm(P, pss[i][:, cb * 512:(cb + 1) * 512], yT[:, k, t * 128:(t + 1) * 128],
                   wo[:, k, cb * 512:(cb + 1) * 512], k == 0, k == 7, [b_yT, b_wo], [psb[i]])
        tt(P, "dve", tm[i], pss[i], M.gate_bc, ALU.mult, [psb[i], M.b_bc_buf], [tb[i]])
        stt(P, "dve", tm[i], xt[i], ALPHA, tm[i], ALU.mult, ALU.add, [xb[i], tb[i]], [tb[i]])
        ln_tile(P, M, tm[i], tb[i], ot[i], ob[i], sm[i], sb[i])
        P.dma("sp", xo_dram[t * 128:(t + 1) * 128, :], ot[i], reads=[ob[i]], writes=[b_xo])
    P.barrier()
    P.top = mark


def phase_moe(P, C, M, x_dram, out_dram, b_out, io, layer):
    if SPARSE_MOE:
        if not hasattr(P, "moe_scr"):
            P.moe_scr = (P.nc.dram_tensor("moe_xd", [NE * CAP, 1024], F32, kind="Internal").ap(),
                         P.nc.dram_tensor("moe_yd", [NE * CAP, 1024], F32, kind="Internal").ap())
        return phase_moe_sparse(P, C, M, x_dram, out_dram, b_out, io, layer, P.moe_scr[0], P.moe_scr[1])
    rw, rb = io["router_w"][layer], io["router_b"][layer]
    wgu, bguT = io["exp_w_gu"][layer], io["b_guT"][layer]
    wdn, bdn = io["exp_w_down"][layer], io["exp_b_down"][layer]
    mark0 = P.top
    rwt = P.alloc([128, 8, NE], BF16)
    b_rw = P.buf()
    P.dma("pool", rwt, rw.rearrange("(k p) n -> p k n", p=128), writes=[b_rw])
    rbc = P.alloc([128, NE], F32)
    b_rb = P.buf()
    P.dma("sp", rbc, rb.partition_broadcast(128), writes=[b_rb])
    bdt = P.alloc([32, 1024], F32)
    b_bd = P.buf()
    P.dma("sp", bdt, bdn, writes=[b_bd])
    bgt = P.alloc([128, 16, NE], F32)
    b_bg = P.buf()
    P.dma("sp", bgt, bguT, writes=[b_bg])
    ts(P, "dve", bgt[:, 8:16, :], bgt[:, 8:16, :], 1.0, None, ALU.add, None, [b_bg], [b_bg])
    wguv = wgu.rearrange("e (k p) n -> e p k n", p=128)
    wdnv = wdn.rearrange("e (k p) n -> e p k n", p=128)
    markh = P.top
    for half in range(2):
        P.top = markh
        t0 = half * 8
        hT = P.alloc([128, 8, 1024], BF16)
        hTb = P.bufs(8)
        build_hT(P, C, M, x_dram[half * 1024:(half + 1) * 1024, :], 8, hT, hTb)
        G = P.alloc([128, 8, NE], F32)
        b_G = P.buf()
        GT = P.alloc([32, 1024], F32)
        b_GT = P.buf()
        yacc = P.alloc([128, 8, 1024], F32)
        yb = P.bufs(8)
        mark1 = P.top
        lg = [P.alloc([128, NE], F32) for _ in range(2)]
        lgb = P.bufs(2)
        m8 = [P.alloc([128, 8], F32) for _ in range(2)]
        m8b = P.bufs(2)
        ex = [P.alloc([128, NE], F32) for _ in range(2)]
        exb = P.bufs(2)
        pss = [P.bank(0), P.bank(1)]
        psb = P.bufs(2)
        pst = [P.bank(2), P.bank(3)]
        pstb = P.bufs(2)
        for t in range(8):
            i = t % 2
            for k in range(8):
                mm(P, pss[i][:, 0:NE], hT[:, k, t * 128:(t + 1) * 128], rwt[:, k, :], k == 0, k == 7,
                   [hTb[t], b_rw], [psb[i]])
            tt(P, "dve", lg[i], pss[i][:, 0:NE], rbc, ALU.add, [psb[i], b_rb], [lgb[i]])
            P.op("dve", lambda e, i=i: e.max(out=m8[i], in_=lg[i]), [lgb[i]], [m8b[i]])
            ts(P, "dve", ex[i], lg[i], m8[i][:, 0:1], None, ALU.subtract, None, [lgb[i], m8b[i]], [exb[i]])
            act(P, ex[i], ex[i], AF.Exp, [exb[i]], [exb[i]])
            stt(P, "dve", ex[i], lg[i], m8[i][:, 3:4], ex[i], ALU.is_ge, ALU.mult, [lgb[i], m8b[i], exb[i]],
                [exb[i]])
            P.op("dve", lambda e, i=i: e.reduce_sum(out=m8[i][:, 4:5], in_=ex[i], axis=AX.X), [exb[i]], [m8b[i]])
            P.op("dve", lambda e, i=i: e.reciprocal(out=m8[i][:, 5:6], in_=m8[i][:, 4:5]), [m8b[i]], [m8b[i]])
            ts(P, "dve", G[:, t, :], ex[i], m8[i][:, 5:6], None, ALU.mult, None, [exb[i], m8b[i]], [b_G])
            tr(P, pst[i][:32, 0:128], G[:, t, :], C.ident, [b_G, C.b_ident], [pstb[i]])
            cp(P, "act", GT[:, t * 128:(t + 1) * 128], pst[i][:32, 0:128], [pstb[i]], [b_GT])
        P.barrier()
        P.top = mark1
        wg = P.alloc([128, 8, 2048], BF16)
        wgb = P.bufs(2)
        wd = P.alloc([128, 8, 1024], BF16)
        wdb = P.buf()
        aT = P.alloc([128, 8, 1024], BF16)
        aTb = P.bufs(2)
        gt = [P.alloc([128, 512], F32) for _ in range(2)]
        gtb = P.bufs(2)
        sg = [P.alloc([128, 512], F32) for _ in range(2)]
        sgb = P.bufs(2)
        up = [P.alloc([128, 512], F32) for _ in range(2)]
        upb = P.bufs(2)
        psg = [P.bank(0), P.bank(1)]
        psgb = P.bufs(2)
        psu = [P.bank(2), P.bank(3)]
        psub = P.bufs(2)
        psd = [P.bank(4, 2), P.bank(6, 2)]
        psdb = P.bufs(2)
        it = 0
        for e in range(NE):
            for c in range(2):
                P.dma("pool", wg[:, :, c * 512:(c + 1) * 512], wguv[e][:, :, c * 512:(c + 1) * 512], writes=[wgb[c]])
                P.dma("pool", wg[:, :, 1024 + c * 512:1024 + (c + 1) * 512],
                      wguv[e][:, :, 1024 + c * 512:1024 + (c + 1) * 512], writes=[wgb[c]])
            for ct in range(8):
                c = ct // 4
                for tb_ in range(2):
                    i = it % 2
                    it += 1
                    tok = slice(tb_ * 512, (tb_ + 1) * 512)
                    hb = hTb[tb_ * 4: tb_ * 4 + 4]
                    for k in range(8):
                        mm(P, psg[i], wg[:, k, ct * 128:(ct + 1) * 128], hT[:, k, tok], k == 0, k == 7,
                           [wgb[c]] + hb, [psgb[i]])
                    for k in range(8):
                        mm(P, psu[i], wg[:, k, 1024 + ct * 128:1024 + (ct + 1) * 128], hT[:, k, tok], k == 0, k == 7,
                           [wgb[c]] + hb, [psub[i]])
                    ts(P, "dve", gt[i], psg[i], bgt[:, ct, e:e + 1], 7.0, ALU.add, ALU.min, [psgb[i], b_bg], [gtb[i]])
                    act(P, sg[i], gt[i], AF.Sigmoid, [gtb[i]], [sgb[i]], scale=1.702)
                    ts(P, "dve", up[i], psu[i], bgt[:, 8 + ct, e:e + 1], -6.0, ALU.add, ALU.max, [psub[i], b_bg],
                       [upb[i]])
                    tt(P, "pool", sg[i], sg[i], gt[i], ALU.mult, [sgb[i], gtb[i]], [sgb[i]])
                    stt(P, "dve", aT[:, ct, tok], up[i], 8.0, sg[i], ALU.min, ALU.mult, [sgb[i], upb[i]], [aTb[tb_]])
            for h2 in range(2):
                P.dma("pool", wd[:, :, h2 * 512:(h2 + 1) * 512], wdnv[e][:, :, h2 * 512:(h2 + 1) * 512],
                      writes=[wdb])
            for t in range(8):
                i = t % 2
                for cb in range(2):
                    for k in range(8):
                        mm(P, psd[i][:, cb * 512:(cb + 1) * 512], aT[:, k, t * 128:(t + 1) * 128],
                           wd[:, k, cb * 512:(cb + 1) * 512], k == 0, k == 7, [aTb[t // 4], wdb], [psdb[i]])
                if e == 0:
                    ts(P, "dve", yacc[:, t, :], psd[i], G[:, t, e:e + 1], None, ALU.mult, None,
                       [psdb[i], b_G], [yb[t]])
                else:
                    stt(P, "dve", yacc[:, t, :], psd[i], G[:, t, e:e + 1], yacc[:, t, :], ALU.mult, ALU.add,
                        [psdb[i], b_G, yb[t]], [yb[t]])
        P.barrier()
        P.top = mark1
        xt = [P.alloc([128, 1024], F32) for _ in range(2)]
        xb = P.bufs(2)
        ot = [P.alloc([128, 1024], F32) for _ in range(2)]
        ob = P.bufs(2)
        sm = [P.alloc([128, 16], F32) for _ in range(2)]
        sb = P.bufs(2)
        for t in range(8):
            i = t % 2
            tg = t0 + t
            P.dma("sp", xt[i], x_dram[tg * 128:(tg + 1) * 128, :], writes=[xb[i]])
            for cb in range(2):
                mm(P, psd[i][:, cb * 512:(cb + 1) * 512], GT[:, t * 128:(t + 1) * 128],
                   bdt[:, cb * 512:(cb + 1) * 512], True, True, [b_GT, b_bd], [psdb[i]])
            tt(P, "dve", yacc[:, t, :], yacc[:, t, :], psd[i], ALU.add, [psdb[i], yb[t]], [yb[t]])
            tt(P, "dve", yacc[:, t, :], yacc[:, t, :], M.gate_bc, ALU.mult, [yb[t], M.b_bc_buf], [yb[t]])
            stt(P, "dve", yacc[:, t, :], xt[i], ALPHA, yacc[:, t, :], ALU.mult, ALU.add, [xb[i], yb[t]], [yb[t]])
            ln_tile(P, M, yacc[:, t, :], yb[t], ot[i], ob[i], sm[i], sb[i])
            P.dma("sp", out_dram[tg * 128:(tg + 1) * 128, :], ot[i], reads=[ob[i]], writes=[b_out])
        P.barrier()
    P.top = mark0


def phase_even_mixer(P, C, M, io, x_dram_halo, yT, b_yT):
    w_in = io["even_w_in"]
    mark0 = P.top
    NU = 2176
    uT = P.alloc([128, 4, NU], BF16)
    ub = P.bufs(4)
    qT = P.alloc([128, 4, 2048], BF16)
    qb = P.bufs(4)
    kT = P.alloc([128, 4, 2560], BF16)
    kb = P.bufs(4)
    V = P.alloc([128, 20, 8 * 65], BF16)
    vb = P.bufs(20)
    b_vones = P.buf()
    memset(P, "pool", V, 1.0, [b_vones])
    flag = P.alloc([128, 2], F32)
    b_flag = P.buf()
    P.dma("sp", flag, io["flag"], writes=[b_flag])
    icnt = P.alloc([128, 4, 16], F32)
    b_icnt = P.buf()
    P.dma("sp", icnt, io["icnt"], writes=[b_icnt])
    psc = P.alloc([128, 4], F32)
    b_psc = P.buf()
    P.dma("sp", psc, io["pool_scaleT"], writes=[b_psc])
    wp = P.alloc([128, 4, 128], BF16)
    b_wp = P.buf()
    P.dma("pool", wp, io["pool_w"].rearrange("g c d -> c g d"), writes=[b_wp])
    mark_tmp = P.top
    hT = P.alloc([128, 8, 2560], BF16)
    hTb = P.bufs(20)
    build_hT(P, C, M, x_dram_halo, 20, hT, hTb)
    wi2 = [P.alloc([128, 8, 512], BF16) for _ in range(2)]
    wi2b = P.bufs(2)
    wv = w_in.rearrange("(k p) n -> p k n", p=128)

    class WI:
        loaded = -1

        def __getitem__(self, key):
            _, k, cols = key
            c = cols.start // 512
            if c > WI.loaded:
                assert c == WI.loaded + 1
                P.dma("pool", wi2[c % 2], wv[:, :, c * 512:(c + 1) * 512], writes=[wi2b[c % 2]])
                WI.loaded = c
            return wi2[c % 2][:, k, cols.start - c * 512:cols.stop - c * 512]
    wi = WI()
    wib = [wi2b[0], wi2b[1], wi2b[0], wi2b[1]]
    pss = [P.bank(i) for i in range(4)]
    psb = P.bufs(4)
    it = 0
    allh = hTb

    def proj(cols, tok0, ntok, evac):
        nonlocal it
        i = it % 4
        it += 1
        t_lo, t_hi = tok0 // 128, (tok0 + ntok + 127) // 128
        for k in range(8):
            mm(P, pss[i][:, 0:ntok], wi[:, k, cols], hT[:, k, tok0:tok0 + ntok], k == 0, k == 7,
               [wib[cols.start // 512]] + allh[t_lo:t_hi], [psb[i]])
        evac(pss[i][:, 0:ntok], psb[i])

    for g in range(4):
        for blk in range(5):
            tok0 = 384 + blk * 512
            ntok = min(512, 2560 - tok0)
            proj(slice(g * 128, (g + 1) * 128), tok0, ntok,
                 lambda ps, b, g=g, blk=blk, ntok=ntok: cp(P, "act", uT[:, g, blk * 512:blk * 512 + ntok], ps, [b],
                                                          [ub[g]]))
    for c in range(4):
        for blk in range(4):
            proj(slice(512 + c * 128, 512 + (c + 1) * 128), 512 + blk * 512, 512,
                 lambda ps, b, c=c, blk=blk: act(P, qT[:, c, blk * 512:(blk + 1) * 512], ps, AF.Copy, [b], [qb[c]],
                                                 scale=0.125))
        for blk in range(5):
            proj(slice(1024 + c * 128, 1024 + (c + 1) * 128), blk * 512, 512,
                 lambda ps, b, c=c, blk=blk: cp(P, "dve", kT[:, c, blk * 512:(blk + 1) * 512], ps, [b], [kb[c]]))
    for t in range(20):
        i = it % 4
        it += 1
        for k in range(8):
            mm(P, pss[i], hT[:, k, t * 128:(t + 1) * 128], wi[:, k, 1536:2048], k == 0, k == 7, [wib[3], hTb[t]],
               [psb[i]])
        cp(P, "dve" if t % 2 else "act", V[:, t, :].rearrange("p (h d) -> p h d", d=65)[:, :, 0:64],
           pss[i].rearrange("p (h d) -> p h d", d=64), [psb[i], b_vones], [vb[t]])
    P.dump("hT", hT[:, :, 512:640], hTb[4], [128, 8, 128], BF16)
    P.dump("ident", C.ident, C.b_ident, [128, 128])
    P.dump("uT", uT, ub[0], [128, 4, NU], BF16)
    P.dump("qT", qT, qb[0], [128, 4, 2048], BF16)
    P.dump("kT", kT, kb[0], [128, 4, 2560], BF16)
    P.dump("V", V, vb[0], [128, 20, 520], BF16)
    P.barrier()
    P.top = mark_tmp
    for g in range(4):
        w = 2 ** (g + 1)
        ts(P, "dve", uT[:, g, 0:128], uT[:, g, 0:128], flag[:, 0:1], None, ALU.mult, None, [ub[g], b_flag], [ub[g]])
    sA = P.alloc([128, NU], F32)
    sB = P.alloc([128, NU], F32)
    b_sA, b_sB = P.buf(), P.buf()
    dT = P.alloc([128, 4, 2048], BF16)
    db = P.bufs(4)
    for g in range(4):
        w = 2 ** (g + 1)
        eng = "dve" if g % 2 == 0 else "pool"
        src, b_src = uT[:, g, :], ub[g]
        m = 1
        cur, b_cur = sA, b_sA
        lo = 0
        while m < w:
            lo_new = lo + m
            tt(P, eng, cur[:, lo_new:NU], src[:, lo_new:NU], src[:, lo_new - m:NU - m], ALU.add, [b_src], [b_cur])
            src, b_src = cur, b_cur
            cur, b_cur = (sB, b_sB) if cur is sA else (sA, b_sA)
            lo = lo_new
            m *= 2
        stt(P, "dve", dT[:, g, 16:2048], src[:, 144:NU], 1.0 / w, uT[:, g, 144:NU], ALU.mult, ALU.subtract,
            [b_src, ub[g]], [db[g]])
        tt(P, eng, cur[:, 0:16], src[:, 128:144], icnt[:, g, :], ALU.mult, [b_src, b_icnt], [b_cur])
        tt(P, eng, dT[:, g, 0:16], cur[:, 0:16], uT[:, g, 128:144], ALU.subtract, [b_cur, ub[g]], [db[g]])
    it = 0
    for g in range(4):
        for blk in range(4):
            i = it % 4
            it += 1
            mm(P, pss[i], wp[:, g, :], dT[:, g, blk * 512:(blk + 1) * 512], True, True, [b_wp, db[g]], [psb[i]])
            act(P, yT[:, g, blk * 512:(blk + 1) * 512], pss[i], AF.Copy, [psb[i], b_psc], [b_yT],
                scale=psc[:, g:g + 1])
    P.barrier()
    P.top = P.top
    BT = P.alloc([128, 8, 5, 256], BF16)
    b_BT = P.buf()
    P.dma("sp", BT, io["biasT"], writes=[b_BT])
    zero = P.alloc([128, 1], F32)
    b_zero = P.buf()
    memset(P, "dve", zero, 0.0, [b_zero])
    pT = [P.alloc([128, 5, 128], BF16) for _ in range(2)]
    pTb = P.bufs(2)
    rs = [P.alloc([128, 8], F32) for _ in range(2)]
    rsb = P.bufs(2)
    ya = [P.alloc([128, 512], BF16) for _ in range(2)]
    yab = P.bufs(2)
    psS = [P.bank(0, 2), P.bank(2, 2)]
    psSb = P.bufs(2)
    psO = [P.bank(4, 2), P.bank(6, 2)]
    psOb = P.bufs(2)
    psT = P.bank(4, 2)
    def emit_S(qt, h, si):
        c, r0 = h // 2, (h % 2) * 64
        for j in range(5):
            kt = qt + j
            mm(P, psS[si][:, j * 128:(j + 1) * 128], kT[r0:r0 + 64, c, kt * 128:(kt + 1) * 128],
               qT[r0:r0 + 64, c, qt * 128:(qt + 1) * 128], True, False, [kb[c], qb[c]], [psSb[si]])
            mm(P, psS[si][:, j * 128:(j + 1) * 128], C.identb, BT[:, h, j, 0:128], False, False,
               [C.b_identb, b_BT], [psSb[si]])
            mm(P, psS[si][:, j * 128:(j + 1) * 128], C.identb, BT[:, h, j, 128:256], False, True,
               [C.b_identb, b_BT], [psSb[si]])
        for j in range(5):
            kt = qt + j
            bias = flag[:, 1:2] if kt < 4 else zero[:, 0:1]
            act(P, pT[si][:, j, :], psS[si][:, j * 128:(j + 1) * 128], AF.Exp, [psSb[si], b_flag, b_zero],
                [pTb[si]], bias=bias)

    def emit_PV(qt, h, si):
        oi = qt % 2
        ob = psO[oi][:, (h // 4) * 512 + (h % 4) * 65:(h // 4) * 512 + (h % 4) * 65 + 65]
        for j in range(5):
            kt = qt + j
            mm(P, ob, pT[si][:, j, :], V[:, kt, h * 65:(h + 1) * 65], j == 0, j == 4, [pTb[si], vb[kt]],
               [psOb[oi]])
        if h != 7:
            return
        for hb_ in range(2):
            ov = psO[oi][:, hb_ * 512:hb_ * 512 + 260].rearrange("p (h d) -> p h d", d=65)
            P.op("dve", lambda e, ov=ov, oi=oi, hb_=hb_: e.reciprocal(out=rs[oi][:, hb_ * 4:(hb_ + 1) * 4],
                                                                        in_=ov[:, :, 64]), [psOb[oi]], [rsb[oi]])
            for hh in range(4):
                h2 = hb_ * 4 + hh
                ts(P, "dve", ya[oi][:, h2 * 64:(h2 + 1) * 64], ov[:, hh, 0:64], rs[oi][:, h2:h2 + 1], None, ALU.mult,
                   None, [psOb[oi], rsb[oi]], [yab[oi]])
        sT = (si + 1) % 2
        psTv = psS[si].bitcast(BF16)
        for c in range(4):
            tr(P, psTv[:, c * 128:(c + 1) * 128], ya[oi][:, c * 128:(c + 1) * 128], C.identb,
               [yab[oi], C.b_identb], [psSb[si]])
        cp(P, "act", yT[:, 4:8, qt * 128:(qt + 1) * 128],
           psTv[:, 0:512].rearrange("p (c q) -> p c q", q=128), [psSb[si]], [b_yT])

    items = [(qt, h) for qt in range(16) for h in range(8)]
    emit_S(items[0][0], items[0][1], 0)
    for n_ in range(len(items)):
        if n_ + 1 < len(items):
            emit_S(items[n_ + 1][0], items[n_ + 1][1], (n_ + 1) % 2)
        emit_PV(items[n_][0], items[n_][1], n_ % 2)
    P.dump("yT", yT, b_yT, [128, 8, 2048], BF16)
    P.barrier()
    P.top = mark0


def dram_in(nc, name, shape, dtype=F32):
    return nc.dram_tensor(name, list(shape), dtype, kind="ExternalInput").ap()


def build_stage_a():
    nc = bass.Bass("TRN2", target_bir_lowering=False)
    io = {}
    io["x"] = dram_in(nc, "x", [2560, 1024])
    io["ustrict"] = dram_in(nc, "ustrict", [128, 128], BF16)
    io["iotaC"] = dram_in(nc, "iotaC", [128, NE])
    io["ident"] = dram_in(nc, "ident", [128, 128])
    io["ccol"] = dram_in(nc, "ccol", [128, 8])
    io["mod_w"] = dram_in(nc, "mod_w", [3, 1024, 3072])
    io["mod_bT"] = dram_in(nc, "mod_bT", [3, 128, 24])
    io["ln_g"] = dram_in(nc, "ln_g", [2, 1024])
    io["ln_b"] = dram_in(nc, "ln_b", [2, 1024])
    io["even_w_in"] = dram_in(nc, "even_w_in", [1024, 2048])
    io["flag"] = dram_in(nc, "flag", [128, 2])
    io["icnt"] = dram_in(nc, "icnt", [128, 4, 16])
    io["pool_scaleT"] = dram_in(nc, "pool_scaleT", [128, 4])
    io["pool_w"] = dram_in(nc, "pool_w", [4, 128, 128])
    io["biasT"] = dram_in(nc, "biasT", [128, 8, 5, 256], BF16)
    io["even_w_out"] = dram_in(nc, "even_w_out", [1024, 1024])
    io["router_w"] = dram_in(nc, "router_w", [1, 1024, 32])
    io["router_b"] = dram_in(nc, "router_b", [1, 32])
    if not STAGE_A_MOE:
        NEX = 1
    else:
        NEX = 32
    io["exp_w_gu"] = dram_in(nc, "exp_w_gu", [1, NEX, 1024, 2048])
    io["b_guT"] = dram_in(nc, "b_guT", [1, 128, 16, 32])
    io["exp_w_down"] = dram_in(nc, "exp_w_down", [1, NEX, 1024, 1024])
    io["exp_b_down"] = dram_in(nc, "exp_b_down", [1, 32, 1024])
    xmid = nc.dram_tensor("xmid", [2048, 1024], F32, kind="ExternalOutput").ap()
    xout = nc.dram_tensor("xout", [2048, 1024], F32, kind="ExternalOutput").ap()
    scr = nc.dram_tensor("scr_gate", [3, 1024], F32, kind="ExternalOutput").ap()
    h1T_out = nc.dram_tensor("h1T", [128, 8, 2048], BF16, kind="ExternalOutput").ap()
    P = Prog(nc)
    C = phase_consts(P, io)
    P.barrier()
    stage_a_body(P, C, io, scr, xmid, xout, h1T_out)
    P.emit(final_waits=[P.last[k] for k in P.last if k.startswith("dma:")])
    P.close()
    return nc


def stage_a_body(P, C, io, scr, xmid, xout, h1T_out):
    base = P.top
    M = phase_mod(P, C, io["mod_w"][0], io["mod_bT"][0], scr[0], io["ln_g"][0], io["ln_b"][0])
    yT = P.alloc([128, 8, 2048], BF16)
    b_yT = P.buf()
    phase_even_mixer(P, C, M, io, io["x"], yT, b_yT)
    b_xmid = P.buf()
    phase_outproj_ln(P, C, M, yT, b_yT, io["even_w_out"], io["x"][512:2560, :], xmid, b_xmid)
    P.barrier()
    P.top = base
    M2 = phase_mod(P, C, io["mod_w"][1], io["mod_bT"][1], scr[1], io["ln_g"][1], io["ln_b"][1])
    b_xout = P.buf()
    if STAGE_A_MOE:
        phase_moe(P, C, M2, xmid, xout, b_xout, io, 0)
        P.barrier()
        P.top = base
        M3 = phase_mod(P, C, io["mod_w"][2], io["mod_bT"][2], scr[2], io["ln_g"][0], io["ln_b"][0])
        h1 = P.alloc([128, 8, 2048], BF16)
        h1b = P.bufs(16)
        build_hT(P, C, M3, xout, 16, h1, h1b)
        P.dma("sp", h1T_out, h1, reads=h1b, writes=[P.buf()])
    P.barrier()
    P.top = base


STAGE_A_MOE = True


def _f32(a):
    return np.ascontiguousarray(np.asarray(a, dtype=np.float32))


def _colT(v, ncol):
    return _f32(np.asarray(v).reshape(ncol, 128).T)


def _bias_tables(rel_bias):
    j = np.arange(640)[:, None]
    i = np.arange(128)[None, :]
    d = i + 512 - j
    idx = np.clip(d, -128, 128) + 128
    ck, cq = j // 64, i // 64
    allowed = (ck >= cq) & (ck <= 8 + cq)
    full = np.where(allowed[None], np.asarray(rel_bias, np.float32)[:, idx], np.float32(NEG))
    hi = full.astype(ml_dtypes.bfloat16)
    lo = (full - hi.astype(np.float32)).astype(ml_dtypes.bfloat16)
    out = np.zeros((128, 8, 5, 256), ml_dtypes.bfloat16)
    hi5 = hi.reshape(8, 5, 128, 128).transpose(2, 0, 1, 3)
    lo5 = lo.reshape(8, 5, 128, 128).transpose(2, 0, 1, 3)
    out[..., 0:128] = hi5
    out[..., 128:256] = lo5
    return out


def _moe_consts():
    tp = np.arange(128)[:, None]
    tq = np.arange(128)[None, :]
    return {"ustrict": (tp < tq).astype(np.float32).astype(ml_dtypes.bfloat16),
            "iotaC": _f32(np.broadcast_to((np.arange(NE) * CAP).astype(np.float32)[None], (128, NE)))}


def prep_stage_a(inp):
    x = _f32(inp["x"])
    c = _f32(inp["c"])
    common = {
        **_moe_consts(),
        "ident": np.eye(128, dtype=np.float32),
        "mod_w": _f32(np.concatenate([inp["mod_w"][0], inp["mod_w"][1, 0:1]])),
        "mod_bT": _f32(np.stack([_colT(inp["mod_b"][0, 0], 24), _colT(inp["mod_b"][0, 1], 24),
                                 _colT(inp["mod_b"][1, 0], 24)])),
        "ln_g": _f32(inp["ln_g"][0]),
        "ln_b": _f32(inp["ln_b"][0]),
        "even_w_in": _f32(inp["even_w_in"][0]),
        "pool_scaleT": _colT(inp["pool_scale"][0], 4),
        "pool_w": _f32(inp["pool_w"][0]),
        "biasT": _bias_tables(inp["rel_bias"][0]),
        "even_w_out": _f32(inp["even_w_out"][0]),
        "router_w": _f32(inp["router_w"][0:1]),
        "router_b": _f32(inp["router_b"][0:1]),
        "exp_w_gu": _f32(inp["exp_w_gu"][0:1, 0:(32 if STAGE_A_MOE else 1)]),
        "b_guT": _f32(np.asarray(inp["exp_b_gu"][0]).reshape(32, 16, 128).transpose(2, 1, 0)[None]),
        "exp_w_down": _f32(inp["exp_w_down"][0:1, 0:(32 if STAGE_A_MOE else 1)]),
        "exp_b_down": _f32(inp["exp_b_down"][0:1]),
    }
    wins = np.array([2, 4, 8, 16])
    maps = []
    for core in range(8):
        b, seg = core // 4, core % 4
        t0 = seg * 2048
        xs = np.zeros((2560, 1024), np.float32)
        if seg > 0:
            xs[:] = x[b, t0 - 512:t0 + 2048]
        else:
            xs[512:] = x[b, 0:2048]
        flag = np.zeros((128, 2), np.float32)
        flag[:, 0] = 1.0 if seg > 0 else 0.0
        flag[:, 1] = 0.0 if seg > 0 else NEG
        tpos = np.arange(16)
        if seg > 0:
            cnt = np.broadcast_to(wins[:, None], (4, 16))
        else:
            cnt = np.minimum(tpos[None, :] + 1, wins[:, None])
        icnt = np.broadcast_to((1.0 / cnt).astype(np.float32)[None], (128, 4, 16))
        m = dict(common)
        m["x"] = xs
        m["ccol"] = _colT(c[b], 8)
        m["flag"] = flag
        m["icnt"] = _f32(icnt)
        maps.append(m)
    return maps


I32 = mybir.dt.int32
TWO_PI = 2.0 * math.pi
SB_T = 1024
B_STOP = 0
B_SKIP_FOX = False


def sin_table(P, out, b_out, turns, b_turns, tmpf, tmpi, b_tmp):
    ts(P, "dve", turns, turns, 0.5, None, ALU.add, None, [b_turns], [b_turns])
    cp(P, "dve", tmpi, turns, [b_turns], [b_tmp])
    cp(P, "dve", tmpf, tmpi, [b_tmp], [b_tmp])
    tt(P, "dve", turns, turns, tmpf, ALU.subtract, [b_turns, b_tmp], [b_turns])
    ts(P, "dve", tmpf, turns, 0.0, None, ALU.is_lt, None, [b_turns], [b_tmp])
    tt(P, "dve", turns, turns, tmpf, ALU.add, [b_turns, b_tmp], [b_turns])
    act(P, out, turns, AF.Sin, [b_turns], [b_out], scale=TWO_PI, bias=-math.pi)


def stage_b_body(P, C, io, ygT_out, ydT_out):
    hT_blk = io["hT_blk"]
    NTOK = 8192
    base = P.top
    W = P.alloc([128, 8, 514], BF16)
    b_W = P.buf()
    P.dma("pool", W, io["w"].rearrange("(k p) n -> p k n", p=128), writes=[b_W])
    fb = P.alloc([2, 1], F32)
    b_fb = P.buf()
    P.dma("sp", fb, io["fb"], writes=[b_fb])
    maskT = P.alloc([128, 128], BF16)
    b_mask = P.buf()
    P.dma("sp", maskT, io["maskT"], writes=[b_mask])
    mark_fox = P.top
    QK = [[P.alloc([68, NTOK], BF16) for _ in range(2)] for _ in range(2)]
    qkb = [[P.buf() for _ in range(2)] for _ in range(2)]
    V = P.alloc([128, 64, 130], BF16)
    b_vones = P.buf()
    vb = P.bufs(64)
    memset(P, "pool", V, 1.0, [b_vones])
    FL = P.alloc([2, NTOK], F32)
    b_FL = P.buf()
    ydT = P.alloc([128, NTOK], BF16)
    b_ydT = P.buf()
    for hd in range(2):
        memset(P, "pool", QK[0][hd][64:68], -1.0, [qkb[0][hd]])
        memset(P, "pool", QK[1][hd][64:68], 1.0, [qkb[1][hd]])
    mark_in = P.top
    hblk = [P.alloc([128, 8, 512], BF16) for _ in range(2)]
    hbb = P.bufs(2)
    pss = [P.bank(i) for i in range(8)]
    psb = P.bufs(8)
    it = 0
    for blk in range(16):
        i = blk % 2
        tok = slice(blk * 512, (blk + 1) * 512)
        P.dma("sp", hblk[i], hT_blk(blk), writes=[hbb[i]])
        for which in range(2):
            for hd in range(2):
                cols = slice(128 + which * 128 + hd * 64, 128 + which * 128 + hd * 64 + 64)
                pi = it % 8
                it += 1
                for k in range(8):
                    mm(P, pss[pi][:64, :], W[:, k, cols], hblk[i][:, k, :], k == 0, k == 7, [b_W, hbb[i]], [psb[pi]])
                if which == 0:
                    act(P, QK[0][hd][0:64, tok], pss[pi][:64, :], AF.Copy, [psb[pi]], [qkb[0][hd]], scale=0.125)
                else:
                    cp(P, "dve", QK[1][hd][0:64, tok], pss[pi][:64, :], [psb[pi]], [qkb[1][hd]])
        pi = it % 8
        it += 1
        for k in range(8):
            mm(P, pss[pi][:2, :], W[:, k, 512:514], hblk[i][:, k, :], k == 0, k == 7, [b_W, hbb[i]], [psb[pi]])
        act(P, FL[:, tok], pss[pi][:2, :], AF.Identity, [psb[pi], b_fb], [b_FL], bias=fb[:, 0:1])
        for t4 in range(4):
            t = blk * 4 + t4
            pi = it % 8
            it += 1
            for k in range(8):
                mm(P, pss[pi][:, 0:128], hblk[i][:, k, t4 * 128:(t4 + 1) * 128], W[:, k, 384:512], k == 0, k == 7,
                   [b_W, hbb[i]], [psb[pi]])
            cp(P, "dve" if t4 % 2 else "act", V[:, t, :].rearrange("p (h d) -> p h d", d=65)[:, :, 0:64],
               pss[pi][:, 0:128].rearrange("p (h d) -> p h d", d=64), [psb[pi], b_vones], [vb[t]])
    P.barrier()
    if B_STOP == 1:
        return
    P.top = mark_in
    one2 = P.alloc([2, 1], F32)
    b_one2 = P.buf()
    memset(P, "dve", one2, 1.0, [b_one2])
    act(P, FL, FL, AF.Exp, [b_FL], [b_FL], scale=-1.0)
    act(P, FL, FL, AF.Ln, [b_FL], [b_FL], bias=1.0)
    ts(P, "dve", FL, FL, -1.0, None, ALU.mult, None, [b_FL], [b_FL])
    P.op("dve", lambda e: e.tensor_tensor_scan(out=FL, data0=one2.to_broadcast([2, NTOK]), data1=FL, initial=0.0,
                                               op0=ALU.mult, op1=ALU.add), [b_FL, b_one2], [b_FL])
    CH = 2048
    hrow = [[P.alloc([2, CH], BF16) for _ in range(3)] for _ in range(2)]
    hrb = [[P.buf() for _ in range(3)] for _ in range(2)]
    rres = [P.alloc([2, CH], F32) for _ in range(2)]
    rrb = P.bufs(2)
    FKb = P.alloc([128, 2, 64], F32)
    for cidx in range(NTOK // CH):
        i = cidx % 2
        sl = slice(cidx * CH, (cidx + 1) * CH)
        cp(P, "dve", hrow[i][0], FL[:, sl], [b_FL], [hrb[i][0]])
        tt(P, "dve", rres[i], FL[:, sl], hrow[i][0], ALU.subtract, [b_FL, hrb[i][0]], [rrb[i]])
        cp(P, "dve", hrow[i][1], rres[i], [rrb[i]], [hrb[i][1]])
        tt(P, "dve", rres[i], rres[i], hrow[i][1], ALU.subtract, [rrb[i], hrb[i][1]], [rrb[i]])
        cp(P, "dve", hrow[i][2], rres[i], [rrb[i]], [hrb[i][2]])
        for hd in range(2):
            P.dma("sp", QK[0][hd][64:65, sl], hrow[i][0][hd:hd + 1, :], reads=[hrb[i][0]], writes=[qkb[0][hd]])
            for j in range(3):
                P.dma("sp", QK[1][hd][65 + j:66 + j, sl], hrow[i][j][hd:hd + 1, :], reads=[hrb[i][j]],
                      writes=[qkb[1][hd]])
    P.barrier()
    if B_STOP == 2:
        return
    P.top = mark_in
    pT = [P.alloc([128, 512], BF16) for _ in range(4)]
    pTb = P.bufs(4)
    rs = [P.alloc([128, 2], F32) for _ in range(2)]
    rsb = P.bufs(2)
    yd = [P.alloc([128, 128], BF16) for _ in range(2)]
    ydb = P.bufs(2)
    psS = [P.bank(i) for i in range(4)]
    psSb = P.bufs(4)
    psO = [[P.bank(4), P.bank(5)], [P.bank(6), P.bank(7)]]
    psOb = [[P.buf(), P.buf()], [P.buf(), P.buf()]]
    items = []
    for qt in range(0 if not B_SKIP_FOX else 64, 64):
        for hd in range(2):
            for g0 in range(0, qt + 1, 4):
                items.append((qt, hd, list(range(g0, min(g0 + 4, qt + 1)))))
    ctr = {"n": 0}

    def emit_S(item):
        qt, hd, kts = item
        Q, K = QK[0][hd], QK[1][hd]
        qs = slice(qt * 128, (qt + 1) * 128)
        si = ctr["n"] % 4
        ctr["n"] += 1
        for jj, kt in enumerate(kts):
            mm(P, psS[si][:, jj * 128:(jj + 1) * 128], K[:, kt * 128:(kt + 1) * 128], Q[:, qs], True, kt != qt,
               [qkb[1][hd], qkb[0][hd]], [psSb[si]])
            if kt == qt:
                mm(P, psS[si][:, jj * 128:(jj + 1) * 128], C.identb, maskT, False, True,
                   [C.b_identb, b_mask], [psSb[si]])
        w = len(kts) * 128
        act(P, pT[si][:, 0:w], psS[si][:, 0:w], AF.Exp, [psSb[si]], [pTb[si]])
        return si

    def emit_PV(item, si):
        qt, hd, kts = item
        oi = qt % 2
        qs = slice(qt * 128, (qt + 1) * 128)
        ob = psO[oi][hd][:, 0:65]
        for jj, kt in enumerate(kts):
            mm(P, ob, pT[si][:, jj * 128:(jj + 1) * 128], V[:, kt, hd * 65:(hd + 1) * 65], kt == 0, kt == qt,
               [pTb[si], vb[kt]], [psOb[oi][hd]])
        if kts[-1] != qt:
            return
        P.op("dve", lambda e, oi=oi, hd=hd: e.reciprocal(out=rs[oi][:, hd:hd + 1], in_=psO[oi][hd][:, 64:65]),
             [psOb[oi][hd]], [rsb[oi]])
        ts(P, "dve", yd[oi][:, hd * 64:(hd + 1) * 64], psO[oi][hd][:, 0:64], rs[oi][:, hd:hd + 1], None, ALU.mult,
           None, [psOb[oi][hd], rsb[oi]], [ydb[oi]])
        if hd == 1:
            s2 = ctr["n"] % 4
            ctr["n"] += 1
            pv = psS[s2].bitcast(BF16)
            tr(P, pv[:, 0:128], yd[oi], C.identb, [ydb[oi], C.b_identb], [psSb[s2]])
            cp(P, "act", ydT[:, qs], pv[:, 0:128], [psSb[s2]], [b_ydT])

    if items:
        cur = emit_S(items[0])
        for n_ in range(len(items)):
            nxt = emit_S(items[n_ + 1]) if n_ + 1 < len(items) else None
            emit_PV(items[n_], cur)
            cur = nxt
    P.dma("sp", ydT_out, ydT, reads=[b_ydT], writes=[P.buf()])
    P.barrier()
    if B_STOP == 3:
        return
    P.top = mark_fox
    lam = P.alloc([128, 4, 3], F32)
    b_lam = P.buf()
    P.dma("sp", lam, io["lam"], writes=[b_lam])
    bS = P.alloc([128, 4, 2, 128], F32)
    b_bS = P.buf()
    P.dma("sp", bS, io["bS"], writes=[b_bS])
    cS = P.alloc([128, 4, 2, 128], F32)
    b_cS = P.buf()
    P.dma("sp", cS, io["cS"], writes=[b_cS])
    dcol = P.alloc([128, 1], F32)
    b_dcol = P.buf()
    P.dma("sp", dcol, io["dcol"], writes=[b_dcol])
    jrow = P.alloc([128, SB_T], F32)
    b_jrow = P.buf()
    P.dma("sp", jrow, io["jrow"], writes=[b_jrow])
    sp_ = P.alloc([128, 16, 4], F32)
    b_sp = P.buf()
    DT, LRE, R, TH, KRE, KIM, GRE, GIM, T0, T1, T2, T3, DEN, KT = [sp_[:, i, :] for i in range(14)]
    lre_in, lim_in, ldt_in = lam[:, :, 0], lam[:, :, 1], lam[:, :, 2]
    act(P, DT, ldt_in, AF.Exp, [b_lam], [b_sp])
    ts(P, "dve", LRE, lre_in, -1e-4, None, ALU.min, None, [b_lam], [b_sp])
    tt(P, "dve", T0, LRE, DT, ALU.mult, [b_sp], [b_sp])
    act(P, R, T0, AF.Exp, [b_sp], [b_sp])
    tt(P, "dve", TH, lim_in, DT, ALU.mult, [b_lam, b_sp], [b_sp])
    ts(P, "dve", TH, TH, 1.0 / TWO_PI, None, ALU.mult, None, [b_sp], [b_sp])
    if B_STOP == 5:
        P.barrier()
        return
    cosT = P.alloc([128, 4, SB_T], F32)
    sinT = P.alloc([128, 4, SB_T], F32)
    b_tab = P.buf()
    BD = P.alloc([128, 4, 2, 128], BF16)
    b_BD = P.buf()
    CB = P.alloc([128, 4, 2, 128], BF16)
    b_CB = P.buf()
    Y = P.alloc([128, 2, 4], F32)
    b_Y = P.buf()
    mark_tmp5 = P.top
    tmpa = P.alloc([128, SB_T], F32)
    tmpb = P.alloc([128, SB_T], F32)
    tmpi = P.alloc([128, SB_T], F32).bitcast(I32)
    b_ta, b_tb = P.buf(), P.buf()
    for st in range(4):
        ts(P, "dve", tmpa, jrow, TH[:, st:st + 1], None, ALU.mult, None, [b_jrow, b_sp], [b_ta])
        sin_table(P, sinT[:, st, :], b_tab, tmpa, b_ta, tmpb, tmpi, b_tb)
        ts(P, "dve", tmpa, jrow, TH[:, st:st + 1], 0.25, ALU.mult, ALU.add, [b_jrow, b_sp], [b_ta])
        sin_table(P, cosT[:, st, :], b_tab, tmpa, b_ta, tmpb, tmpi, b_tb)
    if B_STOP == 6:
        P.barrier()
        return
    ts(P, "dve", KT, TH, float(SB_T), None, ALU.mult, None, [b_sp], [b_sp])
    sin_table(P, KIM, b_sp, KT, b_sp, T1, T2.bitcast(I32), b_sp)
    ts(P, "dve", KT, TH, float(SB_T), 0.25, ALU.mult, ALU.add, [b_sp], [b_sp])
    sin_table(P, KRE, b_sp, KT, b_sp, T1, T2.bitcast(I32), b_sp)
    tt(P, "dve", KRE, KRE, R, ALU.mult, [b_sp], [b_sp])
    tt(P, "dve", KIM, KIM, R, ALU.mult, [b_sp], [b_sp])
    if B_STOP == 7:
        P.barrier()
        return
    tt(P, "dve", T0, cosT[:, :, 1], R, ALU.mult, [b_tab, b_sp], [b_sp])
    ts(P, "dve", T0, T0, 1.0, None, ALU.subtract, None, [b_sp], [b_sp])
    tt(P, "dve", T1, sinT[:, :, 1], R, ALU.mult, [b_tab, b_sp], [b_sp])
    tt(P, "dve", DEN, LRE, LRE, ALU.mult, [b_sp], [b_sp])
    tt(P, "dve", T2, lim_in, lim_in, ALU.mult, [b_lam], [b_sp])
    tt(P, "dve", DEN, DEN, T2, ALU.add, [b_sp], [b_sp])
    P.op("dve", lambda e: e.reciprocal(out=DEN, in_=DEN), [b_sp], [b_sp])
    tt(P, "dve", T2, T0, LRE, ALU.mult, [b_sp], [b_sp])
    tt(P, "dve", T3, T1, lim_in, ALU.mult, [b_sp, b_lam], [b_sp])
    tt(P, "dve", T2, T2, T3, ALU.add, [b_sp], [b_sp])
    tt(P, "dve", GRE, T2, DEN, ALU.mult, [b_sp], [b_sp])
    tt(P, "dve", T2, T1, LRE, ALU.mult, [b_sp], [b_sp])
    tt(P, "dve", T3, T0, lim_in, ALU.mult, [b_sp, b_lam], [b_sp])
    tt(P, "dve", T2, T2, T3, ALU.subtract, [b_sp], [b_sp])
    tt(P, "dve", GIM, T2, DEN, ALU.mult, [b_sp], [b_sp])
    if B_STOP == 8:
        P.barrier()
        return
    bbf = P.alloc([128, 128], F32)
    b_bbf = P.buf()
    bbb = P.alloc([128, 128], BF16)
    b_bbb = P.buf()
    pst = P.bank(0)
    b_pst = P.buf()
    pstv = pst.bitcast(BF16)
    for st in range(4):
        for ri in range(2):
            a_, b__ = (bS[:, st, 0, :], bS[:, st, 1, :]) if ri == 0 else (bS[:, st, 1, :], bS[:, st, 0, :])
            ts(P, "dve", bbf, b__, GIM[:, st:st + 1], None, ALU.mult, None, [b_bS, b_sp], [b_bbf])
            stt(P, "dve", bbb, a_, GRE[:, st:st + 1], bbf, ALU.mult, ALU.subtract if ri == 0 else ALU.add,
                [b_bS, b_sp, b_bbf], [b_bbb])
            tr(P, pstv[:, 0:128], bbb, C.identb, [b_bbb, C.b_identb], [b_pst])
            cp(P, "act", BD[:, st, ri, :], pstv[:, 0:128], [b_pst], [b_BD])
        cp(P, "dve", CB[:, st, 0, :], cS[:, st, 0, :], [b_cS], [b_CB])
        ts(P, "dve", CB[:, st, 1, :], cS[:, st, 1, :], -1.0, None, ALU.mult, None, [b_cS], [b_CB])
    memset(P, "dve", Y, 0.0, [b_Y])
    P.barrier()
    if B_STOP == 4:
        return
    P.top = mark_tmp5
    hblk = [P.alloc([128, 8, 512], BF16) for _ in range(2)]
    hbb = P.bufs(2)
    uB = [P.alloc([128, SB_T], BF16) for _ in range(2)]
    uBb = P.bufs(2)
    uF = [P.alloc([128, SB_T], F32) for _ in range(2)]
    uFb = P.bufs(2)
    NB = 2
    BuR = [P.alloc([128, SB_T], F32) for _ in range(NB)]
    BuI = [P.alloc([128, SB_T], F32) for _ in range(NB)]
    t1 = [P.alloc([128, SB_T], F32) for _ in range(NB)]
    t2 = [P.alloc([128, SB_T], F32) for _ in range(NB)]
    t3 = [P.alloc([128, SB_T], F32) for _ in range(NB)]
    t4 = [P.alloc([128, SB_T], F32) for _ in range(NB)]
    bub, t1b, t2b, t3b, t4b = P.bufs(NB), P.bufs(NB), P.bufs(NB), P.bufs(NB), P.bufs(NB)
    xr = [P.alloc([128, 4, SB_T], BF16) for _ in range(2)]
    xi = [P.alloc([128, 4, SB_T], BF16) for _ in range(2)]
    xrb = [P.bufs(4) for _ in range(2)]
    yg = [P.alloc([128, SB_T], F32) for _ in range(2)]
    ygb = P.bufs(2)
    yo = [P.alloc([128, SB_T], BF16) for _ in range(2)]
    yob = P.bufs(2)
    sm4 = P.alloc([128, 8], F32)
    b_sm4 = P.buf()
    psu = [P.bank(0), P.bank(1)]
    psub = P.bufs(2)
    psB = [P.bank(2), P.bank(3), P.bank(4), P.bank(5)]
    psBb = P.bufs(4)
    psy = [P.bank(6), P.bank(7)]
    psyb = P.bufs(2)
    nb = 0
    nbu = 0
    b_ygout = P.buf()
    for sbk in range(NTOK // SB_T):
        si = sbk % 2
        for half in range(2):
            blk = sbk * 2 + half
            i = blk % 2
            P.dma("sp", hblk[i], hT_blk(blk), writes=[hbb[i]])
            for k in range(8):
                mm(P, psu[i], W[:, k, 0:128], hblk[i][:, k, :], k == 0, k == 7, [b_W, hbb[i]], [psub[i]])
            cp(P, "dve", uF[si][:, half * 512:(half + 1) * 512], psu[i], [psub[i]], [uFb[si]])
            cp(P, "pool", uB[si][:, half * 512:(half + 1) * 512], uF[si][:, half * 512:(half + 1) * 512],
               [uFb[si]], [uBb[si]])
        if B_STOP == 9:
            P.barrier()
            return
        for st in range(4):
            bi_ = nb % NB
            nb += 1
            for ri, dst in ((0, BuR[bi_]), (1, BuI[bi_])):
                for half in range(2):
                    pi = nbu % 4
                    nbu += 1
                    mm(P, psB[pi], BD[:, st, ri, :], uB[si][:, half * 512:(half + 1) * 512], True, True,
                       [b_BD, uBb[si]], [psBb[pi]])
                    cp(P, "act", dst[:, half * 512:(half + 1) * 512], psB[pi], [psBb[pi]], [bub[bi_]])
            cs, sn = cosT[:, st, :], sinT[:, st, :]
            tt(P, "pool", t1[bi_], cs, BuR[bi_], ALU.mult, [b_tab, bub[bi_]], [t1b[bi_]])
            tt(P, "dve", t2[bi_], sn, BuI[bi_], ALU.mult, [b_tab, bub[bi_]], [t2b[bi_]])
            tt(P, "dve", t1[bi_], t1[bi_], t2[bi_], ALU.add, [t1b[bi_], t2b[bi_]], [t1b[bi_]])
            tt(P, "pool", t3[bi_], cs, BuI[bi_], ALU.mult, [b_tab, bub[bi_]], [t3b[bi_]])
            tt(P, "pool", t4[bi_], sn, BuR[bi_], ALU.mult, [b_tab, bub[bi_]], [t4b[bi_]])
            tt(P, "dve", t3[bi_], t3[bi_], t4[bi_], ALU.subtract, [t3b[bi_], t4b[bi_]], [t3b[bi_]])
            tt(P, "dve", t1[bi_][:, 0:1], t1[bi_][:, 0:1], Y[:, 0, st:st + 1], ALU.add, [t1b[bi_], b_Y], [t1b[bi_]])
            tt(P, "dve", t3[bi_][:, 0:1], t3[bi_][:, 0:1], Y[:, 1, st:st + 1], ALU.add, [t3b[bi_], b_Y], [t3b[bi_]])
            P.op("dve", lambda e, bi_=bi_, st=st: e.tensor_tensor_scan(out=t1[bi_], data0=R[:, st:st + 1].to_broadcast([128, SB_T]), data1=t1[bi_],
                                                                         initial=0.0, op0=ALU.mult, op1=ALU.add),
                 [t1b[bi_], b_sp], [t1b[bi_]])
            P.op("dve", lambda e, bi_=bi_, st=st: e.tensor_tensor_scan(out=t3[bi_], data0=R[:, st:st + 1].to_broadcast([128, SB_T]), data1=t3[bi_],
                                                                         initial=0.0, op0=ALU.mult, op1=ALU.add),
                 [t3b[bi_], b_sp], [t3b[bi_]])
            er, ei = t1[bi_][:, SB_T - 1:SB_T], t3[bi_][:, SB_T - 1:SB_T]
            ts(P, "dve", sm4[:, 0:1], ei, KIM[:, st:st + 1], None, ALU.mult, None, [t3b[bi_], b_sp], [b_sm4])
            stt(P, "dve", Y[:, 0, st:st + 1], er, KRE[:, st:st + 1], sm4[:, 0:1], ALU.mult, ALU.subtract,
                [t1b[bi_], b_sp, b_sm4], [b_Y])
            ts(P, "dve", sm4[:, 1:2], er, KIM[:, st:st + 1], None, ALU.mult, None, [t1b[bi_], b_sp], [b_sm4])
            stt(P, "dve", Y[:, 1, st:st + 1], ei, KRE[:, st:st + 1], sm4[:, 1:2], ALU.mult, ALU.add,
                [t3b[bi_], b_sp, b_sm4], [b_Y])
            tt(P, "pool", t2[bi_], cs, t1[bi_], ALU.mult, [b_tab, t1b[bi_]], [t2b[bi_]])
            tt(P, "dve", t4[bi_], sn, t3[bi_], ALU.mult, [b_tab, t3b[bi_]], [t4b[bi_]])
            tt(P, "dve", xr[si][:, st, :], t2[bi_], t4[bi_], ALU.subtract, [t2b[bi_], t4b[bi_]], [xrb[si][st]])
            tt(P, "pool", t2[bi_], sn, t1[bi_], ALU.mult, [b_tab, t1b[bi_]], [t2b[bi_]])
            tt(P, "dve", t4[bi_], cs, t3[bi_], ALU.mult, [b_tab, t3b[bi_]], [t4b[bi_]])
            tt(P, "dve", xi[si][:, st, :], t2[bi_], t4[bi_], ALU.add, [t2b[bi_], t4b[bi_]], [xrb[si][st]])
            if B_STOP == 10:
                P.barrier()
                return
        for half in range(2):
            hs = slice(half * 512, (half + 1) * 512)
            pi = half
            for st in range(4):
                mm(P, psy[pi], CB[:, st, 0, :], xr[si][:, st, hs], st == 0, False, [b_CB, xrb[si][st]], [psyb[pi]])
                mm(P, psy[pi], CB[:, st, 1, :], xi[si][:, st, hs], False, st == 3, [b_CB, xrb[si][st]], [psyb[pi]])
            stt(P, "dve", yg[si][:, hs], uF[si][:, hs], dcol[:, 0:1], psy[pi], ALU.mult, ALU.add,
                [uFb[si], b_dcol, psyb[pi]], [ygb[si]])
        g_ = t2[0]
        act(P, g_, yg[si], AF.Square, [ygb[si]], [t2b[0]])
        ts(P, "dve", g_, g_, 0.044715, 1.0, ALU.mult, ALU.add, [t2b[0]], [t2b[0]])
        tt(P, "dve", g_, g_, yg[si], ALU.mult, [t2b[0], ygb[si]], [t2b[0]])
        act(P, g_, g_, AF.Sigmoid, [t2b[0]], [t2b[0]], scale=1.5957691216057308)
        tt(P, "pool", yo[si], g_, yg[si], ALU.mult, [t2b[0], ygb[si]], [yob[si]])
        P.dma("sp", ygT_out[:, sbk * SB_T:(sbk + 1) * SB_T], yo[si], reads=[yob[si]], writes=[b_ygout])
        if B_STOP == 11:
            P.barrier()
            return
    P.barrier()
    P.top = base


def build_stage_b():
    nc = bass.Bass("TRN2", target_bir_lowering=False)
    io = {}
    io["ident"] = dram_in(nc, "ident", [128, 128])
    io["ccol"] = dram_in(nc, "ccol", [128, 8])
    io["hT"] = dram_in(nc, "hT", [128, 8, 8192], BF16)
    io["hT_blk"] = lambda blk: io["hT"][:, :, blk * 512:(blk + 1) * 512]
    io["w"] = dram_in(nc, "w", [1024, 514])
    io["fb"] = dram_in(nc, "fb", [2, 1])
    io["maskT"] = dram_in(nc, "maskT", [128, 128], BF16)
    io["lam"] = dram_in(nc, "lam", [128, 4, 3])
    io["bS"] = dram_in(nc, "bS", [128, 4, 2, 128])
    io["cS"] = dram_in(nc, "cS", [128, 4, 2, 128])
    io["dcol"] = dram_in(nc, "dcol", [128, 1])
    io["jrow"] = dram_in(nc, "jrow", [128, SB_T])
    ygT = nc.dram_tensor("ygT", [128, 8192], BF16, kind="ExternalOutput").ap()
    ydT = nc.dram_tensor("ydT", [128, 8192], BF16, kind="ExternalOutput").ap()
    P = Prog(nc)
    C = phase_consts(P, io)
    P.barrier()
    stage_b_body(P, C, io, ygT, ydT)
    P.emit(final_waits=[P.last[k] for k in P.last if k.startswith("dma:")])
    P.close()
    return nc


def prep_stage_b(inp, h1T):
    c = _f32(inp["c"])
    W = np.asarray(inp["odd_w_in"][0], np.float32)
    k_ = np.arange(128)[:, None]
    q_ = np.arange(128)[None, :]
    maskT = np.where(k_ <= q_, 0.0, NEG).astype(ml_dtypes.bfloat16)
    maps = []
    for core in range(8):
        b, j = core // 4, core % 4
        hT = np.concatenate(h1T[b * 4:(b + 1) * 4], axis=2)
        cols = np.concatenate([np.arange(128 * j, 128 * j + 128), 512 + np.arange(128 * j, 128 * j + 128),
                               1024 + np.arange(128 * j, 128 * j + 128), 1536 + np.arange(128 * j, 128 * j + 128),
                               2048 + np.arange(2 * j, 2 * j + 2)])
        gs = np.arange(8 * j, 8 * j + 8)
        lam = np.zeros((128, 4, 3), np.float32)
        bS = np.zeros((128, 4, 2, 128), np.float32)
        cS = np.zeros((128, 4, 2, 128), np.float32)
        for st in range(4):
            for hh in range(2):
                gl = 2 * st + hh
                g = gs[gl]
                ps_ = slice(hh * 64, (hh + 1) * 64)
                lam[ps_, st, 0] = inp["ssm_lam_re"][0, g]
                lam[ps_, st, 1] = inp["ssm_lam_im"][0, g]
                lam[ps_, st, 2] = inp["ssm_log_dt"][0, g]
                bS[ps_, st, 0, 16 * gl:16 * gl + 16] = inp["ssm_b_re"][0, g]
                bS[ps_, st, 1, 16 * gl:16 * gl + 16] = inp["ssm_b_im"][0, g]
                cS[ps_, st, 0, 16 * gl:16 * gl + 16] = np.asarray(inp["ssm_c_re"][0, g]).T
                cS[ps_, st, 1, 16 * gl:16 * gl + 16] = np.asarray(inp["ssm_c_im"][0, g]).T
        maps.append({
            "ident": np.eye(128, dtype=np.float32),
            "ccol": _colT(c[b], 8),
            "hT": np.ascontiguousarray(hT),
            "w": _f32(W[:, cols]),
            "fb": _f32(np.asarray(inp["forget_b"][0, 2 * j:2 * j + 2]).reshape(2, 1)),
            "maskT": maskT,
            "lam": lam, "bS": bS, "cS": cS,
            "dcol": _f32(np.asarray(inp["ssm_d"][0, gs]).reshape(128, 1)),
            "jrow": _f32(np.broadcast_to(np.arange(SB_T, dtype=np.float32)[None], (128, SB_T))),
        })
    return maps


def build_stage_c():
    nc = bass.Bass("TRN2", target_bir_lowering=False)
    io = {}
    io["ustrict"] = dram_in(nc, "ustrict", [128, 128], BF16)
    io["iotaC"] = dram_in(nc, "iotaC", [128, NE])
    io["ident"] = dram_in(nc, "ident", [128, 128])
    io["ccol"] = dram_in(nc, "ccol", [128, 8])
    io["x1"] = dram_in(nc, "x1", [2048, 1024])
    io["ygT"] = dram_in(nc, "ygT", [128, 4, 2048], BF16)
    io["ydT"] = dram_in(nc, "ydT", [128, 4, 2048], BF16)
    io["mod_w"] = dram_in(nc, "mod_w", [2, 1024, 3072])
    io["mod_bT"] = dram_in(nc, "mod_bT", [2, 128, 24])
    io["ln_g"] = dram_in(nc, "ln_g", [2, 1024])
    io["ln_b"] = dram_in(nc, "ln_b", [2, 1024])
    io["glu_w"] = dram_in(nc, "glu_w", [512, 512])
    io["glu_bT"] = dram_in(nc, "glu_bT", [128, 4])
    io["odd_w_out"] = dram_in(nc, "odd_w_out", [1024, 1024])
    io["router_w"] = dram_in(nc, "router_w", [1, 1024, 32])
    io["router_b"] = dram_in(nc, "router_b", [1, 32])
    io["exp_w_gu"] = dram_in(nc, "exp_w_gu", [1, 32, 1024, 2048])
    io["b_guT"] = dram_in(nc, "b_guT", [1, 128, 16, 32])
    io["exp_w_down"] = dram_in(nc, "exp_w_down", [1, 32, 1024, 1024])
    io["exp_b_down"] = dram_in(nc, "exp_b_down", [1, 32, 1024])
    xmid = nc.dram_tensor("xmid", [2048, 1024], F32, kind="ExternalOutput").ap()
    xout = nc.dram_tensor("xout", [2048, 1024], F32, kind="ExternalOutput").ap()
    scr = nc.dram_tensor("scr_gate", [2, 1024], F32, kind="ExternalOutput").ap()
    P = Prog(nc)
    C = phase_consts(P, io)
    P.barrier()
    stage_c_body(P, C, io, scr, xmid, xout, 0, 0)
    P.emit(final_waits=[P.last[k] for k in P.last if k.startswith("dma:")])
    P.close()
    return nc


def stage_c_body(P, C, io, scr, xmid, xout, mi, layer):
    base = P.top
    M = phase_mod(P, C, io["mod_w"][mi], io["mod_bT"][mi], scr[0], io["ln_g"][mi], io["ln_b"][mi])
    yT = P.alloc([128, 8, 2048], BF16)
    b_yT = P.buf()
    mark = P.top
    yg = P.alloc([128, 4, 2048], BF16)
    b_yg = P.buf()
    P.dma("sp", yg, io["ygT"], writes=[b_yg])
    P.dma("sp", yT[:, 4:8, :], io["ydT"], writes=[b_yT])
    gw = P.alloc([128, 4, 512], BF16)
    b_gw = P.buf()
    P.dma("pool", gw, io["glu_w"].rearrange("(k p) n -> p k n", p=128), writes=[b_gw])
    gb = P.alloc([128, 4], F32)
    b_gb = P.buf()
    P.dma("sp", gb, io["glu_bT"], writes=[b_gb])
    sg = [P.alloc([128, 512], F32) for _ in range(2)]
    sgb = P.bufs(2)
    pss = [P.bank(0), P.bank(1)]
    psb = P.bufs(2)
    n = 0
    for ct in range(4):
        for blk in range(4):
            i = n % 2
            n += 1
            tok = slice(blk * 512, (blk + 1) * 512)
            for ci in range(4):
                mm(P, pss[i], gw[:, ci, ct * 128:(ct + 1) * 128], yg[:, ci, tok], ci == 0, ci == 3, [b_gw, b_yg],
                   [psb[i]])
            act(P, sg[i], pss[i], AF.Sigmoid, [psb[i], b_gb], [sgb[i]], bias=gb[:, ct:ct + 1])
            tt(P, "dve", yT[:, ct, tok], yg[:, ct, tok], sg[i], ALU.mult, [b_yg, sgb[i]], [b_yT])
    P.barrier()
    P.top = mark
    b_xmid = P.buf()
    phase_outproj_ln(P, C, M, yT, b_yT, io["odd_w_out"], io["x1"], xmid, b_xmid)
    P.barrier()
    P.top = base
    M2 = phase_mod(P, C, io["mod_w"][mi + 1], io["mod_bT"][mi + 1], scr[1], io["ln_g"][mi + 1], io["ln_b"][mi + 1])
    b_xout = P.buf()
    phase_moe(P, C, M2, xmid, xout, b_xout, io, layer)
    P.barrier()
    P.top = base


def prep_stage_c(inp, x1, ygT, ydT):
    c = _f32(inp["c"])
    common = {
        **_moe_consts(),
        "ident": np.eye(128, dtype=np.float32),
        "mod_w": _f32(inp["mod_w"][1]),
        "mod_bT": _f32(np.stack([_colT(inp["mod_b"][1, j], 24) for j in range(2)])),
        "ln_g": _f32(inp["ln_g"][1]),
        "ln_b": _f32(inp["ln_b"][1]),
        "glu_w": _f32(inp["ssm_glu_w"][0]),
        "glu_bT": _colT(inp["ssm_glu_b"][0], 4),
        "odd_w_out": _f32(inp["odd_w_out"][0]),
        "router_w": _f32(inp["router_w"][1:2]),
        "router_b": _f32(inp["router_b"][1:2]),
        "exp_w_gu": _f32(inp["exp_w_gu"][1:2]),
        "b_guT": _f32(np.asarray(inp["exp_b_gu"][1]).reshape(32, 16, 128).transpose(2, 1, 0)[None]),
        "exp_w_down": _f32(inp["exp_w_down"][1:2]),
        "exp_b_down": _f32(inp["exp_b_down"][1:2]),
    }
    maps = []
    for core in range(8):
        b, seg = core // 4, core % 4
        tok = slice(seg * 2048, (seg + 1) * 2048)
        m = dict(common)
        m["ccol"] = _colT(c[b], 8)
        m["x1"] = _f32(x1[core])
        m["ygT"] = np.ascontiguousarray(np.stack([ygT[b * 4 + j][:, tok] for j in range(4)], axis=1))
        m["ydT"] = np.ascontiguousarray(np.stack([ydT[b * 4 + j][:, tok] for j in range(4)], axis=1))
        maps.append(m)
    return maps


_NC_CACHE = {}


def _get(name, fn):
    if name not in _NC_CACHE:
        _NC_CACHE[name] = fn()
    return _NC_CACHE[name]


def kernel(**inputs):
    if FUSED:
        return kernel_fused(**inputs)
    inp = {k: np.asarray(v) for k, v in inputs.items()}
    cores = list(range(8))
    ra = run_bass_kernel_spmd(_get("a", build_stage_a), prep_stage_a(inp), core_ids=cores).results
    x1 = [r["xout"] for r in ra]
    h1T = [r["h1T"] for r in ra]
    rb = run_bass_kernel_spmd(_get("b", build_stage_b), prep_stage_b(inp, h1T), core_ids=cores).results
    ygT = [r["ygT"] for r in rb]
    ydT = [r["ydT"] for r in rb]
    rc = run_bass_kernel_spmd(_get("c", build_stage_c), prep_stage_c(inp, x1, ygT, ydT), core_ids=cores).results
    out = np.stack([r["xout"] for r in rc]).reshape(2, 8192, 1024).astype(np.float32)
    return out


def build_fused():
    nc = bass.Bass("TRN2", target_bir_lowering=False)
    io = {}
    io["ustrict"] = dram_in(nc, "ustrict", [128, 128], BF16)
    io["iotaC"] = dram_in(nc, "iotaC", [128, NE])
    io["ident"] = dram_in(nc, "ident", [128, 128])
    io["ccol"] = dram_in(nc, "ccol", [128, 8])
    io["xs"] = dram_in(nc, "xs", [4, 2560, 1024])
    io["flags"] = dram_in(nc, "flags", [4, 128, 2])
    io["icnts"] = dram_in(nc, "icnts", [4, 128, 4, 16])
    io["sel"] = dram_in(nc, "sel", [128, 4])
    io["mod_w"] = dram_in(nc, "mod_w", [4, 1024, 3072])
    io["mod_bT"] = dram_in(nc, "mod_bT", [4, 128, 24])
    io["ln_g"] = dram_in(nc, "ln_g", [4, 1024])
    io["ln_b"] = dram_in(nc, "ln_b", [4, 1024])
    io["even_w_in"] = dram_in(nc, "even_w_in", [1024, 2048])
    io["pool_scaleT"] = dram_in(nc, "pool_scaleT", [128, 4])
    io["pool_w"] = dram_in(nc, "pool_w", [4, 128, 128])
    io["biasT"] = dram_in(nc, "biasT", [128, 8, 5, 256], BF16)
    io["even_w_out"] = dram_in(nc, "even_w_out", [1024, 1024])
    io["router_w"] = dram_in(nc, "router_w", [2, 1024, 32])
    io["router_b"] = dram_in(nc, "router_b", [2, 32])
    io["exp_w_gu"] = dram_in(nc, "exp_w_gu", [2, 32, 1024, 2048])
    io["b_guT"] = dram_in(nc, "b_guT", [2, 128, 16, 32])
    io["exp_w_down"] = dram_in(nc, "exp_w_down", [2, 32, 1024, 1024])
    io["exp_b_down"] = dram_in(nc, "exp_b_down", [2, 32, 1024])
    io["wB"] = dram_in(nc, "wB", [4, 1024, 514])
    io["fbB"] = dram_in(nc, "fbB", [4, 2, 1])
    io["maskT"] = dram_in(nc, "maskT", [128, 128], BF16)
    io["lamB"] = dram_in(nc, "lamB", [4, 128, 4, 3])
    io["bSB"] = dram_in(nc, "bSB", [4, 128, 4, 2, 128])
    io["cSB"] = dram_in(nc, "cSB", [4, 128, 4, 2, 128])
    io["dcolB"] = dram_in(nc, "dcolB", [4, 128, 1])
    io["jrow"] = dram_in(nc, "jrow", [128, SB_T])
    io["glu_w"] = dram_in(nc, "glu_w", [512, 512])
    io["glu_bT"] = dram_in(nc, "glu_bT", [128, 4])
    io["odd_w_out"] = dram_in(nc, "odd_w_out", [1024, 1024])
    out = nc.dram_tensor("out", [2048, 1024], F32, kind="ExternalOutput").ap()

    def scratch(name, shape, dt=F32):
        return nc.dram_tensor(name, list(shape), dt, kind="Internal").ap()
    scr = scratch("scr_gate", [3, 1024])
    xmid = scratch("xmid", [2048, 1024])
    x1_all = scratch("x1_all", [4, 2048, 1024])
    h1T_all = scratch("h1T_all", [4, 128, 8, 2048], BF16)
    ygT_all = scratch("ygT_all", [4, 128, 8192], BF16)
    ydT_all = scratch("ydT_all", [4, 128, 8192], BF16)
    x1_own = scratch("x1_own", [2048, 1024])
    ygT_own = scratch("ygT_own", [128, 4, 2048], BF16)
    ydT_own = scratch("ydT_own", [128, 4, 2048], BF16)
    P = Prog(nc)
    P.mod_cache = {}
    C = phase_consts(P, io)
    P.barrier()
    for seg in range(4):
        ioa = dict(io)
        ioa["x"] = io["xs"][seg]
        ioa["flag"] = io["flags"][seg]
        ioa["icnt"] = io["icnts"][seg]
        stage_a_body(P, C, ioa, scr, xmid, x1_all[seg], h1T_all[seg])
        P.barrier()
    for j in range(4):
        iob = dict(io)
        iob["hT_blk"] = lambda blk: h1T_all[blk // 4][:, :, (blk % 4) * 512:(blk % 4 + 1) * 512]
        iob["w"], iob["fb"], iob["lam"] = io["wB"][j], io["fbB"][j], io["lamB"][j]
        iob["bS"], iob["cS"], iob["dcol"] = io["bSB"][j], io["cSB"][j], io["dcolB"][j]
        stage_b_body(P, C, iob, ygT_all[j], ydT_all[j])
        P.barrier()
    base = P.top
    sel = P.alloc([128, 4], F32)
    b_sel = P.buf()
    P.dma("sp", sel, io["sel"], writes=[b_sel])
    tl = [[P.alloc([128, 1024], F32) for _ in range(4)] for _ in range(2)]
    tlb = [P.bufs(4) for _ in range(2)]
    acc = [P.alloc([128, 1024], F32) for _ in range(2)]
    accb = P.bufs(2)
    b_own = P.buf()
    for t in range(16):
        i = t % 2
        for sg_ in range(4):
            P.dma("sp", tl[i][sg_], x1_all[sg_][t * 128:(t + 1) * 128, :], writes=[tlb[i][sg_]])
        ts(P, "dve", acc[i], tl[i][0], sel[:, 0:1], None, ALU.mult, None, [tlb[i][0], b_sel], [accb[i]])
        for sg_ in range(1, 4):
            stt(P, "dve", acc[i], tl[i][sg_], sel[:, sg_:sg_ + 1], acc[i], ALU.mult, ALU.add,
                [tlb[i][sg_], b_sel, accb[i]], [accb[i]])
        P.dma("sp", x1_own[t * 128:(t + 1) * 128, :], acc[i], reads=[accb[i]], writes=[b_own])
    P.barrier()
    P.top = base
    sel = P.alloc([128, 4], F32)
    b_sel = P.buf()
    P.dma("sp", sel, io["sel"], writes=[b_sel])
    tb2 = [[P.alloc([128, 2048], BF16) for _ in range(4)] for _ in range(2)]
    tb2b = [P.bufs(4) for _ in range(2)]
    ac2 = [P.alloc([128, 2048], F32) for _ in range(2)]
    ac2b = P.bufs(2)
    ao2 = [P.alloc([128, 2048], BF16) for _ in range(2)]
    ao2b = P.bufs(2)
    n = 0
    for src_all, dst in ((ygT_all, ygT_own), (ydT_all, ydT_own)):
        for j in range(4):
            i = n % 2
            n += 1
            for sg_ in range(4):
                P.dma("sp", tb2[i][sg_], src_all[j][:, sg_ * 2048:(sg_ + 1) * 2048], writes=[tb2b[i][sg_]])
            ts(P, "dve", ac2[i], tb2[i][0], sel[:, 0:1], None, ALU.mult, None, [tb2b[i][0], b_sel], [ac2b[i]])
            for sg_ in range(1, 4):
                stt(P, "dve", ac2[i], tb2[i][sg_], sel[:, sg_:sg_ + 1], ac2[i], ALU.mult, ALU.add,
                    [tb2b[i][sg_], b_sel, ac2b[i]], [ac2b[i]])
            cp(P, "act", ao2[i], ac2[i], [ac2b[i]], [ao2b[i]])
            P.dma("sp", dst[:, j, :], ao2[i], reads=[ao2b[i]], writes=[b_own])
    P.barrier()
    P.top = base
    ioc = dict(io)
    ioc["x1"], ioc["ygT"], ioc["ydT"] = x1_own, ygT_own, ydT_own
    stage_c_body(P, C, ioc, scr, xmid, out, 2, 1)
    P.barrier()
    P.emit(final_waits=[P.last[k] for k in P.last if k.startswith("dma:")])
    P.close()
    return nc


def prep_fused(inp):
    x = _f32(inp["x"])
    c = _f32(inp["c"])
    pa = prep_stage_a(inp)
    dummy_h = [np.zeros((128, 8, 2048), ml_dtypes.bfloat16)] * 8
    pb = prep_stage_b(inp, dummy_h)
    mods = [(0, 0), (0, 1), (1, 0), (1, 1)]
    common = {
        **_moe_consts(),
        "ident": np.eye(128, dtype=np.float32),
        "mod_w": _f32(np.stack([inp["mod_w"][l, j] for l, j in mods])),
        "mod_bT": _f32(np.stack([_colT(inp["mod_b"][l, j], 24) for l, j in mods])),
        "ln_g": _f32(np.stack([inp["ln_g"][l, j] for l, j in mods])),
        "ln_b": _f32(np.stack([inp["ln_b"][l, j] for l, j in mods])),
        "even_w_in": pa[0]["even_w_in"], "pool_scaleT": pa[0]["pool_scaleT"], "pool_w": pa[0]["pool_w"],
        "biasT": pa[0]["biasT"], "even_w_out": pa[0]["even_w_out"],
        "router_w": _f32(inp["router_w"]), "router_b": _f32(inp["router_b"]),
        "exp_w_gu": _f32(inp["exp_w_gu"]),
        "b_guT": _f32(np.stack([np.asarray(inp["exp_b_gu"][l]).reshape(32, 16, 128).transpose(2, 1, 0)
                                for l in range(2)])),
        "exp_w_down": _f32(inp["exp_w_down"]), "exp_b_down": _f32(inp["exp_b_down"]),
        "wB": _f32(np.stack([pb[j]["w"] for j in range(4)])),
        "fbB": _f32(np.stack([pb[j]["fb"] for j in range(4)])),
        "maskT": pb[0]["maskT"],
        "lamB": _f32(np.stack([pb[j]["lam"] for j in range(4)])),
        "bSB": _f32(np.stack([pb[j]["bS"] for j in range(4)])),
        "cSB": _f32(np.stack([pb[j]["cS"] for j in range(4)])),
        "dcolB": _f32(np.stack([pb[j]["dcol"] for j in range(4)])),
        "jrow": pb[0]["jrow"],
        "glu_w": _f32(inp["ssm_glu_w"][0]), "glu_bT": _colT(inp["ssm_glu_b"][0], 4),
        "odd_w_out": _f32(inp["odd_w_out"][0]),
    }
    maps = []
    for core in range(8):
        b, seg = core // 4, core % 4
        m = dict(common)
        m["ccol"] = _colT(c[b], 8)
        m["xs"] = np.stack([pa[b * 4 + s]["x"] for s in range(4)])
        m["flags"] = np.stack([pa[b * 4 + s]["flag"] for s in range(4)])
        m["icnts"] = np.stack([pa[b * 4 + s]["icnt"] for s in range(4)])
        sel = np.zeros((128, 4), np.float32)
        sel[:, seg] = 1.0
        m["sel"] = sel
        maps.append(m)
    return maps


FUSED = True


def kernel_fused(**inputs):
    inp = {k: np.asarray(v) for k, v in inputs.items()}
    res = run_bass_kernel_spmd(_get("f", build_fused), prep_fused(inp), core_ids=list(range(8))).results
    return np.stack([r["out"] for r in res]).reshape(2, 8192, 1024).astype(np.float32)


CAP = 768
NCH = CAP // 128
U32 = mybir.dt.uint32
SPARSE_MOE = True


def phase_moe_sparse(P, C, M, x_dram, out_dram, b_out, io, layer, xd, yd):
    rw, rb = io["router_w"][layer], io["router_b"][layer]
    wgu, bguT = io["exp_w_gu"][layer], io["b_guT"][layer]
    wdn, bdn = io["exp_w_down"][layer], io["exp_b_down"][layer]
    BOUND = NE * CAP - 1
    mark0 = P.top
    rwt = P.alloc([128, 8, NE], BF16)
    b_rw = P.buf()
    P.dma("pool", rwt, rw.rearrange("(k p) n -> p k n", p=128), writes=[b_rw])
    rbc = P.alloc([128, NE], F32)
    b_rb = P.buf()
    P.dma("sp", rbc, rb.partition_broadcast(128), writes=[b_rb])
    bdt = P.alloc([32, 1024], F32)
    b_bd = P.buf()
    P.dma("sp", bdt, bdn, writes=[b_bd])
    bgt = P.alloc([128, 16, NE], F32)
    b_bg = P.buf()
    P.dma("sp", bgt, bguT, writes=[b_bg])
    ts(P, "dve", bgt[:, 8:16, :], bgt[:, 8:16, :], 1.0, None, ALU.add, None, [b_bg], [b_bg])
    ustr = P.alloc([128, 128], BF16)
    b_us = P.buf()
    P.dma("sp", ustr, io["ustrict"], writes=[b_us])
    ones = P.alloc([128, 128], BF16)
    b_ones = P.buf()
    memset(P, "dve", ones, 1.0, [b_ones])
    iotaC = P.alloc([128, NE], F32)
    b_io = P.buf()
    P.dma("sp", iotaC, io["iotaC"], writes=[b_io])
    G = P.alloc([128, 16, NE], F32)
    b_G = P.buf()
    GT = P.alloc([32, 2048], F32)
    b_GT = P.buf()
    destu = P.alloc([128, 64], F32).bitcast(U32)
    b_dest = P.bufs(16)
    gk = P.alloc([128, 16, 4], F32)
    b_gk = P.bufs(16)
    maccf = P.alloc([128, NE], F32)
    maccb = P.alloc([128, NE], BF16)
    b_macc = P.buf()
    memset(P, "dve", maccf, 0.0, [b_macc])
    memset(P, "dve", maccb, 0.0, [b_macc])
    b_xd = P.buf()
    b_sc = P.bufs(8)
    mark1 = P.top
    xt = [P.alloc([128, 1024], F32) for _ in range(2)]
    xb = P.bufs(2)
    hTt = [P.alloc([128, 8, 128], BF16) for _ in range(2)]
    hTb = P.bufs(2)
    lg = [P.alloc([128, NE], F32) for _ in range(2)]
    lgb = P.bufs(2)
    m8 = [P.alloc([128, 8], F32) for _ in range(2)]
    m8b = P.bufs(2)
    ex = [P.alloc([128, NE], F32) for _ in range(2)]
    exb = P.bufs(2)
    mk = [P.alloc([128, NE], BF16) for _ in range(2)]
    mkb = P.bufs(2)
    Dt = [P.alloc([128, NE], F32) for _ in range(2)]
    Dtb = P.bufs(2)
    ov = [P.alloc([128, NE], F32) for _ in range(2)]
    ovb = P.bufs(2)
    oh = [P.alloc([128, NE], F32) for _ in range(2)]
    ohb = P.bufs(2)
    tm = [P.alloc([128, NE], F32) for _ in range(2)]
    tmb = P.bufs(2)
    dsf = [P.alloc([128, 8], F32) for _ in range(2)]
    dsb = P.bufs(2)
    psx = [P.bank(0, 2), P.bank(2, 2)]
    psxb = P.bufs(2)
    psl = [P.bank(4), P.bank(5)]
    pslb = P.bufs(2)
    psr = [P.bank(6), P.bank(7)]
    psrb = P.bufs(2)
    for t in range(16):
        i = t % 2
        P.dma("sp", xt[i], x_dram[t * 128:(t + 1) * 128, :], writes=[xb[i]])
        for k in range(8):
            tr(P, psx[i][:, k * 128:(k + 1) * 128], xt[i][:, k * 128:(k + 1) * 128], C.ident, [xb[i], C.b_ident],
               [psxb[i]])
        for k in range(8):
            act(P, hTt[i][:, k, :], psx[i][:, k * 128:(k + 1) * 128], AF.Identity, [psxb[i], M.b_mod], [hTb[i]],
                bias=M.modT[:, k:k + 1], scale=M.modT[:, 8 + k:9 + k])
        for k in range(8):
            mm(P, psl[i][:, 0:NE], hTt[i][:, k, :], rwt[:, k, :], k == 0, k == 7, [hTb[i], b_rw], [pslb[i]])
        tt(P, "dve", lg[i], psl[i][:, 0:NE], rbc, ALU.add, [pslb[i], b_rb], [lgb[i]])
        P.op("dve", lambda e, i=i: e.max(out=m8[i], in_=lg[i]), [lgb[i]], [m8b[i]])
        ts(P, "dve", ex[i], lg[i], m8[i][:, 0:1], None, ALU.subtract, None, [lgb[i], m8b[i]], [exb[i]])
        act(P, ex[i], ex[i], AF.Exp, [exb[i]], [exb[i]])
        ts(P, "dve", mk[i], lg[i], m8[i][:, 3:4], None, ALU.is_ge, None, [lgb[i], m8b[i]], [mkb[i]])
        tt(P, "dve", ex[i], ex[i], mk[i], ALU.mult, [exb[i], mkb[i]], [exb[i]])
        P.op("dve", lambda e, i=i: e.reduce_sum(out=m8[i][:, 4:5], in_=ex[i], axis=AX.X), [exb[i]], [m8b[i]])
        P.op("dve", lambda e, i=i: e.reciprocal(out=m8[i][:, 5:6], in_=m8[i][:, 4:5]), [m8b[i]], [m8b[i]])
        ts(P, "dve", G[:, t, :], ex[i], m8[i][:, 5:6], None, ALU.mult, None, [exb[i], m8b[i]], [b_G])
        mm(P, psr[i][:, 0:NE], ustr, mk[i], True, False, [b_us, mkb[i]], [psrb[i]])
        mm(P, psr[i][:, 0:NE], ones, maccb, False, True, [b_ones, b_macc], [psrb[i]])
        ts(P, "dve", ov[i], psr[i][:, 0:NE], float(CAP), None, ALU.is_ge, None, [psrb[i]], [ovb[i]])
        tt(P, "dve", Dt[i], psr[i][:, 0:NE], iotaC, ALU.add, [psrb[i], b_io], [Dtb[i]])
        stt(P, "dve", Dt[i], ov[i], 1.0e9, Dt[i], ALU.mult, ALU.add, [ovb[i], Dtb[i]], [Dtb[i]])
        tt(P, "dve", maccf, maccf, mk[i], ALU.add, [b_macc, mkb[i]], [b_macc])
        cp(P, "dve", maccb, maccf, [b_macc], [b_macc])
        for k in range(4):
            ts(P, "dve", oh[i], lg[i], m8[i][:, k:k + 1], None, ALU.is_equal, None, [lgb[i], m8b[i]], [ohb[i]])
            tt(P, "dve", tm[i], oh[i], Dt[i], ALU.mult, [ohb[i], Dtb[i]], [tmb[i]])
            P.op("dve", lambda e, i=i, k=k: e.reduce_sum(out=dsf[i][:, k:k + 1], in_=tm[i], axis=AX.X), [tmb[i]],
                 [dsb[i]])
            tt(P, "dve", tm[i], oh[i], G[:, t, :], ALU.mult, [ohb[i], b_G], [tmb[i]])
            P.op("dve", lambda e, i=i, k=k: e.reduce_sum(out=dsf[i][:, 4 + k:5 + k], in_=tm[i], axis=AX.X), [tmb[i]],
                 [dsb[i]])
        ts(P, "dve", tm[i][:, 0:4], dsf[i][:, 0:4], float(BOUND), None, ALU.is_le, None, [dsb[i]], [tmb[i]])
        tt(P, "dve", gk[:, t, :], dsf[i][:, 4:8], tm[i][:, 0:4], ALU.mult, [dsb[i], tmb[i]], [b_gk[t]])
        cp(P, "dve", destu[:, t * 4:t * 4 + 4], dsf[i][:, 0:4], [dsb[i]], [b_dest[t]])
        for k in range(4):
            P._add("pool", lambda e, t=t, k=k, src=xt[i]: e.indirect_dma_start(
                out=xd, out_offset=bass.IndirectOffsetOnAxis(ap=destu[:, t * 4 + k:t * 4 + k + 1], axis=0), in_=src,
                in_offset=None, bounds_check=P.const_reg(e, BOUND), oob_is_err=False), [xb[i], b_dest[t]],
                [b_sc[(t * 4 + k) % 8]], True)
        tr(P, psl[i][:32, 128:256], G[:, t, :], C.ident, [b_G, C.b_ident], [pslb[i]])
        cp(P, "act", GT[:, t * 128:(t + 1) * 128], psl[i][:32, 128:256], [pslb[i]], [b_GT])
    P.barrier()
    P.top = mark1
    wguv = wgu.rearrange("e (k p) n -> e p k n", p=128)
    wdnv = wdn.rearrange("e (k p) n -> e p k n", p=128)
    wg = P.alloc([128, 8, 2048], BF16)
    wgb = P.bufs(2)
    wd = [P.alloc([128, 8, 1024], BF16) for _ in range(2)]
    wdb = P.bufs(2)
    xe = P.alloc([128, NCH, 1024], F32)
    xeb = P.buf()
    xeT = [P.alloc([128, 8, CAP], BF16) for _ in range(2)]
    xeTb = P.bufs(2)
    aT = P.alloc([128, 8, CAP], BF16)
    aTb = P.buf()
    gt = [P.alloc([128, 512], F32) for _ in range(2)]
    gtb = P.bufs(2)
    sg = [P.alloc([128, 512], F32) for _ in range(2)]
    sgb = P.bufs(2)
    up = [P.alloc([128, 512], F32) for _ in range(2)]
    upb = P.bufs(2)
    yo = [P.alloc([128, 1024], F32) for _ in range(2)]
    yob = P.bufs(2)
    b_yd = P.buf()
    pst = [P.bank(0, 2), P.bank(2, 2)]
    pstb = P.bufs(2)
    psg = [P.bank(4), P.bank(5)]
    psgb = P.bufs(2)
    psu = [P.bank(6), P.bank(7)]
    psub = P.bufs(2)
    blks = [(0, 512), (512, CAP)]
    cnt = {"it": 0, "nt": 0}

    def load_wg(e):
        for c in range(2):
            P.dma("pool", wg[:, :, c * 512:(c + 1) * 512], wguv[e][:, :, c * 512:(c + 1) * 512], writes=[wgb[c]])
            P.dma("pool", wg[:, :, 1024 + c * 512:1024 + (c + 1) * 512],
                  wguv[e][:, :, 1024 + c * 512:1024 + (c + 1) * 512], writes=[wgb[c]])

    def load_wd(e):
        for h2 in range(2):
            P.dma("pool", wd[e % 2][:, :, h2 * 512:(h2 + 1) * 512], wdnv[e][:, :, h2 * 512:(h2 + 1) * 512],
                  writes=[wdb[e % 2]])

    def load_xe(e):
        P.dma("sp", xe, xd[e * CAP:(e + 1) * CAP, :].rearrange("(c p) f -> p c f", p=128), reads=[b_xd],
              writes=[xeb])

    def transposes(e):
        for k in range(8):
            i = cnt["nt"] % 2
            cnt["nt"] += 1
            for ch in range(NCH):
                tr(P, pst[i][:, ch * 128:(ch + 1) * 128], xe[:, ch, k * 128:(k + 1) * 128], C.ident,
                   [xeb, C.b_ident], [pstb[i]])
            act(P, xeT[e % 2][:, k, :], pst[i][:, 0:CAP], AF.Identity, [pstb[i], M.b_mod], [xeTb[e % 2]],
                bias=M.modT[:, k:k + 1], scale=M.modT[:, 8 + k:9 + k])

    def gu(e):
        xT, xTb = xeT[e % 2], xeTb[e % 2]
        for ct in range(8):
            c = ct // 4
            for (b0, b1) in blks:
                i = cnt["it"] % 2
                cnt["it"] += 1
                w_ = b1 - b0
                for k in range(8):
                    mm(P, psg[i][:, 0:w_], wg[:, k, ct * 128:(ct + 1) * 128], xT[:, k, b0:b1], k == 0, k == 7,
                       [wgb[c], xTb], [psgb[i]])
                for k in range(8):
                    mm(P, psu[i][:, 0:w_], wg[:, k, 1024 + ct * 128:1024 + (ct + 1) * 128], xT[:, k, b0:b1], k == 0,
                       k == 7, [wgb[c], xTb], [psub[i]])
                ts(P, "dve", gt[i][:, 0:w_], psg[i][:, 0:w_], bgt[:, ct, e:e + 1], 7.0, ALU.add, ALU.min,
                   [psgb[i], b_bg], [gtb[i]])
                act(P, sg[i][:, 0:w_], gt[i][:, 0:w_], AF.Sigmoid, [gtb[i]], [sgb[i]], scale=1.702)
                ts(P, "dve", up[i][:, 0:w_], psu[i][:, 0:w_], bgt[:, 8 + ct, e:e + 1], -6.0, ALU.add, ALU.max,
                   [psub[i], b_bg], [upb[i]])
                tt(P, "dve", sg[i][:, 0:w_], sg[i][:, 0:w_], gt[i][:, 0:w_], ALU.mult, [sgb[i], gtb[i]], [sgb[i]])
                stt(P, "dve", aT[:, ct, b0:b1], up[i][:, 0:w_], 8.0, sg[i][:, 0:w_], ALU.min, ALU.mult,
                    [sgb[i], upb[i]], [aTb])

    def down(e):
        for ch in range(NCH):
            i = cnt["nt"] % 2
            cnt["nt"] += 1
            for cb in range(2):
                for k in range(8):
                    mm(P, pst[i][:, cb * 512:(cb + 1) * 512], aT[:, k, ch * 128:(ch + 1) * 128],
                       wd[e % 2][:, k, cb * 512:(cb + 1) * 512], k == 0, k == 7, [aTb, wdb[e % 2]], [pstb[i]])
            cp(P, "act", yo[i], pst[i], [pstb[i]], [yob[i]])
            P.dma("sp", yd[e * CAP + ch * 128:e * CAP + (ch + 1) * 128, :], yo[i], reads=[yob[i]], writes=[b_yd])

    load_wg(0)
    load_wd(0)
    load_xe(0)
    transposes(0)
    for e in range(NE):
        if e + 1 < NE:
            load_xe(e + 1)
        gu(e)
        if e + 1 < NE:
            load_wg(e + 1)
            load_wd(e + 1)
            transposes(e + 1)
        down(e)
    P.barrier()
    P.top = mark1
    yk = [[P.alloc([128, 1024], F32) for _ in range(4)] for _ in range(2)]
    ykb = [P.bufs(4) for _ in range(2)]
    for i in range(2):
        for k in range(4):
            memset(P, "pool", yk[i][k], 0.0, [ykb[i][k]])
    xt = [P.alloc([128, 1024], F32) for _ in range(2)]
    xb = P.bufs(2)
    acc = [P.alloc([128, 1024], F32) for _ in range(2)]
    accb = P.bufs(2)
    ot = [P.alloc([128, 1024], F32) for _ in range(2)]
    ob = P.bufs(2)
    sm = [P.alloc([128, 16], F32) for _ in range(2)]
    sb = P.bufs(2)
    psd = [P.bank(4, 2), P.bank(6, 2)]
    psdb = P.bufs(2)
    for t in range(16):
        i = t % 2
        P.dma("sp", xt[i], x_dram[t * 128:(t + 1) * 128, :], writes=[xb[i]])
        for k in range(4):
            P._add("pool", lambda e, t=t, k=k, dst=yk[i][k]: e.indirect_dma_start(
                out=dst, out_offset=None, in_=yd,
                in_offset=bass.IndirectOffsetOnAxis(ap=destu[:, t * 4 + k:t * 4 + k + 1], axis=0),
                bounds_check=P.const_reg(e, BOUND), oob_is_err=False), [b_yd, b_dest[t]], [ykb[i][k]], True)
        for cb in range(2):
            mm(P, psd[i][:, cb * 512:(cb + 1) * 512], GT[:, t * 128:(t + 1) * 128], bdt[:, cb * 512:(cb + 1) * 512],
               True, True, [b_GT, b_bd], [psdb[i]])
        stt(P, "dve", acc[i], yk[i][0], gk[:, t, 0:1], psd[i], ALU.mult, ALU.add, [ykb[i][0], b_gk[t], psdb[i]],
            [accb[i]])
        for k in range(1, 4):
            stt(P, "dve", acc[i], yk[i][k], gk[:, t, k:k + 1], acc[i], ALU.mult, ALU.add,
                [ykb[i][k], b_gk[t], accb[i]], [accb[i]])
        tt(P, "dve", acc[i], acc[i], M.gate_bc, ALU.mult, [accb[i], M.b_bc_buf], [accb[i]])
        stt(P, "dve", acc[i], xt[i], ALPHA, acc[i], ALU.mult, ALU.add, [xb[i], accb[i]], [accb[i]])
        ln_tile(P, M, acc[i], accb[i], ot[i], ob[i], sm[i], sb[i])
        P.dma("sp", out_dram[t * 128:(t + 1) * 128, :], ot[i], reads=[ob[i]], writes=[b_out])
    P.barrier()
    P.top = mark0
```

```python
import contextlib
import math
import numpy as np
import ml_dtypes
import concourse.bass as bass
import concourse.mybir as mybir
from concourse.bass_utils import run_bass_kernel_spmd

F32 = mybir.dt.float32
BF16 = mybir.dt.bfloat16
AF = mybir.ActivationFunctionType
ALU = mybir.AluOpType
AX = mybir.AxisListType

SAME_ENGINE_SYNC = True
DEBUG = False
NEG = -30000.0
D = 1024
NE = 32
ALPHA = (2.0 * 2) ** 0.25
LN_EPS = 1e-5


class Buf:
    __slots__ = ("name", "w", "r")

    def __init__(self, name=""):
        self.name = name
        self.w = None
        self.r = []


class Op:
    __slots__ = ("eng", "idx", "fn", "deps", "signal", "count", "is_dma", "sem_key", "snap")


class Prog:
    ENGS = ("pe", "act", "dve", "pool", "sp")

    def __init__(self, nc, sbuf_bytes=184 * 1024):
        self.nc = nc
        self.ops = {e: [] for e in self.ENGS}
        self.known = {e: {} for e in self.ENGS}
        self.stack = contextlib.ExitStack()
        self._cnt = {}
        self.last = {}
        self.streams = {}
        self.free_slots = []
        self.nslots = 0
        self.all_ops = []
        self._regs = {}
        self.arena_words = sbuf_bytes // 4
        self.arena = self.stack.enter_context(nc.sbuf_tensor("arena", [128, self.arena_words], F32))
        self.ps = self.stack.enter_context(nc.psum_tensor("psall", [128, 4096], F32))
        self.top = 0
        self.nb = 0

    def alloc(self, shape, dtype=F32):
        free = 1
        for s in shape[1:]:
            free *= s
        esz = 4 if dtype == F32 else 2
        words = (free * esz + 3) // 4
        words = (words + 15) // 16 * 16
        assert self.top + words <= self.arena_words, f"SBUF arena overflow {self.top}+{words}"
        v = self.arena[:, self.top:self.top + words]
        self.top += words
        if dtype != F32:
            v = v.bitcast(dtype)
        v = v[:, :free]
        if len(shape) == 3:
            v = v.rearrange("p (a b) -> p a b", b=shape[2])
        elif len(shape) == 4:
            v = v.rearrange("p (a b c) -> p a b c", b=shape[2], c=shape[3])
        if shape[0] < 128:
            v = v[:shape[0]]
        return v

    def bank(self, i, n=1):
        return self.ps[:, i * 512:(i + n) * 512]

    def buf(self, name=""):
        self.nb += 1
        return Buf(name or f"b{self.nb}")

    def bufs(self, n):
        return [self.buf() for _ in range(n)]

    def _add(self, eng, fn, reads, writes, is_dma, extra=()):
        op = Op()
        op.eng = eng
        op.fn = fn
        op.is_dma = is_dma
        op.signal = bool(is_dma)
        op.count = None
        if is_dma:
            sb_ = writes[0] if writes else reads[0]
            key = self.streams.get(sb_)
            if key is None:
                if self.free_slots:
                    key = self.free_slots.pop()
                else:
                    key = "dma:%d" % self.nslots
                    self.nslots += 1
                self.streams[sb_] = key
            op.sem_key = key
        else:
            op.sem_key = eng
        op.idx = self._cnt.get(op.sem_key, 0)
        if fn is not None:
            self._cnt[op.sem_key] = op.idx + 1
        deps = list(extra)
        for b in reads:
            if b.w is not None:
                deps.append(b.w)
        for b in writes:
            if b.w is not None:
                deps.append(b.w)
            deps.extend(b.r)
        kn = self.known[eng]
        need = {}
        for d in deps:
            if d.sem_key == eng and (eng == "pe" or not SAME_ENGINE_SYNC):
                continue
            if kn.get(d.sem_key, -1) >= d.idx:
                continue
            cur = need.get(d.sem_key)
            if cur is None or cur.idx < d.idx:
                need[d.sem_key] = d
        op.deps = list(need.values())
        for d in op.deps:
            d.signal = True
            if kn.get(d.sem_key, -1) < d.idx:
                kn[d.sem_key] = d.idx
            for k, v in d.snap.items():
                if kn.get(k, -1) < v:
                    kn[k] = v
        op.snap = dict(kn)
        if fn is not None:
            for b in reads:
                b.r.append(op)
            for b in writes:
                b.w = op
                b.r = []
            self.last[op.sem_key] = op
        self.ops[eng].append(op)
        self.all_ops.append(op)
        return op

    def op(self, eng, fn, reads=(), writes=()):
        return self._add(eng, fn, reads, writes, False)

    def dma(self, eng, out, in_, reads=(), writes=(), **kw):
        return self._add(eng, lambda e: e.dma_start(out=out, in_=in_, **kw), reads, writes, True)

    def dump(self, name, ap, b, shape, dtype=F32):
        if not DEBUG:
            return
        d = self.nc.dram_tensor("dbg_" + name, list(shape), dtype, kind="ExternalOutput").ap()
        self.dma("sp", d, ap, reads=[b])

    def const_reg(self, e, value):
        if value not in self._regs:
            r = e.alloc_register("c%d" % value)
            e.reg_mov(r, value)
            self._regs[value] = r
        return self._regs[value]

    def barrier(self):
        lasts = list(self.last.values())
        for e in self.ENGS:
            self._add(e, None, (), (), False, extra=lasts)
        self.streams = {}
        self.free_slots = ["dma:%d" % i for i in range(self.nslots)]

    def emit(self, final_waits=()):
        nc = self.nc
        for d in final_waits:
            d.signal = True
        keys = set()
        for e in self.ENGS:
            for o in self.ops[e]:
                if o.fn is not None:
                    keys.add(o.sem_key)
        counts = {k: 0 for k in keys}
        for o in self.all_ops:
            if o.fn is not None and o.signal:
                counts[o.sem_key] += 16 if o.is_dma else 1
                o.count = counts[o.sem_key]
        self.maxcounts = counts
        sems = {}
        for k in sorted(keys):
            sems[k] = self.stack.enter_context(nc.semaphore(k.replace(":", "_")))
        block = self.stack.enter_context(nc.Block())

        def run(eng_name, e):
            for o in self.ops[eng_name]:
                for d in o.deps:
                    e.wait_ge(sems[d.sem_key], d.count)
                if o.fn is None:
                    continue
                ins = o.fn(e)
                if o.signal:
                    ins.then_inc(sems[o.sem_key], 16 if o.is_dma else 1)
            if eng_name == "sp":
                for d in final_waits:
                    e.wait_ge(sems[d.sem_key], d.count)

        @block.sync
        def _(e):
            run("sp", e)

        @block.scalar
        def _(e):
            run("act", e)

        @block.vector
        def _(e):
            run("dve", e)

        @block.gpsimd
        def _(e):
            run("pool", e)

        @block.tensor
        def _(e):
            run("pe", e)

    def close(self):
        self.stack.close()


def mm(P, out, lhsT, rhs, start, stop, reads, writes):
    return P.op("pe", lambda e: e.matmul(out, lhsT=lhsT, rhs=rhs, start=start, stop=stop), reads, writes)


def tr(P, out, in_, ident, reads, writes):
    return P.op("pe", lambda e: e.transpose(out, in_, ident), reads, writes)


def act(P, out, in_, func, reads, writes, bias=0.0, scale=1.0, eng="act"):
    return P.op(eng, lambda e: e.activation(out=out, in_=in_, func=func, bias=bias, scale=scale), reads, writes)


def tt(P, eng, out, in0, in1, op, reads, writes):
    return P.op(eng, lambda e: e.tensor_tensor(out=out, in0=in0, in1=in1, op=op), reads, writes)


def ts(P, eng, out, in0, s1, s2, op0, op1, reads, writes):
    if s2 is None:
        return P.op(eng, lambda e: e.tensor_scalar(out=out, in0=in0, scalar1=s1, scalar2=None, op0=op0), reads, writes)
    return P.op(eng, lambda e: e.tensor_scalar(out=out, in0=in0, scalar1=s1, scalar2=s2, op0=op0, op1=op1), reads, writes)


def stt(P, eng, out, in0, scalar, in1, op0, op1, reads, writes):
    return P.op(eng, lambda e: e.scalar_tensor_tensor(out=out, in0=in0, scalar=scalar, in1=in1, op0=op0, op1=op1),
                reads, writes)


def cp(P, eng, out, in_, reads, writes):
    if eng == "act":
        return P.op("act", lambda e: e.copy(out=out, in_=in_), reads, writes)
    return P.op(eng, lambda e: e.tensor_copy(out=out, in_=in_), reads, writes)


def memset(P, eng, ap, val, writes):
    return P.op(eng, lambda e: e.memset(ap, val), (), writes)


class Ctx:
    pass


def phase_consts(P, io):
    C = Ctx()
    C.ident = P.alloc([128, 128], F32)
    C.identb = P.alloc([128, 128], BF16)
    C.b_ident = P.buf()
    P.dma("sp", C.ident, io["ident"], writes=[C.b_ident])
    C.b_identb = P.buf()
    cp(P, "dve", C.identb, C.ident, [C.b_ident], [C.b_identb])
    C.ccol = P.alloc([128, 8], F32)
    C.b_ccol = P.buf()
    P.dma("sp", C.ccol, io["ccol"], writes=[C.b_ccol])
    act(P, C.ccol, C.ccol, AF.Silu, [C.b_ccol], [C.b_ccol])
    return C


def phase_mod(P, C, mod_w, mod_b, scr_gate, lng, lnb):
    M = Ctx()
    M.modT = P.alloc([128, 24], F32)
    M.gate_bc = P.alloc([128, 1024], F32)
    M.g_bc = P.alloc([128, 1024], F32)
    M.b_bc = P.alloc([128, 1024], F32)
    M.b_mod = P.buf()
    M.b_bc_buf = P.buf()
    key = int(mod_w.offset)
    cache = getattr(P, "mod_cache", None)
    if cache is not None and key in cache:
        P.dma("sp", M.modT, cache[key][0], writes=[M.b_mod])
        P.dma("sp", M.gate_bc, cache[key][1].partition_broadcast(128), writes=[M.b_bc_buf])
        P.dma("sp", M.g_bc, lng.partition_broadcast(128), writes=[M.b_bc_buf])
        P.dma("sp", M.b_bc, lnb.partition_broadcast(128), writes=[M.b_bc_buf])
        P.barrier()
        return M
    mark = P.top
    wt = [P.alloc([128, 8, 512], F32) for _ in range(2)]
    wb = P.bufs(2)
    mb = P.alloc([128, 24], F32)
    b_mb = P.buf()
    P.dma("sp", mb, mod_b, writes=[b_mb])
    ps = P.bank(0)
    b_ps = P.buf()
    wv = mod_w.rearrange("(k p) n -> p k n", p=128)
    for cb in range(6):
        w = wt[cb % 2]
        P.dma("sp", w, wv[:, :, cb * 512:(cb + 1) * 512], writes=[wb[cb % 2]])
        for c4 in range(4):
            ct = cb * 4 + c4
            for k in range(8):
                mm(P, ps[:, ct:ct + 1], w[:, k, c4 * 128:(c4 + 1) * 128], C.ccol[:, k:k + 1], k == 0, k == 7,
                   [wb[cb % 2], C.b_ccol], [b_ps])
    tt(P, "dve", M.modT, ps[:, 0:24], mb, ALU.add, [b_ps, b_mb], [M.b_mod])
    ts(P, "dve", M.modT[:, 8:24], M.modT[:, 8:24], 1.0, None, ALU.add, None, [M.b_mod], [M.b_mod])
    b_scr = P.buf()
    gsb = P.alloc([8, 128], F32)
    b_gsb = P.buf()
    ps2 = P.bank(1)
    b_ps2 = P.buf()
    tr(P, ps2[:8, 0:128], M.modT[:, 16:24], C.ident, [M.b_mod, C.b_ident], [b_ps2])
    cp(P, "act", gsb, ps2[:8, 0:128], [b_ps2], [b_gsb])
    P.dma("sp", scr_gate.rearrange("(c p) -> c p", p=128), gsb, reads=[b_gsb], writes=[b_scr])
    P.dma("sp", M.gate_bc, scr_gate.partition_broadcast(128), reads=[b_scr], writes=[M.b_bc_buf])
    P.dma("sp", M.g_bc, lng.partition_broadcast(128), writes=[M.b_bc_buf])
    P.dma("sp", M.b_bc, lnb.partition_broadcast(128), writes=[M.b_bc_buf])
    if cache is not None and key is not None:
        cache[key] = (P.nc.dram_tensor("modT_cache%d" % len(cache), [128, 24], F32, kind="Internal").ap(), scr_gate)
        P.dma("sp", cache[key][0], M.modT, reads=[M.b_mod], writes=[P.buf()])
    P.dump("modT%d" % P.nb, M.modT, M.b_mod, [128, 24])
    P.dump("gbc%d" % P.nb, M.gate_bc, M.b_bc_buf, [128, 1024])
    P.barrier()
    P.top = mark
    return M


def build_hT(P, C, M, x_dram, ntiles, hT, hT_bufs, ps_banks=(0, 4)):
    mark = P.top
    xt = [P.alloc([128, 1024], F32) for _ in range(2)]
    xb = P.bufs(2)
    pss = [P.bank(ps_banks[0], 2), P.bank(ps_banks[1], 2)]
    psb = P.bufs(2)
    for t in range(ntiles):
        x_ = xt[t % 2]
        P.dma("sp", x_, x_dram[t * 128:(t + 1) * 128, :], writes=[xb[t % 2]])
        ps = pss[t % 2]
        for k in range(8):
            tr(P, ps[:, k * 128:(k + 1) * 128], x_[:, k * 128:(k + 1) * 128], C.ident, [xb[t % 2], C.b_ident],
               [psb[t % 2]])
        for k in range(8):
            eng = "act"
            act(P, hT[:, k, t * 128:(t + 1) * 128], ps[:, k * 128:(k + 1) * 128], AF.Identity,
                [psb[t % 2], M.b_mod], [hT_bufs[t]], bias=M.modT[:, k:k + 1], scale=M.modT[:, 8 + k:9 + k])
    P.barrier()
    P.top = mark


def ln_tile(P, M, tmp, b_tmp, out, b_out, sm, b_sm):
    P.op("dve", lambda e: e.bn_stats(out=sm[:, 0:6], in_=tmp[:, 0:512]), [b_tmp], [b_sm])
    P.op("dve", lambda e: e.bn_stats(out=sm[:, 6:12], in_=tmp[:, 512:1024]), [b_tmp], [b_sm])
    P.op("dve", lambda e: e.bn_aggr(out=sm[:, 12:14], in_=sm[:, 0:12].rearrange("p (a b) -> p a b", b=6)),
         [b_sm], [b_sm])
    ts(P, "dve", sm[:, 15:16], sm[:, 13:14], LN_EPS, None, ALU.add, None, [b_sm], [b_sm])
    act(P, sm[:, 15:16], sm[:, 15:16], AF.Sqrt, [b_sm], [b_sm])
    P.op("dve", lambda e: e.reciprocal(out=sm[:, 14:15], in_=sm[:, 15:16]), [b_sm], [b_sm])
    ts(P, "dve", tmp, tmp, sm[:, 12:13], sm[:, 14:15], ALU.subtract, ALU.mult, [b_tmp, b_sm], [b_tmp])
    tt(P, "pool", tmp, tmp, M.g_bc, ALU.mult, [b_tmp, M.b_bc_buf], [b_tmp])
    tt(P, "pool", out, tmp, M.b_bc, ALU.add, [b_tmp, M.b_bc_buf], [b_out])


def phase_outproj_ln(P, C, M, yT, b_yT, w_out, x_dram, xo_dram, b_xo, ntiles=16):
    mark = P.top
    wo = P.alloc([128, 8, 1024], BF16)
    b_wo = P.buf()
    wv = w_out.rearrange("(k p) n -> p k n", p=128)
    for h in range(2):
        P.dma("pool", wo[:, :, h * 512:(h + 1) * 512], wv[:, :, h * 512:(h + 1) * 512], writes=[b_wo])
    xt = [P.alloc([128, 1024], F32) for _ in range(2)]
    xb = P.bufs(2)
    tm = [P.alloc([128, 1024], F32) for _ in range(2)]
    tb = P.bufs(2)
    ot = [P.alloc([128, 1024], F32) for _ in range(2)]
    ob = P.bufs(2)
    sm = [P.alloc([128, 16], F32) for _ in range(2)]
    sb = P.bufs(2)
    pss = [P.bank(0, 2), P.bank(2, 2)]
    psb = P.bufs(2)
    for t in range(ntiles):
        i = t % 2
        P.dma("sp", xt[i], x_dram[t * 128:(t + 1) * 128, :], writes=[xb[i]])
        for cb in range(2):
            for k in range(8):
                mm(P, pss[i][:, cb * 512:(cb + 1) * 512], yT[:, k, t * 128:(t + 1) * 128],
                   wo[:, k, cb * 512:(cb + 1) * 512], k == 0, k == 7, [b_yT, b_wo], [psb[i]])
        tt(P, "dve", tm[i], pss[i], M.gate_bc, ALU.mult, [psb[i], M.b_bc_buf], [tb[i]])
        stt(P, "dve", tm[i], xt[i], ALPHA, tm[i], ALU.mult, ALU.add, [xb[i], tb[i]], [tb[i]])
        ln_tile(P, M, tm[i], tb[i], ot[i], ob[i], sm[i], sb[i])
        P.dma("sp", xo_dram[t * 128:(t + 1) * 128, :], ot[i], reads=[ob[i]], writes=[b_xo])
    P.barrier()
    P.top = mark


def phase_moe(P, C, M, x_dram, out_dram, b_out, io, layer):
    if SPARSE_MOE:
        if not hasattr(P, "moe_scr"):
            P.moe_scr = (P.nc.dram_tensor("moe_xd", [NE * CAP, 1024], F32, kind="Internal").ap(),
                         P.nc.dram_tensor("moe_yd", [NE * CAP, 1024], F32, kind="Internal").ap())
        return phase_moe_sparse(P, C, M, x_dram, out_dram, b_out, io, layer, P.moe_scr[0], P.moe_scr[1])
    rw, rb = io["router_w"][layer], io["router_b"][layer]
    wgu, bguT = io["exp_w_gu"][layer], io["b_guT"][layer]
    wdn, bdn = io["exp_w_down"][layer], io["exp_b_down"][layer]
    mark0 = P.top
    rwt = P.alloc([128, 8, NE], BF16)
    b_rw = P.buf()
    P.dma("pool", rwt, rw.rearrange("(k p) n -> p k n", p=128), writes=[b_rw])
    rbc = P.alloc([128, NE], F32)
    b_rb = P.buf()
    P.dma("sp", rbc, rb.partition_broadcast(128), writes=[b_rb])
    bdt = P.alloc([32, 1024], F32)
    b_bd = P.buf()
    P.dma("sp", bdt, bdn, writes=[b_bd])
    bgt = P.alloc([128, 16, NE], F32)
    b_bg = P.buf()
    P.dma("sp", bgt, bguT, writes=[b_bg])
    ts(P, "dve", bgt[:, 8:16, :], bgt[:, 8:16, :], 1.0, None, ALU.add, None, [b_bg], [b_bg])
    wguv = wgu.rearrange("e (k p) n -> e p k n", p=128)
    wdnv = wdn.rearrange("e (k p) n -> e p k n", p=128)
    markh = P.top
    for half in range(2):
        P.top = markh
        t0 = half * 8
        hT = P.alloc([128, 8, 1024], BF16)
        hTb = P.bufs(8)
        build_hT(P, C, M, x_dram[half * 1024:(half + 1) * 1024, :], 8, hT, hTb)
        G = P.alloc([128, 8, NE], F32)
        b_G = P.buf()
        GT = P.alloc([32, 1024], F32)
        b_GT = P.buf()
        yacc = P.alloc([128, 8, 1024], F32)
        yb = P.bufs(8)
        mark1 = P.top
        lg = [P.alloc([128, NE], F32) for _ in range(2)]
        lgb = P.bufs(2)
        m8 = [P.alloc([128, 8], F32) for _ in range(2)]
        m8b = P.bufs(2)
        ex = [P.alloc([128, NE], F32) for _ in range(2)]
        exb = P.bufs(2)
        pss = [P.bank(0), P.bank(1)]
        psb = P.bufs(2)
        pst = [P.bank(2), P.bank(3)]
        pstb = P.bufs(2)
        for t in range(8):
            i = t % 2
            for k in range(8):
                mm(P, pss[i][:, 0:NE], hT[:, k, t * 128:(t + 1) * 128], rwt[:, k, :], k == 0, k == 7,
                   [hTb[t], b_rw], [psb[i]])
            tt(P, "dve", lg[i], pss[i][:, 0:NE], rbc, ALU.add, [psb[i], b_rb], [lgb[i]])
            P.op("dve", lambda e, i=i: e.max(out=m8[i], in_=lg[i]), [lgb[i]], [m8b[i]])
            ts(P, "dve", ex[i], lg[i], m8[i][:, 0:1], None, ALU.subtract, None, [lgb[i], m8b[i]], [exb[i]])
            act(P, ex[i], ex[i], AF.Exp, [exb[i]], [exb[i]])
            stt(P, "dve", ex[i], lg[i], m8[i][:, 3:4], ex[i], ALU.is_ge, ALU.mult, [lgb[i], m8b[i], exb[i]],
                [exb[i]])
            P.op("dve", lambda e, i=i: e.reduce_sum(out=m8[i][:, 4:5], in_=ex[i], axis=AX.X), [exb[i]], [m8b[i]])
            P.op("dve", lambda e, i=i: e.reciprocal(out=m8[i][:, 5:6], in_=m8[i][:, 4:5]), [m8b[i]], [m8b[i]])
            ts(P, "dve", G[:, t, :], ex[i], m8[i][:, 5:6], None, ALU.mult, None, [exb[i], m8b[i]], [b_G])
            tr(P, pst[i][:32, 0:128], G[:, t, :], C.ident, [b_G, C.b_ident], [pstb[i]])
            cp(P, "act", GT[:, t * 128:(t + 1) * 128], pst[i][:32, 0:128], [pstb[i]], [b_GT])
        P.barrier()
        P.top = mark1
        wg = P.alloc([128, 8, 2048], BF16)
        wgb = P.bufs(2)
        wd = P.alloc([128, 8, 1024], BF16)
        wdb = P.buf()
        aT = P.alloc([128, 8, 1024], BF16)
        aTb = P.bufs(2)
        gt = [P.alloc([128, 512], F32) for _ in range(2)]
        gtb = P.bufs(2)
        sg = [P.alloc([128, 512], F32) for _ in range(2)]
        sgb = P.bufs(2)
        up = [P.alloc([128, 512], F32) for _ in range(2)]
        upb = P.bufs(2)
        psg = [P.bank(0), P.bank(1)]
        psgb = P.bufs(2)
        psu = [P.bank(2), P.bank(3)]
        psub = P.bufs(2)
        psd = [P.bank(4, 2), P.bank(6, 2)]
        psdb = P.bufs(2)
        it = 0
        for e in range(NE):
            for c in range(2):
                P.dma("pool", wg[:, :, c * 512:(c + 1) * 512], wguv[e][:, :, c * 512:(c + 1) * 512], writes=[wgb[c]])
                P.dma("pool", wg[:, :, 1024 + c * 512:1024 + (c + 1) * 512],
                      wguv[e][:, :, 1024 + c * 512:1024 + (c + 1) * 512], writes=[wgb[c]])
            for ct in range(8):
                c = ct // 4
                for tb_ in range(2):
                    i = it % 2
                    it += 1
                    tok = slice(tb_ * 512, (tb_ + 1) * 512)
                    hb = hTb[tb_ * 4: tb_ * 4 + 4]
                    for k in range(8):
                        mm(P, psg[i], wg[:, k, ct * 128:(ct + 1) * 128], hT[:, k, tok], k == 0, k == 7,
                           [wgb[c]] + hb, [psgb[i]])
                    for k in range(8):
                        mm(P, psu[i], wg[:, k, 1024 + ct * 128:1024 + (ct + 1) * 128], hT[:, k, tok], k == 0, k == 7,
                           [wgb[c]] + hb, [psub[i]])
                    ts(P, "dve", gt[i], psg[i], bgt[:, ct, e:e + 1], 7.0, ALU.add, ALU.min, [psgb[i], b_bg], [gtb[i]])
                    act(P, sg[i], gt[i], AF.Sigmoid, [gtb[i]], [sgb[i]], scale=1.702)
                    ts(P, "dve", up[i], psu[i], bgt[:, 8 + ct, e:e + 1], -6.0, ALU.add, ALU.max, [psub[i], b_bg],
                       [upb[i]])
                    tt(P, "pool", sg[i], sg[i], gt[i], ALU.mult, [sgb[i], gtb[i]], [sgb[i]])
                    stt(P, "dve", aT[:, ct, tok], up[i], 8.0, sg[i], ALU.min, ALU.mult, [sgb[i], upb[i]], [aTb[tb_]])
            for h2 in range(2):
                P.dma("pool", wd[:, :, h2 * 512:(h2 + 1) * 512], wdnv[e][:, :, h2 * 512:(h2 + 1) * 512],
                      writes=[wdb])
            for t in range(8):
                i = t % 2
                for cb in range(2):
                    for k in range(8):
                        mm(P, psd[i][:, cb * 512:(cb + 1) * 512], aT[:, k, t * 128:(t + 1) * 128],
                           wd[:, k, cb * 512:(cb + 1) * 512], k == 0, k == 7, [aTb[t // 4], wdb], [psdb[i]])
                if e == 0:
                    ts(P, "dve", yacc[:, t, :], psd[i], G[:, t, e:e + 1], None, ALU.mult, None,
                       [psdb[i], b_G], [yb[t]])
                else:
                    stt(P, "dve", yacc[:, t, :], psd[i], G[:, t, e:e + 1], yacc[:, t, :], ALU.mult, ALU.add,
                        [psdb[i], b_G, yb[t]], [yb[t]])
        P.barrier()
        P.top = mark1
        xt = [P.alloc([128, 1024], F32) for _ in range(2)]
        xb = P.bufs(2)
        ot = [P.alloc([128, 1024], F32) for _ in range(2)]
        ob = P.bufs(2)
        sm = [P.alloc([128, 16], F32) for _ in range(2)]
        sb = P.bufs(2)
        for t in range(8):
            i = t % 2
            tg = t0 + t
            P.dma("sp", xt[i], x_dram[tg * 128:(tg + 1) * 128, :], writes=[xb[i]])
            for cb in range(2):
                mm(P, psd[i][:, cb * 512:(cb + 1) * 512], GT[:, t * 128:(t + 1) * 128],
                   bdt[:, cb * 512:(cb + 1) * 512], True, True, [b_GT, b_bd], [psdb[i]])
            tt(P, "dve", yacc[:, t, :], yacc[:, t, :], psd[i], ALU.add, [psdb[i], yb[t]], [yb[t]])
            tt(P, "dve", yacc[:, t, :], yacc[:, t, :], M.gate_bc, ALU.mult, [yb[t], M.b_bc_buf], [yb[t]])
            stt(P, "dve", yacc[:, t, :], xt[i], ALPHA, yacc[:, t, :], ALU.mult, ALU.add, [xb[i], yb[t]], [yb[t]])
            ln_tile(P, M, yacc[:, t, :], yb[t], ot[i], ob[i], sm[i], sb[i])
            P.dma("sp", out_dram[tg * 128:(tg + 1) * 128, :], ot[i], reads=[ob[i]], writes=[b_out])
        P.barrier()
    P.top = mark0


def phase_even_mixer(P, C, M, io, x_dram_halo, yT, b_yT):
    w_in = io["even_w_in"]
    mark0 = P.top
    NU = 2176
    uT = P.alloc([128, 4, NU], BF16)
    ub = P.bufs(4)
    qT = P.alloc([128, 4, 2048], BF16)
    qb = P.bufs(4)
    kT = P.alloc([128, 4, 2560], BF16)
    kb = P.bufs(4)
    V = P.alloc([128, 20, 8 * 65], BF16)
    vb = P.bufs(20)
    b_vones = P.buf()
    memset(P, "pool", V, 1.0, [b_vones])
    flag = P.alloc([128, 2], F32)
    b_flag = P.buf()
    P.dma("sp", flag, io["flag"], writes=[b_flag])
    icnt = P.alloc([128, 4, 16], F32)
    b_icnt = P.buf()
    P.dma("sp", icnt, io["icnt"], writes=[b_icnt])
    psc = P.alloc([128, 4], F32)
    b_psc = P.buf()
    P.dma("sp", psc, io["pool_scaleT"], writes=[b_psc])
    wp = P.alloc([128, 4, 128], BF16)
    b_wp = P.buf()
    P.dma("pool", wp, io["pool_w"].rearrange("g c d -> c g d"), writes=[b_wp])
    mark_tmp = P.top
    hT = P.alloc([128, 8, 2560], BF16)
    hTb = P.bufs(20)
    build_hT(P, C, M, x_dram_halo, 20, hT, hTb)
    wi2 = [P.alloc([128, 8, 512], BF16) for _ in range(2)]
    wi2b = P.bufs(2)
    wv = w_in.rearrange("(k p) n -> p k n", p=128)

    class WI:
        loaded = -1

        def __getitem__(self, key):
            _, k, cols = key
            c = cols.start // 512
            if c > WI.loaded:
                assert c == WI.loaded + 1
                P.dma("pool", wi2[c % 2], wv[:, :, c * 512:(c + 1) * 512], writes=[wi2b[c % 2]])
                WI.loaded = c
            return wi2[c % 2][:, k, cols.start - c * 512:cols.stop - c * 512]
    wi = WI()
    wib = [wi2b[0], wi2b[1], wi2b[0], wi2b[1]]
    pss = [P.bank(i) for i in range(4)]
    psb = P.bufs(4)
    it = 0
    allh = hTb

    def proj(cols, tok0, ntok, evac):
        nonlocal it
        i = it % 4
        it += 1
        t_lo, t_hi = tok0 // 128, (tok0 + ntok + 127) // 128
        for k in range(8):
            mm(P, pss[i][:, 0:ntok], wi[:, k, cols], hT[:, k, tok0:tok0 + ntok], k == 0, k == 7,
               [wib[cols.start // 512]] + allh[t_lo:t_hi], [psb[i]])
        evac(pss[i][:, 0:ntok], psb[i])

    for g in range(4):
        for blk in range(5):
            tok0 = 384 + blk * 512
            ntok = min(512, 2560 - tok0)
            proj(slice(g * 128, (g + 1) * 128), tok0, ntok,
                 lambda ps, b, g=g, blk=blk, ntok=ntok: cp(P, "act", uT[:, g, blk * 512:blk * 512 + ntok], ps, [b],
                                                          [ub[g]]))
    for c in range(4):
        for blk in range(4):
            proj(slice(512 + c * 128, 512 + (c + 1) * 128), 512 + blk * 512, 512,
                 lambda ps, b, c=c, blk=blk: act(P, qT[:, c, blk * 512:(blk + 1) * 512], ps, AF.Copy, [b], [qb[c]],
                                                 scale=0.125))
        for blk in range(5):
            proj(slice(1024 + c * 128, 1024 + (c + 1) * 128), blk * 512, 512,
                 lambda ps, b, c=c, blk=blk: cp(P, "dve", kT[:, c, blk * 512:(blk + 1) * 512], ps, [b], [kb[c]]))
    for t in range(20):
        i = it % 4
        it += 1
        for k in range(8):
            mm(P, pss[i], hT[:, k, t * 128:(t + 1) * 128], wi[:, k, 1536:2048], k == 0, k == 7, [wib[3], hTb[t]],
               [psb[i]])
        cp(P, "dve" if t % 2 else "act", V[:, t, :].rearrange("p (h d) -> p h d", d=65)[:, :, 0:64],
           pss[i].rearrange("p (h d) -> p h d", d=64), [psb[i], b_vones], [vb[t]])
    P.dump("hT", hT[:, :, 512:640], hTb[4], [128, 8, 128], BF16)
    P.dump("ident", C.ident, C.b_ident, [128, 128])
    P.dump("uT", uT, ub[0], [128, 4, NU], BF16)
    P.dump("qT", qT, qb[0], [128, 4, 2048], BF16)
    P.dump("kT", kT, kb[0], [128, 4, 2560], BF16)
    P.dump("V", V, vb[0], [128, 20, 520], BF16)
    P.barrier()
    P.top = mark_tmp
    for g in range(4):
        w = 2 ** (g + 1)
        ts(P, "dve", uT[:, g, 0:128], uT[:, g, 0:128], flag[:, 0:1], None, ALU.mult, None, [ub[g], b_flag], [ub[g]])
    sA = P.alloc([128, NU], F32)
    sB = P.alloc([128, NU], F32)
    b_sA, b_sB = P.buf(), P.buf()
    dT = P.alloc([128, 4, 2048], BF16)
    db = P.bufs(4)
    for g in range(4):
        w = 2 ** (g + 1)
        eng = "dve" if g % 2 == 0 else "pool"
        src, b_src = uT[:, g, :], ub[g]
        m = 1
        cur, b_cur = sA, b_sA
        lo = 0
        while m < w:
            lo_new = lo + m
            tt(P, eng, cur[:, lo_new:NU], src[:, lo_new:NU], src[:, lo_new - m:NU - m], ALU.add, [b_src], [b_cur])
            src, b_src = cur, b_cur
            cur, b_cur = (sB, b_sB) if cur is sA else (sA, b_sA)
            lo = lo_new
            m *= 2
        stt(P, "dve", dT[:, g, 16:2048], src[:, 144:NU], 1.0 / w, uT[:, g, 144:NU], ALU.mult, ALU.subtract,
            [b_src, ub[g]], [db[g]])
        tt(P, eng, cur[:, 0:16], src[:, 128:144], icnt[:, g, :], ALU.mult, [b_src, b_icnt], [b_cur])
        tt(P, eng, dT[:, g, 0:16], cur[:, 0:16], uT[:, g, 128:144], ALU.subtract, [b_cur, ub[g]], [db[g]])
    it = 0
    for g in range(4):
        for blk in range(4):
            i = it % 4
            it += 1
            mm(P, pss[i], wp[:, g, :], dT[:, g, blk * 512:(blk + 1) * 512], True, True, [b_wp, db[g]], [psb[i]])
            act(P, yT[:, g, blk * 512:(blk + 1) * 512], pss[i], AF.Copy, [psb[i], b_psc], [b_yT],
                scale=psc[:, g:g + 1])
    P.barrier()
    P.top = P.top
    BT = P.alloc([128, 8, 5, 256], BF16)
    b_BT = P.buf()
    P.dma("sp", BT, io["biasT"], writes=[b_BT])
    zero = P.alloc([128, 1], F32)
    b_zero = P.buf()
    memset(P, "dve", zero, 0.0, [b_zero])
    pT = [P.alloc([128, 5, 128], BF16) for _ in range(2)]
    pTb = P.bufs(2)
    rs = [P.alloc([128, 8], F32) for _ in range(2)]
    rsb = P.bufs(2)
    ya = [P.alloc([128, 512], BF16) for _ in range(2)]
    yab = P.bufs(2)
    psS = [P.bank(0, 2), P.bank(2, 2)]
    psSb = P.bufs(2)
    psO = [P.bank(4, 2), P.bank(6, 2)]
    psOb = P.bufs(2)
    psT = P.bank(4, 2)
    def emit_S(qt, h, si):
        c, r0 = h // 2, (h % 2) * 64
        for j in range(5):
            kt = qt + j
            mm(P, psS[si][:, j * 128:(j + 1) * 128], kT[r0:r0 + 64, c, kt * 128:(kt + 1) * 128],
               qT[r0:r0 + 64, c, qt * 128:(qt + 1) * 128], True, False, [kb[c], qb[c]], [psSb[si]])
            mm(P, psS[si][:, j * 128:(j + 1) * 128], C.identb, BT[:, h, j, 0:128], False, False,
               [C.b_identb, b_BT], [psSb[si]])
            mm(P, psS[si][:, j * 128:(j + 1) * 128], C.identb, BT[:, h, j, 128:256], False, True,
               [C.b_identb, b_BT], [psSb[si]])
        for j in range(5):
            kt = qt + j
            bias = flag[:, 1:2] if kt < 4 else zero[:, 0:1]
            act(P, pT[si][:, j, :], psS[si][:, j * 128:(j + 1) * 128], AF.Exp, [psSb[si], b_flag, b_zero],
                [pTb[si]], bias=bias)

    def emit_PV(qt, h, si):
        oi = qt % 2
        ob = psO[oi][:, (h // 4) * 512 + (h % 4) * 65:(h // 4) * 512 + (h % 4) * 65 + 65]
        for j in range(5):
            kt = qt + j
            mm(P, ob, pT[si][:, j, :], V[:, kt, h * 65:(h + 1) * 65], j == 0, j == 4, [pTb[si], vb[kt]],
               [psOb[oi]])
        if h != 7:
            return
        for hb_ in range(2):
            ov = psO[oi][:, hb_ * 512:hb_ * 512 + 260].rearrange("p (h d) -> p h d", d=65)
            P.op("dve", lambda e, ov=ov, oi=oi, hb_=hb_: e.reciprocal(out=rs[oi][:, hb_ * 4:(hb_ + 1) * 4],
                                                                        in_=ov[:, :, 64]), [psOb[oi]], [rsb[oi]])
            for hh in range(4):
                h2 = hb_ * 4 + hh
                ts(P, "dve", ya[oi][:, h2 * 64:(h2 + 1) * 64], ov[:, hh, 0:64], rs[oi][:, h2:h2 + 1], None, ALU.mult,
                   None, [psOb[oi], rsb[oi]], [yab[oi]])
        sT = (si + 1) % 2
        psTv = psS[si].bitcast(BF16)
        for c in range(4):
            tr(P, psTv[:, c * 128:(c + 1) * 128], ya[oi][:, c * 128:(c + 1) * 128], C.identb,
               [yab[oi], C.b_identb], [psSb[si]])
        cp(P, "act", yT[:, 4:8, qt * 128:(qt + 1) * 128],
           psTv[:, 0:512].rearrange("p (c q) -> p c q", q=128), [psSb[si]], [b_yT])

    items = [(qt, h) for qt in range(16) for h in range(8)]
    emit_S(items[0][0], items[0][1], 0)
    for n_ in range(len(items)):
        if n_ + 1 < len(items):
            emit_S(items[n_ + 1][0], items[n_ + 1][1], (n_ + 1) % 2)
        emit_PV(items[n_][0], items[n_][1], n_ % 2)
    P.dump("yT", yT, b_yT, [128, 8, 2048], BF16)
    P.barrier()
    P.top = mark0


def dram_in(nc, name, shape, dtype=F32):
    return nc.dram_tensor(name, list(shape), dtype, kind="ExternalInput").ap()


def build_stage_a():
    nc = bass.Bass("TRN2", target_bir_lowering=False)
    io = {}
    io["x"] = dram_in(nc, "x", [2560, 1024])
    io["ustrict"] = dram_in(nc, "ustrict", [128, 128], BF16)
    io["iotaC"] = dram_in(nc, "iotaC", [128, NE])
    io["ident"] = dram_in(nc, "ident", [128, 128])
    io["ccol"] = dram_in(nc, "ccol", [128, 8])
    io["mod_w"] = dram_in(nc, "mod_w", [3, 1024, 3072])
    io["mod_bT"] = dram_in(nc, "mod_bT", [3, 128, 24])
    io["ln_g"] = dram_in(nc, "ln_g", [2, 1024])
    io["ln_b"] = dram_in(nc, "ln_b", [2, 1024])
    io["even_w_in"] = dram_in(nc, "even_w_in", [1024, 2048])
    io["flag"] = dram_in(nc, "flag", [128, 2])
    io["icnt"] = dram_in(nc, "icnt", [128, 4, 16])
    io["pool_scaleT"] = dram_in(nc, "pool_scaleT", [128, 4])
    io["pool_w"] = dram_in(nc, "pool_w", [4, 128, 128])
    io["biasT"] = dram_in(nc, "biasT", [128, 8, 5, 256], BF16)
    io["even_w_out"] = dram_in(nc, "even_w_out", [1024, 1024])
    io["router_w"] = dram_in(nc, "router_w", [1, 1024, 32])
    io["router_b"] = dram_in(nc, "router_b", [1, 32])
    if not STAGE_A_MOE:
        NEX = 1
    else:
        NEX = 32
    io["exp_w_gu"] = dram_in(nc, "exp_w_gu", [1, NEX, 1024, 2048])
    io["b_guT"] = dram_in(nc, "b_guT", [1, 128, 16, 32])
    io["exp_w_down"] = dram_in(nc, "exp_w_down", [1, NEX, 1024, 1024])
    io["exp_b_down"] = dram_in(nc, "exp_b_down", [1, 32, 1024])
    xmid = nc.dram_tensor("xmid", [2048, 1024], F32, kind="ExternalOutput").ap()
    xout = nc.dram_tensor("xout", [2048, 1024], F32, kind="ExternalOutput").ap()
    scr = nc.dram_tensor("scr_gate", [3, 1024], F32, kind="ExternalOutput").ap()
    h1T_out = nc.dram_tensor("h1T", [128, 8, 2048], BF16, kind="ExternalOutput").ap()
    P = Prog(nc)
    C = phase_consts(P, io)
    P.barrier()
    stage_a_body(P, C, io, scr, xmid, xout, h1T_out)
    P.emit(final_waits=[P.last[k] for k in P.last if k.startswith("dma:")])
    P.close()
    return nc


def stage_a_body(P, C, io, scr, xmid, xout, h1T_out):
    base = P.top
    M = phase_mod(P, C, io["mod_w"][0], io["mod_bT"][0], scr[0], io["ln_g"][0], io["ln_b"][0])
    yT = P.alloc([128, 8, 2048], BF16)
    b_yT = P.buf()
    phase_even_mixer(P, C, M, io, io["x"], yT, b_yT)
    b_xmid = P.buf()
    phase_outproj_ln(P, C, M, yT, b_yT, io["even_w_out"], io["x"][512:2560, :], xmid, b_xmid)
    P.barrier()
    P.top = base
    M2 = phase_mod(P, C, io["mod_w"][1], io["mod_bT"][1], scr[1], io["ln_g"][1], io["ln_b"][1])
    b_xout = P.buf()
    if STAGE_A_MOE:
        phase_moe(P, C, M2, xmid, xout, b_xout, io, 0)
        P.barrier()
        P.top = base
        M3 = phase_mod(P, C, io["mod_w"][2], io["mod_bT"][2], scr[2], io["ln_g"][0], io["ln_b"][0])
        h1 = P.alloc([128, 8, 2048], BF16)
        h1b = P.bufs(16)
        build_hT(P, C, M3, xout, 16, h1, h1b)
        P.dma("sp", h1T_out, h1, reads=h1b, writes=[P.buf()])
    P.barrier()
    P.top = base


STAGE_A_MOE = True


def _f32(a):
    return np.ascontiguousarray(np.asarray(a, dtype=np.float32))


def _colT(v, ncol):
    return _f32(np.asarray(v).reshape(ncol, 128).T)


def _bias_tables(rel_bias):
    j = np.arange(640)[:, None]
    i = np.arange(128)[None, :]
    d = i + 512 - j
    idx = np.clip(d, -128, 128) + 128
    ck, cq = j // 64, i // 64
    allowed = (ck >= cq) & (ck <= 8 + cq)
    full = np.where(allowed[None], np.asarray(rel_bias, np.float32)[:, idx], np.float32(NEG))
    hi = full.astype(ml_dtypes.bfloat16)
    lo = (full - hi.astype(np.float32)).astype(ml_dtypes.bfloat16)
    out = np.zeros((128, 8, 5, 256), ml_dtypes.bfloat16)
    hi5 = hi.reshape(8, 5, 128, 128).transpose(2, 0, 1, 3)
    lo5 = lo.reshape(8, 5, 128, 128).transpose(2, 0, 1, 3)
    out[..., 0:128] = hi5
    out[..., 128:256] = lo5
    return out


def _moe_consts():
    tp = np.arange(128)[:, None]
    tq = np.arange(128)[None, :]
    return {"ustrict": (tp < tq).astype(np.float32).astype(ml_dtypes.bfloat16),
            "iotaC": _f32(np.broadcast_to((np.arange(NE) * CAP).astype(np.float32)[None], (128, NE)))}


def prep_stage_a(inp):
    x = _f32(inp["x"])
    c = _f32(inp["c"])
    common = {
        **_moe_consts(),
        "ident": np.eye(128, dtype=np.float32),
        "mod_w": _f32(np.concatenate([inp["mod_w"][0], inp["mod_w"][1, 0:1]])),
        "mod_bT": _f32(np.stack([_colT(inp["mod_b"][0, 0], 24), _colT(inp["mod_b"][0, 1], 24),
                                 _colT(inp["mod_b"][1, 0], 24)])),
        "ln_g": _f32(inp["ln_g"][0]),
        "ln_b": _f32(inp["ln_b"][0]),
        "even_w_in": _f32(inp["even_w_in"][0]),
        "pool_scaleT": _colT(inp["pool_scale"][0], 4),
        "pool_w": _f32(inp["pool_w"][0]),
        "biasT": _bias_tables(inp["rel_bias"][0]),
        "even_w_out": _f32(inp["even_w_out"][0]),
        "router_w": _f32(inp["router_w"][0:1]),
        "router_b": _f32(inp["router_b"][0:1]),
        "exp_w_gu": _f32(inp["exp_w_gu"][0:1, 0:(32 if STAGE_A_MOE else 1)]),
        "b_guT": _f32(np.asarray(inp["exp_b_gu"][0]).reshape(32, 16, 128).transpose(2, 1, 0)[None]),
        "exp_w_down": _f32(inp["exp_w_down"][0:1, 0:(32 if STAGE_A_MOE else 1)]),
        "exp_b_down": _f32(inp["exp_b_down"][0:1]),
    }
    wins = np.array([2, 4, 8, 16])
    maps = []
    for core in range(8):
        b, seg = core // 4, core % 4
        t0 = seg * 2048
        xs = np.zeros((2560, 1024), np.float32)
        if seg > 0:
            xs[:] = x[b, t0 - 512:t0 + 2048]
        else:
            xs[512:] = x[b, 0:2048]
        flag = np.zeros((128, 2), np.float32)
        flag[:, 0] = 1.0 if seg > 0 else 0.0
        flag[:, 1] = 0.0 if seg > 0 else NEG
        tpos = np.arange(16)
        if seg > 0:
            cnt = np.broadcast_to(wins[:, None], (4, 16))
        else:
            cnt = np.minimum(tpos[None, :] + 1, wins[:, None])
        icnt = np.broadcast_to((1.0 / cnt).astype(np.float32)[None], (128, 4, 16))
        m = dict(common)
        m["x"] = xs
        m["ccol"] = _colT(c[b], 8)
        m["flag"] = flag
        m["icnt"] = _f32(icnt)
        maps.append(m)
    return maps


I32 = mybir.dt.int32
TWO_PI = 2.0 * math.pi
SB_T = 1024
B_STOP = 0
B_SKIP_FOX = False


def sin_table(P, out, b_out, turns, b_turns, tmpf, tmpi, b_tmp):
    ts(P, "dve", turns, turns, 0.5, None, ALU.add, None, [b_turns], [b_turns])
    cp(P, "dve", tmpi, turns, [b_turns], [b_tmp])
    cp(P, "dve", tmpf, tmpi, [b_tmp], [b_tmp])
    tt(P, "dve", turns, turns, tmpf, ALU.subtract, [b_turns, b_tmp], [b_turns])
    ts(P, "dve", tmpf, turns, 0.0, None, ALU.is_lt, None, [b_turns], [b_tmp])
    tt(P, "dve", turns, turns, tmpf, ALU.add, [b_turns, b_tmp], [b_turns])
    act(P, out, turns, AF.Sin, [b_turns], [b_out], scale=TWO_PI, bias=-math.pi)


def stage_b_body(P, C, io, ygT_out, ydT_out):
    hT_blk = io["hT_blk"]
    NTOK = 8192
    base = P.top
    W = P.alloc([128, 8, 514], BF16)
    b_W = P.buf()
    P.dma("pool", W, io["w"].rearrange("(k p) n -> p k n", p=128), writes=[b_W])
    fb = P.alloc([2, 1], F32)
    b_fb = P.buf()
    P.dma("sp", fb, io["fb"], writes=[b_fb])
    maskT = P.alloc([128, 128], BF16)
    b_mask = P.buf()
    P.dma("sp", maskT, io["maskT"], writes=[b_mask])
    mark_fox = P.top
    QK = [[P.alloc([68, NTOK], BF16) for _ in range(2)] for _ in range(2)]
    qkb = [[P.buf() for _ in range(2)] for _ in range(2)]
    V = P.alloc([128, 64, 130], BF16)
    b_vones = P.buf()
    vb = P.bufs(64)
    memset(P, "pool", V, 1.0, [b_vones])
    FL = P.alloc([2, NTOK], F32)
    b_FL = P.buf()
    ydT = P.alloc([128, NTOK], BF16)
    b_ydT = P.buf()
    for hd in range(2):
        memset(P, "pool", QK[0][hd][64:68], -1.0, [qkb[0][hd]])
        memset(P, "pool", QK[1][hd][64:68], 1.0, [qkb[1][hd]])
    mark_in = P.top
    hblk = [P.alloc([128, 8, 512], BF16) for _ in range(2)]
    hbb = P.bufs(2)
    pss = [P.bank(i) for i in range(8)]
    psb = P.bufs(8)
    it = 0
    for blk in range(16):
        i = blk % 2
        tok = slice(blk * 512, (blk + 1) * 512)
        P.dma("sp", hblk[i], hT_blk(blk), writes=[hbb[i]])
        for which in range(2):
            for hd in range(2):
                cols = slice(128 + which * 128 + hd * 64, 128 + which * 128 + hd * 64 + 64)
                pi = it % 8
                it += 1
                for k in range(8):
                    mm(P, pss[pi][:64, :], W[:, k, cols], hblk[i][:, k, :], k == 0, k == 7, [b_W, hbb[i]], [psb[pi]])
                if which == 0:
                    act(P, QK[0][hd][0:64, tok], pss[pi][:64, :], AF.Copy, [psb[pi]], [qkb[0][hd]], scale=0.125)
                else:
                    cp(P, "dve", QK[1][hd][0:64, tok], pss[pi][:64, :], [psb[pi]], [qkb[1][hd]])
        pi = it % 8
        it += 1
        for k in range(8):
            mm(P, pss[pi][:2, :], W[:, k, 512:514], hblk[i][:, k, :], k == 0, k == 7, [b_W, hbb[i]], [psb[pi]])
        act(P, FL[:, tok], pss[pi][:2, :], AF.Identity, [psb[pi], b_fb], [b_FL], bias=fb[:, 0:1])
        for t4 in range(4):
            t = blk * 4 + t4
            pi = it % 8
            it += 1
            for k in range(8):
                mm(P, pss[pi][:, 0:128], hblk[i][:, k, t4 * 128:(t4 + 1) * 128], W[:, k, 384:512], k == 0, k == 7,
                   [b_W, hbb[i]], [psb[pi]])
            cp(P, "dve" if t4 % 2 else "act", V[:, t, :].rearrange("p (h d) -> p h d", d=65)[:, :, 0:64],
               pss[pi][:, 0:128].rearrange("p (h d) -> p h d", d=64), [psb[pi], b_vones], [vb[t]])
    P.barrier()
    if B_STOP == 1:
        return
    P.top = mark_in
    one2 = P.alloc([2, 1], F32)
    b_one2 = P.buf()
    memset(P, "dve", one2, 1.0, [b_one2])
    act(P, FL, FL, AF.Exp, [b_FL], [b_FL], scale=-1.0)
    act(P, FL, FL, AF.Ln, [b_FL], [b_FL], bias=1.0)
    ts(P, "dve", FL, FL, -1.0, None, ALU.mult, None, [b_FL], [b_FL])
    P.op("dve", lambda e: e.tensor_tensor_scan(out=FL, data0=one2.to_broadcast([2, NTOK]), data1=FL, initial=0.0,
                                               op0=ALU.mult, op1=ALU.add), [b_FL, b_one2], [b_FL])
    CH = 2048
    hrow = [[P.alloc([2, CH], BF16) for _ in range(3)] for _ in range(2)]
    hrb = [[P.buf() for _ in range(3)] for _ in range(2)]
    rres = [P.alloc([2, CH], F32) for _ in range(2)]
    rrb = P.bufs(2)
    FKb = P.alloc([128, 2, 64], F32)
    for cidx in range(NTOK // CH):
        i = cidx % 2
        sl = slice(cidx * CH, (cidx + 1) * CH)
        cp(P, "dve", hrow[i][0], FL[:, sl], [b_FL], [hrb[i][0]])
        tt(P, "dve", rres[i], FL[:, sl], hrow[i][0], ALU.subtract, [b_FL, hrb[i][0]], [rrb[i]])
        cp(P, "dve", hrow[i][1], rres[i], [rrb[i]], [hrb[i][1]])
        tt(P, "dve", rres[i], rres[i], hrow[i][1], ALU.subtract, [rrb[i], hrb[i][1]], [rrb[i]])
        cp(P, "dve", hrow[i][2], rres[i], [rrb[i]], [hrb[i][2]])
        for hd in range(2):
            P.dma("sp", QK[0][hd][64:65, sl], hrow[i][0][hd:hd + 1, :], reads=[hrb[i][0]], writes=[qkb[0][hd]])
            for j in range(3):
                P.dma("sp", QK[1][hd][65 + j:66 + j, sl], hrow[i][j][hd:hd + 1, :], reads=[hrb[i][j]],
                      writes=[qkb[1][hd]])
    P.barrier()
    if B_STOP == 2:
        return
    P.top = mark_in
    pT = [P.alloc([128, 512], BF16) for _ in range(4)]
    pTb = P.bufs(4)
    rs = [P.alloc([128, 2], F32) for _ in range(2)]
    rsb = P.bufs(2)
    yd = [P.alloc([128, 128], BF16) for _ in range(2)]
    ydb = P.bufs(2)
    psS = [P.bank(i) for i in range(4)]
    psSb = P.bufs(4)
    psO = [[P.bank(4), P.bank(5)], [P.bank(6), P.bank(7)]]
    psOb = [[P.buf(), P.buf()], [P.buf(), P.buf()]]
    items = []
    for qt in range(0 if not B_SKIP_FOX else 64, 64):
        for hd in range(2):
            for g0 in range(0, qt + 1, 4):
                items.append((qt, hd, list(range(g0, min(g0 + 4, qt + 1)))))
    ctr = {"n": 0}

    def emit_S(item):
        qt, hd, kts = item
        Q, K = QK[0][hd], QK[1][hd]
        qs = slice(qt * 128, (qt + 1) * 128)
        si = ctr["n"] % 4
        ctr["n"] += 1
        for jj, kt in enumerate(kts):
            mm(P, psS[si][:, jj * 128:(jj + 1) * 128], K[:, kt * 128:(kt + 1) * 128], Q[:, qs], True, kt != qt,
               [qkb[1][hd], qkb[0][hd]], [psSb[si]])
            if kt == qt:
                mm(P, psS[si][:, jj * 128:(jj + 1) * 128], C.identb, maskT, False, True,
                   [C.b_identb, b_mask], [psSb[si]])
        w = len(kts) * 128
        act(P, pT[si][:, 0:w], psS[si][:, 0:w], AF.Exp, [psSb[si]], [pTb[si]])
        return si

    def emit_PV(item, si):
        qt, hd, kts = item
        oi = qt % 2
        qs = slice(qt * 128, (qt + 1) * 128)
        ob = psO[oi][hd][:, 0:65]
        for jj, kt in enumerate(kts):
            mm(P, ob, pT[si][:, jj * 128:(jj + 1) * 128], V[:, kt, hd * 65:(hd + 1) * 65], kt == 0, kt == qt,
               [pTb[si], vb[kt]], [psOb[oi][hd]])
        if kts[-1] != qt:
            return
        P.op("dve", lambda e, oi=oi, hd=hd: e.reciprocal(out=rs[oi][:, hd:hd + 1], in_=psO[oi][hd][:, 64:65]),
             [psOb[oi][hd]], [rsb[oi]])
        ts(P, "dve", yd[oi][:, hd * 64:(hd + 1) * 64], psO[oi][hd][:, 0:64], rs[oi][:, hd:hd + 1], None, ALU.mult,
           None, [psOb[oi][hd], rsb[oi]], [ydb[oi]])
        if hd == 1:
            s2 = ctr["n"] % 4
            ctr["n"] += 1
            pv = psS[s2].bitcast(BF16)
            tr(P, pv[:, 0:128], yd[oi], C.identb, [ydb[oi], C.b_identb], [psSb[s2]])
            cp(P, "act", ydT[:, qs], pv[:, 0:128], [psSb[s2]], [b_ydT])

    if items:
        cur = emit_S(items[0])
        for n_ in range(len(items)):
            nxt = emit_S(items[n_ + 1]) if n_ + 1 < len(items) else None
            emit_PV(items[n_], cur)
            cur = nxt
    P.dma("sp", ydT_out, ydT, reads=[b_ydT], writes=[P.buf()])
    P.barrier()
    if B_STOP == 3:
        return
    P.top = mark_fox
    lam = P.alloc([128, 4, 3], F32)
    b_lam = P.buf()
    P.dma("sp", lam, io["lam"], writes=[b_lam])
    bS = P.alloc([128, 4, 2, 128], F32)
    b_bS = P.buf()
    P.dma("sp", bS, io["bS"], writes=[b_bS])
    cS = P.alloc([128, 4, 2, 128], F32)
    b_cS = P.buf()
    P.dma("sp", cS, io["cS"], writes=[b_cS])
    dcol = P.alloc([128, 1], F32)
    b_dcol = P.buf()
    P.dma("sp", dcol, io["dcol"], writes=[b_dcol])
    jrow = P.alloc([128, SB_T], F32)
    b_jrow = P.buf()
    P.dma("sp", jrow, io["jrow"], writes=[b_jrow])
    sp_ = P.alloc([128, 16, 4], F32)
    b_sp = P.buf()
    DT, LRE, R, TH, KRE, KIM, GRE, GIM, T0, T1, T2, T3, DEN, KT = [sp_[:, i, :] for i in range(14)]
    lre_in, lim_in, ldt_in = lam[:, :, 0], lam[:, :, 1], lam[:, :, 2]
    act(P, DT, ldt_in, AF.Exp, [b_lam], [b_sp])
    ts(P, "dve", LRE, lre_in, -1e-4, None, ALU.min, None, [b_lam], [b_sp])
    tt(P, "dve", T0, LRE, DT, ALU.mult, [b_sp], [b_sp])
    act(P, R, T0, AF.Exp, [b_sp], [b_sp])
    tt(P, "dve", TH, lim_in, DT, ALU.mult, [b_lam, b_sp], [b_sp])
    ts(P, "dve", TH, TH, 1.0 / TWO_PI, None, ALU.mult, None, [b_sp], [b_sp])
    if B_STOP == 5:
        P.barrier()
        return
    cosT = P.alloc([128, 4, SB_T], F32)
    sinT = P.alloc([128, 4, SB_T], F32)
    b_tab = P.buf()
    BD = P.alloc([128, 4, 2, 128], BF16)
    b_BD = P.buf()
    CB = P.alloc([128, 4, 2, 128], BF16)
    b_CB = P.buf()
    Y = P.alloc([128, 2, 4], F32)
    b_Y = P.buf()
    mark_tmp5 = P.top
    tmpa = P.alloc([128, SB_T], F32)
    tmpb = P.alloc([128, SB_T], F32)
    tmpi = P.alloc([128, SB_T], F32).bitcast(I32)
    b_ta, b_tb = P.buf(), P.buf()
    for st in range(4):
        ts(P, "dve", tmpa, jrow, TH[:, st:st + 1], None, ALU.mult, None, [b_jrow, b_sp], [b_ta])
        sin_table(P, sinT[:, st, :], b_tab, tmpa, b_ta, tmpb, tmpi, b_tb)
        ts(P, "dve", tmpa, jrow, TH[:, st:st + 1], 0.25, ALU.mult, ALU.add, [b_jrow, b_sp], [b_ta])
        sin_table(P, cosT[:, st, :], b_tab, tmpa, b_ta, tmpb, tmpi, b_tb)
    if B_STOP == 6:
        P.barrier()
        return
    ts(P, "dve", KT, TH, float(SB_T), None, ALU.mult, None, [b_sp], [b_sp])
    sin_table(P, KIM, b_sp, KT, b_sp, T1, T2.bitcast(I32), b_sp)
    ts(P, "dve", KT, TH, float(SB_T), 0.25, ALU.mult, ALU.add, [b_sp], [b_sp])
    sin_table(P, KRE, b_sp, KT, b_sp, T1, T2.bitcast(I32), b_sp)
    tt(P, "dve", KRE, KRE, R, ALU.mult, [b_sp], [b_sp])
    tt(P, "dve", KIM, KIM, R, ALU.mult, [b_sp], [b_sp])
    if B_STOP == 7:
        P.barrier()
        return
    tt(P, "dve", T0, cosT[:, :, 1], R, ALU.mult, [b_tab, b_sp], [b_sp])
    ts(P, "dve", T0, T0, 1.0, None, ALU.subtract, None, [b_sp], [b_sp])
    tt(P, "dve", T1, sinT[:, :, 1], R, ALU.mult, [b_tab, b_sp], [b_sp])
    tt(P, "dve", DEN, LRE, LRE, ALU.mult, [b_sp], [b_sp])
    tt(P, "dve", T2, lim_in, lim_in, ALU.mult, [b_lam], [b_sp])
    tt(P, "dve", DEN, DEN, T2, ALU.add, [b_sp], [b_sp])
    P.op("dve", lambda e: e.reciprocal(out=DEN, in_=DEN), [b_sp], [b_sp])
    tt(P, "dve", T2, T0, LRE, ALU.mult, [b_sp], [b_sp])
    tt(P, "dve", T3, T1, lim_in, ALU.mult, [b_sp, b_lam], [b_sp])
    tt(P, "dve", T2, T2, T3, ALU.add, [b_sp], [b_sp])
    tt(P, "dve", GRE, T2, DEN, ALU.mult, [b_sp], [b_sp])
    tt(P, "dve", T2, T1, LRE, ALU.mult, [b_sp], [b_sp])
    tt(P, "dve", T3, T0, lim_in, ALU.mult, [b_sp, b_lam], [b_sp])
    tt(P, "dve", T2, T2, T3, ALU.subtract, [b_sp], [b_sp])
    tt(P, "dve", GIM, T2, DEN, ALU.mult, [b_sp], [b_sp])
    if B_STOP == 8:
        P.barrier()
        return
    bbf = P.alloc([128, 128], F32)
    b_bbf = P.buf()
    bbb = P.alloc([128, 128], BF16)
    b_bbb = P.buf()
    pst = P.bank(0)
    b_pst = P.buf()
    pstv = pst.bitcast(BF16)
    for st in range(4):
        for ri in range(2):
            a_, b__ = (bS[:, st, 0, :], bS[:, st, 1, :]) if ri == 0 else (bS[:, st, 1, :], bS[:, st, 0, :])
            ts(P, "dve", bbf, b__, GIM[:, st:st + 1], None, ALU.mult, None, [b_bS, b_sp], [b_bbf])
            stt(P, "dve", bbb, a_, GRE[:, st:st + 1], bbf, ALU.mult, ALU.subtract if ri == 0 else ALU.add,
                [b_bS, b_sp, b_bbf], [b_bbb])
            tr(P, pstv[:, 0:128], bbb, C.identb, [b_bbb, C.b_identb], [b_pst])
            cp(P, "act", BD[:, st, ri, :], pstv[:, 0:128], [b_pst], [b_BD])
        cp(P, "dve", CB[:, st, 0, :], cS[:, st, 0, :], [b_cS], [b_CB])
        ts(P, "dve", CB[:, st, 1, :], cS[:, st, 1, :], -1.0, None, ALU.mult, None, [b_cS], [b_CB])
    memset(P, "dve", Y, 0.0, [b_Y])
    P.barrier()
    if B_STOP == 4:
        return
    P.top = mark_tmp5
    hblk = [P.alloc([128, 8, 512], BF16) for _ in range(2)]
    hbb = P.bufs(2)
    uB = [P.alloc([128, SB_T], BF16) for _ in range(2)]
    uBb = P.bufs(2)
    uF = [P.alloc([128, SB_T], F32) for _ in range(2)]
    uFb = P.bufs(2)
    NB = 2
    BuR = [P.alloc([128, SB_T], F32) for _ in range(NB)]
    BuI = [P.alloc([128, SB_T], F32) for _ in range(NB)]
    t1 = [P.alloc([128, SB_T], F32) for _ in range(NB)]
    t2 = [P.alloc([128, SB_T], F32) for _ in range(NB)]
    t3 = [P.alloc([128, SB_T], F32) for _ in range(NB)]
    t4 = [P.alloc([128, SB_T], F32) for _ in range(NB)]
    bub, t1b, t2b, t3b, t4b = P.bufs(NB), P.bufs(NB), P.bufs(NB), P.bufs(NB), P.bufs(NB)
    xr = [P.alloc([128, 4, SB_T], BF16) for _ in range(2)]
    xi = [P.alloc([128, 4, SB_T], BF16) for _ in range(2)]
    xrb = [P.bufs(4) for _ in range(2)]
    yg = [P.alloc([128, SB_T], F32) for _ in range(2)]
    ygb = P.bufs(2)
    yo = [P.alloc([128, SB_T], BF16) for _ in range(2)]
    yob = P.bufs(2)
    sm4 = P.alloc([128, 8], F32)
    b_sm4 = P.buf()
    psu = [P.bank(0), P.bank(1)]
    psub = P.bufs(2)
    psB = [P.bank(2), P.bank(3), P.bank(4), P.bank(5)]
    psBb = P.bufs(4)
    psy = [P.bank(6), P.bank(7)]
    psyb = P.bufs(2)
    nb = 0
    nbu = 0
    b_ygout = P.buf()
    for sbk in range(NTOK // SB_T):
        si = sbk % 2
        for half in range(2):
            blk = sbk * 2 + half
            i = blk % 2
            P.dma("sp", hblk[i], hT_blk(blk), writes=[hbb[i]])
            for k in range(8):
                mm(P, psu[i], W[:, k, 0:128], hblk[i][:, k, :], k == 0, k == 7, [b_W, hbb[i]], [psub[i]])
            cp(P, "dve", uF[si][:, half * 512:(half + 1) * 512], psu[i], [psub[i]], [uFb[si]])
            cp(P, "pool", uB[si][:, half * 512:(half + 1) * 512], uF[si][:, half * 512:(half + 1) * 512],
               [uFb[si]], [uBb[si]])
        if B_STOP == 9:
            P.barrier()
            return
        for st in range(4):
            bi_ = nb % NB
            nb += 1
            for ri, dst in ((0, BuR[bi_]), (1, BuI[bi_])):
                for half in range(2):
                    pi = nbu % 4
                    nbu += 1
                    mm(P, psB[pi], BD[:, st, ri, :], uB[si][:, half * 512:(half + 1) * 512], True, True,
                       [b_BD, uBb[si]], [psBb[pi]])
                    cp(P, "act", dst[:, half * 512:(half + 1) * 512], psB[pi], [psBb[pi]], [bub[bi_]])
            cs, sn = cosT[:, st, :], sinT[:, st, :]
            tt(P, "pool", t1[bi_], cs, BuR[bi_], ALU.mult, [b_tab, bub[bi_]], [t1b[bi_]])
            tt(P, "dve", t2[bi_], sn, BuI[bi_], ALU.mult, [b_tab, bub[bi_]], [t2b[bi_]])
            tt(P, "dve", t1[bi_], t1[bi_], t2[bi_], ALU.add, [t1b[bi_], t2b[bi_]], [t1b[bi_]])
            tt(P, "pool", t3[bi_], cs, BuI[bi_], ALU.mult, [b_tab, bub[bi_]], [t3b[bi_]])
            tt(P, "pool", t4[bi_], sn, BuR[bi_], ALU.mult, [b_tab, bub[bi_]], [t4b[bi_]])
            tt(P, "dve", t3[bi_], t3[bi_], t4[bi_], ALU.subtract, [t3b[bi_], t4b[bi_]], [t3b[bi_]])
            tt(P, "dve", t1[bi_][:, 0:1], t1[bi_][:, 0:1], Y[:, 0, st:st + 1], ALU.add, [t1b[bi_], b_Y], [t1b[bi_]])
            tt(P, "dve", t3[bi_][:, 0:1], t3[bi_][:, 0:1], Y[:, 1, st:st + 1], ALU.add, [t3b[bi_], b_Y], [t3b[bi_]])
            P.op("dve", lambda e, bi_=bi_, st=st: e.tensor_tensor_scan(out=t1[bi_], data0=R[:, st:st + 1].to_broadcast([128, SB_T]), data1=t1[bi_],
                                                                         initial=0.0, op0=ALU.mult, op1=ALU.add),
                 [t1b[bi_], b_sp], [t1b[bi_]])
            P.op("dve", lambda e, bi_=bi_, st=st: e.tensor_tensor_scan(out=t3[bi_], data0=R[:, st:st + 1].to_broadcast([128, SB_T]), data1=t3[bi_],
                                                                         initial=0.0, op0=ALU.mult, op1=ALU.add),
                 [t3b[bi_], b_sp], [t3b[bi_]])
            er, ei = t1[bi_][:, SB_T - 1:SB_T], t3[bi_][:, SB_T - 1:SB_T]
            ts(P, "dve", sm4[:, 0:1], ei, KIM[:, st:st + 1], None, ALU.mult, None, [t3b[bi_], b_sp], [b_sm4])
            stt(P, "dve", Y[:, 0, st:st + 1], er, KRE[:, st:st + 1], sm4[:, 0:1], ALU.mult, ALU.subtract,
                [t1b[bi_], b_sp, b_sm4], [b_Y])
            ts(P, "dve", sm4[:, 1:2], er, KIM[:, st:st + 1], None, ALU.mult, None, [t1b[bi_], b_sp], [b_sm4])
            stt(P, "dve", Y[:, 1, st:st + 1], ei, KRE[:, st:st + 1], sm4[:, 1:2], ALU.mult, ALU.add,
                [t3b[bi_], b_sp, b_sm4], [b_Y])
            tt(P, "pool", t2[bi_], cs, t1[bi_], ALU.mult, [b_tab, t1b[bi_]], [t2b[bi_]])
            tt(P, "dve", t4[bi_], sn, t3[bi_], ALU.mult, [b_tab, t3b[bi_]], [t4b[bi_]])
            tt(P, "dve", xr[si][:, st, :], t2[bi_], t4[bi_], ALU.subtract, [t2b[bi_], t4b[bi_]], [xrb[si][st]])
            tt(P, "pool", t2[bi_], sn, t1[bi_], ALU.mult, [b_tab, t1b[bi_]], [t2b[bi_]])
            tt(P, "dve", t4[bi_], cs, t3[bi_], ALU.mult, [b_tab, t3b[bi_]], [t4b[bi_]])
            tt(P, "dve", xi[si][:, st, :], t2[bi_], t4[bi_], ALU.add, [t2b[bi_], t4b[bi_]], [xrb[si][st]])
            if B_STOP == 10:
                P.barrier()
                return
        for half in range(2):
            hs = slice(half * 512, (half + 1) * 512)
            pi = half
            for st in range(4):
                mm(P, psy[pi], CB[:, st, 0, :], xr[si][:, st, hs], st == 0, False, [b_CB, xrb[si][st]], [psyb[pi]])
                mm(P, psy[pi], CB[:, st, 1, :], xi[si][:, st, hs], False, st == 3, [b_CB, xrb[si][st]], [psyb[pi]])
            stt(P, "dve", yg[si][:, hs], uF[si][:, hs], dcol[:, 0:1], psy[pi], ALU.mult, ALU.add,
                [uFb[si], b_dcol, psyb[pi]], [ygb[si]])
        g_ = t2[0]
        act(P, g_, yg[si], AF.Square, [ygb[si]], [t2b[0]])
        ts(P, "dve", g_, g_, 0.044715, 1.0, ALU.mult, ALU.add, [t2b[0]], [t2b[0]])
        tt(P, "dve", g_, g_, yg[si], ALU.mult, [t2b[0], ygb[si]], [t2b[0]])
        act(P, g_, g_, AF.Sigmoid, [t2b[0]], [t2b[0]], scale=1.5957691216057308)
        tt(P, "pool", yo[si], g_, yg[si], ALU.mult, [t2b[0], ygb[si]], [yob[si]])
        P.dma("sp", ygT_out[:, sbk * SB_T:(sbk + 1) * SB_T], yo[si], reads=[yob[si]], writes=[b_ygout])
        if B_STOP == 11:
            P.barrier()
            return
    P.barrier()
    P.top = base


def build_stage_b():
    nc = bass.Bass("TRN2", target_bir_lowering=False)
    io = {}
    io["ident"] = dram_in(nc, "ident", [128, 128])
    io["ccol"] = dram_in(nc, "ccol", [128, 8])
    io["hT"] = dram_in(nc, "hT", [128, 8, 8192], BF16)
    io["hT_blk"] = lambda blk: io["hT"][:, :, blk * 512:(blk + 1) * 512]
    io["w"] = dram_in(nc, "w", [1024, 514])
    io["fb"] = dram_in(nc, "fb", [2, 1])
    io["maskT"] = dram_in(nc, "maskT", [128, 128], BF16)
    io["lam"] = dram_in(nc, "lam", [128, 4, 3])
    io["bS"] = dram_in(nc, "bS", [128, 4, 2, 128])
    io["cS"] = dram_in(nc, "cS", [128, 4, 2, 128])
    io["dcol"] = dram_in(nc, "dcol", [128, 1])
    io["jrow"] = dram_in(nc, "jrow", [128, SB_T])
    ygT = nc.dram_tensor("ygT", [128, 8192], BF16, kind="ExternalOutput").ap()
    ydT = nc.dram_tensor("ydT", [128, 8192], BF16, kind="ExternalOutput").ap()
    P = Prog(nc)
    C = phase_consts(P, io)
    P.barrier()
    stage_b_body(P, C, io, ygT, ydT)
    P.emit(final_waits=[P.last[k] for k in P.last if k.startswith("dma:")])
    P.close()
    return nc


def prep_stage_b(inp, h1T):
    c = _f32(inp["c"])
    W = np.asarray(inp["odd_w_in"][0], np.float32)
    k_ = np.arange(128)[:, None]
    q_ = np.arange(128)[None, :]
    maskT = np.where(k_ <= q_, 0.0, NEG).astype(ml_dtypes.bfloat16)
    maps = []
    for core in range(8):
        b, j = core // 4, core % 4
        hT = np.concatenate(h1T[b * 4:(b + 1) * 4], axis=2)
        cols = np.concatenate([np.arange(128 * j, 128 * j + 128), 512 + np.arange(128 * j, 128 * j + 128),
                               1024 + np.arange(128 * j, 128 * j + 128), 1536 + np.arange(128 * j, 128 * j + 128),
                               2048 + np.arange(2 * j, 2 * j + 2)])
        gs = np.arange(8 * j, 8 * j + 8)
        lam = np.zeros((128, 4, 3), np.float32)
        bS = np.zeros((128, 4, 2, 128), np.float32)
        cS = np.zeros((128, 4, 2, 128), np.float32)
        for st in range(4):
            for hh in range(2):
                gl = 2 * st + hh
                g = gs[gl]
                ps_ = slice(hh * 64, (hh + 1) * 64)
                lam[ps_, st, 0] = inp["ssm_lam_re"][0, g]
                lam[ps_, st, 1] = inp["ssm_lam_im"][0, g]
                lam[ps_, st, 2] = inp["ssm_log_dt"][0, g]
                bS[ps_, st, 0, 16 * gl:16 * gl + 16] = inp["ssm_b_re"][0, g]
                bS[ps_, st, 1, 16 * gl:16 * gl + 16] = inp["ssm_b_im"][0, g]
                cS[ps_, st, 0, 16 * gl:16 * gl + 16] = np.asarray(inp["ssm_c_re"][0, g]).T
                cS[ps_, st, 1, 16 * gl:16 * gl + 16] = np.asarray(inp["ssm_c_im"][0, g]).T
        maps.append({
            "ident": np.eye(128, dtype=np.float32),
            "ccol": _colT(c[b], 8),
            "hT": np.ascontiguousarray(hT),
            "w": _f32(W[:, cols]),
            "fb": _f32(np.asarray(inp["forget_b"][0, 2 * j:2 * j + 2]).reshape(2, 1)),
            "maskT": maskT,
            "lam": lam, "bS": bS, "cS": cS,
            "dcol": _f32(np.asarray(inp["ssm_d"][0, gs]).reshape(128, 1)),
            "jrow": _f32(np.broadcast_to(np.arange(SB_T, dtype=np.float32)[None], (128, SB_T))),
        })
    return maps


def build_stage_c():
    nc = bass.Bass("TRN2", target_bir_lowering=False)
    io = {}
    io["ustrict"] = dram_in(nc, "ustrict", [128, 128], BF16)
    io["iotaC"] = dram_in(nc, "iotaC", [128, NE])
    io["ident"] = dram_in(nc, "ident", [128, 128])
    io["ccol"] = dram_in(nc, "ccol", [128, 8])
    io["x1"] = dram_in(nc, "x1", [2048, 1024])
    io["ygT"] = dram_in(nc, "ygT", [128, 4, 2048], BF16)
    io["ydT"] = dram_in(nc, "ydT", [128, 4, 2048], BF16)
    io["mod_w"] = dram_in(nc, "mod_w", [2, 1024, 3072])
    io["mod_bT"] = dram_in(nc, "mod_bT", [2, 128, 24])
    io["ln_g"] = dram_in(nc, "ln_g", [2, 1024])
    io["ln_b"] = dram_in(nc, "ln_b", [2, 1024])
    io["glu_w"] = dram_in(nc, "glu_w", [512, 512])
    io["glu_bT"] = dram_in(nc, "glu_bT", [128, 4])
    io["odd_w_out"] = dram_in(nc, "odd_w_out", [1024, 1024])
    io["router_w"] = dram_in(nc, "router_w", [1, 1024, 32])
    io["router_b"] = dram_in(nc, "router_b", [1, 32])
    io["exp_w_gu"] = dram_in(nc, "exp_w_gu", [1, 32, 1024, 2048])
    io["b_guT"] = dram_in(nc, "b_guT", [1, 128, 16, 32])
    io["exp_w_down"] = dram_in(nc, "exp_w_down", [1, 32, 1024, 1024])
    io["exp_b_down"] = dram_in(nc, "exp_b_down", [1, 32, 1024])
    xmid = nc.dram_tensor("xmid", [2048, 1024], F32, kind="ExternalOutput").ap()
    xout = nc.dram_tensor("xout", [2048, 1024], F32, kind="ExternalOutput").ap()
    scr = nc.dram_tensor("scr_gate", [2, 1024], F32, kind="ExternalOutput").ap()
    P = Prog(nc)
    C = phase_consts(P, io)
    P.barrier()
    stage_c_body(P, C, io, scr, xmid, xout, 0, 0)
    P.emit(final_waits=[P.last[k] for k in P.last if k.startswith("dma:")])
    P.close()
    return nc


def stage_c_body(P, C, io, scr, xmid, xout, mi, layer):
    base = P.top
    M = phase_mod(P, C, io["mod_w"][mi], io["mod_bT"][mi], scr[0], io["ln_g"][mi], io["ln_b"][mi])
    yT = P.alloc([128, 8, 2048], BF16)
    b_yT = P.buf()
    mark = P.top
    yg = P.alloc([128, 4, 2048], BF16)
    b_yg = P.buf()
    P.dma("sp", yg, io["ygT"], writes=[b_yg])
    P.dma("sp", yT[:, 4:8, :], io["ydT"], writes=[b_yT])
    gw = P.alloc([128, 4, 512], BF16)
    b_gw = P.buf()
    P.dma("pool", gw, io["glu_w"].rearrange("(k p) n -> p k n", p=128), writes=[b_gw])
    gb = P.alloc([128, 4], F32)
    b_gb = P.buf()
    P.dma("sp", gb, io["glu_bT"], writes=[b_gb])
    sg = [P.alloc([128, 512], F32) for _ in range(2)]
    sgb = P.bufs(2)
    pss = [P.bank(0), P.bank(1)]
    psb = P.bufs(2)
    n = 0
    for ct in range(4):
        for blk in range(4):
            i = n % 2
            n += 1
            tok = slice(blk * 512, (blk + 1) * 512)
            for ci in range(4):
                mm(P, pss[i], gw[:, ci, ct * 128:(ct + 1) * 128], yg[:, ci, tok], ci == 0, ci == 3, [b_gw, b_yg],
                   [psb[i]])
            act(P, sg[i], pss[i], AF.Sigmoid, [psb[i], b_gb], [sgb[i]], bias=gb[:, ct:ct + 1])
            tt(P, "dve", yT[:, ct, tok], yg[:, ct, tok], sg[i], ALU.mult, [b_yg, sgb[i]], [b_yT])
    P.barrier()
    P.top = mark
    b_xmid = P.buf()
    phase_outproj_ln(P, C, M, yT, b_yT, io["odd_w_out"], io["x1"], xmid, b_xmid)
    P.barrier()
    P.top = base
    M2 = phase_mod(P, C, io["mod_w"][mi + 1], io["mod_bT"][mi + 1], scr[1], io["ln_g"][mi + 1], io["ln_b"][mi + 1])
    b_xout = P.buf()
    phase_moe(P, C, M2, xmid, xout, b_xout, io, layer)
    P.barrier()
    P.top = base


def prep_stage_c(inp, x1, ygT, ydT):
    c = _f32(inp["c"])
    common = {
        **_moe_consts(),
        "ident": np.eye(128, dtype=np.float32),
        "mod_w": _f32(inp["mod_w"][1]),
        "mod_bT": _f32(np.stack([_colT(inp["mod_b"][1, j], 24) for j in range(2)])),
        "ln_g": _f32(inp["ln_g"][1]),
        "ln_b": _f32(inp["ln_b"][1]),
        "glu_w": _f32(inp["ssm_glu_w"][0]),
        "glu_bT": _colT(inp["ssm_glu_b"][0], 4),
        "odd_w_out": _f32(inp["odd_w_out"][0]),
        "router_w": _f32(inp["router_w"][1:2]),
        "router_b": _f32(inp["router_b"][1:2]),
        "exp_w_gu": _f32(inp["exp_w_gu"][1:2]),
        "b_guT": _f32(np.asarray(inp["exp_b_gu"][1]).reshape(32, 16, 128).transpose(2, 1, 0)[None]),
        "exp_w_down": _f32(inp["exp_w_down"][1:2]),
        "exp_b_down": _f32(inp["exp_b_down"][1:2]),
    }
    maps = []
    for core in range(8):
        b, seg = core // 4, core % 4
        tok = slice(seg * 2048, (seg + 1) * 2048)
        m = dict(common)
        m["ccol"] = _colT(c[b], 8)
        m["x1"] = _f32(x1[core])
        m["ygT"] = np.ascontiguousarray(np.stack([ygT[b * 4 + j][:, tok] for j in range(4)], axis=1))
        m["ydT"] = np.ascontiguousarray(np.stack([ydT[b * 4 + j][:, tok] for j in range(4)], axis=1))
        maps.append(m)
    return maps


_NC_CACHE = {}


def _get(name, fn):
    if name not in _NC_CACHE:
        _NC_CACHE[name] = fn()
    return _NC_CACHE[name]


def kernel(**inputs):
    if FUSED:
        return kernel_fused(**inputs)
    inp = {k: np.asarray(v) for k, v in inputs.items()}
    cores = list(range(8))
    ra = run_bass_kernel_spmd(_get("a", build_stage_a), prep_stage_a(inp), core_ids=cores).results
    x1 = [r["xout"] for r in ra]
    h1T = [r["h1T"] for r in ra]
    rb = run_bass_kernel_spmd(_get("b", build_stage_b), prep_stage_b(inp, h1T), core_ids=cores).results
    ygT = [r["ygT"] for r in rb]
    ydT = [r["ydT"] for r in rb]
    rc = run_bass_kernel_spmd(_get("c", build_stage_c), prep_stage_c(inp, x1, ygT, ydT), core_ids=cores).results
    out = np.stack([r["xout"] for r in rc]).reshape(2, 8192, 1024).astype(np.float32)
    return out


def build_fused():
    nc = bass.Bass("TRN2", target_bir_lowering=False)
    io = {}
    io["ustrict"] = dram_in(nc, "ustrict", [128, 128], BF16)
    io["iotaC"] = dram_in(nc, "iotaC", [128, NE])
    io["ident"] = dram_in(nc, "ident", [128, 128])
    io["ccol"] = dram_in(nc, "ccol", [128, 8])
    io["xs"] = dram_in(nc, "xs", [4, 2560, 1024])
    io["flags"] = dram_in(nc, "flags", [4, 128, 2])
    io["icnts"] = dram_in(nc, "icnts", [4, 128, 4, 16])
    io["sel"] = dram_in(nc, "sel", [128, 4])
    io["mod_w"] = dram_in(nc, "mod_w", [4, 1024, 3072])
    io["mod_bT"] = dram_in(nc, "mod_bT", [4, 128, 24])
    io["ln_g"] = dram_in(nc, "ln_g", [4, 1024])
    io["ln_b"] = dram_in(nc, "ln_b", [4, 1024])
    io["even_w_in"] = dram_in(nc, "even_w_in", [1024, 2048])
    io["pool_scaleT"] = dram_in(nc, "pool_scaleT", [128, 4])
    io["pool_w"] = dram_in(nc, "pool_w", [4, 128, 128])
    io["biasT"] = dram_in(nc, "biasT", [128, 8, 5, 256], BF16)
    io["even_w_out"] = dram_in(nc, "even_w_out", [1024, 1024])
    io["router_w"] = dram_in(nc, "router_w", [2, 1024, 32])
    io["router_b"] = dram_in(nc, "router_b", [2, 32])
    io["exp_w_gu"] = dram_in(nc, "exp_w_gu", [2, 32, 1024, 2048])
    io["b_guT"] = dram_in(nc, "b_guT", [2, 128, 16, 32])
    io["exp_w_down"] = dram_in(nc, "exp_w_down", [2, 32, 1024, 1024])
    io["exp_b_down"] = dram_in(nc, "exp_b_down", [2, 32, 1024])
    io["wB"] = dram_in(nc, "wB", [4, 1024, 514])
    io["fbB"] = dram_in(nc, "fbB", [4, 2, 1])
    io["maskT"] = dram_in(nc, "maskT", [128, 128], BF16)
    io["lamB"] = dram_in(nc, "lamB", [4, 128, 4, 3])
    io["bSB"] = dram_in(nc, "bSB", [4, 128, 4, 2, 128])
    io["cSB"] = dram_in(nc, "cSB", [4, 128, 4, 2, 128])
    io["dcolB"] = dram_in(nc, "dcolB", [4, 128, 1])
    io["jrow"] = dram_in(nc, "jrow", [128, SB_T])
    io["glu_w"] = dram_in(nc, "glu_w", [512, 512])
    io["glu_bT"] = dram_in(nc, "glu_bT", [128, 4])
    io["odd_w_out"] = dram_in(nc, "odd_w_out", [1024, 1024])
    out = nc.dram_tensor("out", [2048, 1024], F32, kind="ExternalOutput").ap()

    def scratch(name, shape, dt=F32):
        return nc.dram_tensor(name, list(shape), dt, kind="Internal").ap()
    scr = scratch("scr_gate", [3, 1024])
    xmid = scratch("xmid", [2048, 1024])
    x1_all = scratch("x1_all", [4, 2048, 1024])
    h1T_all = scratch("h1T_all", [4, 128, 8, 2048], BF16)
    ygT_all = scratch("ygT_all", [4, 128, 8192], BF16)
    ydT_all = scratch("ydT_all", [4, 128, 8192], BF16)
    x1_own = scratch("x1_own", [2048, 1024])
    ygT_own = scratch("ygT_own", [128, 4, 2048], BF16)
    ydT_own = scratch("ydT_own", [128, 4, 2048], BF16)
    P = Prog(nc)
    P.mod_cache = {}
    C = phase_consts(P, io)
    P.barrier()
    for seg in range(4):
        ioa = dict(io)
        ioa["x"] = io["xs"][seg]
        ioa["flag"] = io["flags"][seg]
        ioa["icnt"] = io["icnts"][seg]
        stage_a_body(P, C, ioa, scr, xmid, x1_all[seg], h1T_all[seg])
        P.barrier()
    for j in range(4):
        iob = dict(io)
        iob["hT_blk"] = lambda blk: h1T_all[blk // 4][:, :, (blk % 4) * 512:(blk % 4 + 1) * 512]
        iob["w"], iob["fb"], iob["lam"] = io["wB"][j], io["fbB"][j], io["lamB"][j]
        iob["bS"], iob["cS"], iob["dcol"] = io["bSB"][j], io["cSB"][j], io["dcolB"][j]
        stage_b_body(P, C, iob, ygT_all[j], ydT_all[j])
        P.barrier()
    base = P.top
    sel = P.alloc([128, 4], F32)
    b_sel = P.buf()
    P.dma("sp", sel, io["sel"], writes=[b_sel])
    tl = [[P.alloc([128, 1024], F32) for _ in range(4)] for _ in range(2)]
    tlb = [P.bufs(4) for _ in range(2)]
    acc = [P.alloc([128, 1024], F32) for _ in range(2)]
    accb = P.bufs(2)
    b_own = P.buf()
    for t in range(16):
        i = t % 2
        for sg_ in range(4):
            P.dma("sp", tl[i][sg_], x1_all[sg_][t * 128:(t + 1) * 128, :], writes=[tlb[i][sg_]])
        ts(P, "dve", acc[i], tl[i][0], sel[:, 0:1], None, ALU.mult, None, [tlb[i][0], b_sel], [accb[i]])
        for sg_ in range(1, 4):
            stt(P, "dve", acc[i], tl[i][sg_], sel[:, sg_:sg_ + 1], acc[i], ALU.mult, ALU.add,
                [tlb[i][sg_], b_sel, accb[i]], [accb[i]])
        P.dma("sp", x1_own[t * 128:(t + 1) * 128, :], acc[i], reads=[accb[i]], writes=[b_own])
    P.barrier()
    P.top = base
    sel = P.alloc([128, 4], F32)
    b_sel = P.buf()
    P.dma("sp", sel, io["sel"], writes=[b_sel])
    tb2 = [[P.alloc([128, 2048], BF16) for _ in range(4)] for _ in range(2)]
    tb2b = [P.bufs(4) for _ in range(2)]
    ac2 = [P.alloc([128, 2048], F32) for _ in range(2)]
    ac2b = P.bufs(2)
    ao2 = [P.alloc([128, 2048], BF16) for _ in range(2)]
    ao2b = P.bufs(2)
    n = 0
    for src_all, dst in ((ygT_all, ygT_own), (ydT_all, ydT_own)):
        for j in range(4):
            i = n % 2
            n += 1
            for sg_ in range(4):
                P.dma("sp", tb2[i][sg_], src_all[j][:, sg_ * 2048:(sg_ + 1) * 2048], writes=[tb2b[i][sg_]])
            ts(P, "dve", ac2[i], tb2[i][0], sel[:, 0:1], None, ALU.mult, None, [tb2b[i][0], b_sel], [ac2b[i]])
            for sg_ in range(1, 4):
                stt(P, "dve", ac2[i], tb2[i][sg_], sel[:, sg_:sg_ + 1], ac2[i], ALU.mult, ALU.add,
                    [tb2b[i][sg_], b_sel, ac2b[i]], [ac2b[i]])
            cp(P, "act", ao2[i], ac2[i], [ac2b[i]], [ao2b[i]])
            P.dma("sp", dst[:, j, :], ao2[i], reads=[ao2b[i]], writes=[b_own])
    P.barrier()
    P.top = base
    ioc = dict(io)
    ioc["x1"], ioc["ygT"], ioc["ydT"] = x1_own, ygT_own, ydT_own
    stage_c_body(P, C, ioc, scr, xmid, out, 2, 1)
    P.barrier()
    P.emit(final_waits=[P.last[k] for k in P.last if k.startswith("dma:")])
    P.close()
    return nc


def prep_fused(inp):
    x = _f32(inp["x"])
    c = _f32(inp["c"])
    pa = prep_stage_a(inp)
    dummy_h = [np.zeros((128, 8, 2048), ml_dtypes.bfloat16)] * 8
    pb = prep_stage_b(inp, dummy_h)
    mods = [(0, 0), (0, 1), (1, 0), (1, 1)]
    common = {
        **_moe_consts(),
        "ident": np.eye(128, dtype=np.float32),
        "mod_w": _f32(np.stack([inp["mod_w"][l, j] for l, j in mods])),
        "mod_bT": _f32(np.stack([_colT(inp["mod_b"][l, j], 24) for l, j in mods])),
        "ln_g": _f32(np.stack([inp["ln_g"][l, j] for l, j in mods])),
        "ln_b": _f32(np.stack([inp["ln_b"][l, j] for l, j in mods])),
        "even_w_in": pa[0]["even_w_in"], "pool_scaleT": pa[0]["pool_scaleT"], "pool_w": pa[0]["pool_w"],
        "biasT": pa[0]["biasT"], "even_w_out": pa[0]["even_w_out"],
        "router_w": _f32(inp["router_w"]), "router_b": _f32(inp["router_b"]),
        "exp_w_gu": _f32(inp["exp_w_gu"]),
        "b_guT": _f32(np.stack([np.asarray(inp["exp_b_gu"][l]).reshape(32, 16, 128).transpose(2, 1, 0)
                                for l in range(2)])),
        "exp_w_down": _f32(inp["exp_w_down"]), "exp_b_down": _f32(inp["exp_b_down"]),
        "wB": _f32(np.stack([pb[j]["w"] for j in range(4)])),
        "fbB": _f32(np.stack([pb[j]["fb"] for j in range(4)])),
        "maskT": pb[0]["maskT"],
        "lamB": _f32(np.stack([pb[j]["lam"] for j in range(4)])),
        "bSB": _f32(np.stack([pb[j]["bS"] for j in range(4)])),
        "cSB": _f32(np.stack([pb[j]["cS"] for j in range(4)])),
        "dcolB": _f32(np.stack([pb[j]["dcol"] for j in range(4)])),
        "jrow": pb[0]["jrow"],
        "glu_w": _f32(inp["ssm_glu_w"][0]), "glu_bT": _colT(inp["ssm_glu_b"][0], 4),
        "odd_w_out": _f32(inp["odd_w_out"][0]),
    }
    maps = []
    for core in range(8):
        b, seg = core // 4, core % 4
        m = dict(common)
        m["ccol"] = _colT(c[b], 8)
        m["xs"] = np.stack([pa[b * 4 + s]["x"] for s in range(4)])
        m["flags"] = np.stack([pa[b * 4 + s]["flag"] for s in range(4)])
        m["icnts"] = np.stack([pa[b * 4 + s]["icnt"] for s in range(4)])
        sel = np.zeros((128, 4), np.float32)
        sel[:, seg] = 1.0
        m["sel"] = sel
        maps.append(m)
    return maps


FUSED = False


def kernel_fused(**inputs):
    inp = {k: np.asarray(v) for k, v in inputs.items()}
    res = run_bass_kernel_spmd(_get("f", build_fused), prep_fused(inp), core_ids=list(range(8))).results
    return np.stack([r["out"] for r in res]).reshape(2, 8192, 1024).astype(np.float32)


CAP = 768
NCH = CAP // 128
U32 = mybir.dt.uint32
SPARSE_MOE = True


def phase_moe_sparse(P, C, M, x_dram, out_dram, b_out, io, layer, xd, yd):
    rw, rb = io["router_w"][layer], io["router_b"][layer]
    wgu, bguT = io["exp_w_gu"][layer], io["b_guT"][layer]
    wdn, bdn = io["exp_w_down"][layer], io["exp_b_down"][layer]
    BOUND = NE * CAP - 1
    mark0 = P.top
    rwt = P.alloc([128, 8, NE], BF16)
    b_rw = P.buf()
    P.dma("pool", rwt, rw.rearrange("(k p) n -> p k n", p=128), writes=[b_rw])
    rbc = P.alloc([128, NE], F32)
    b_rb = P.buf()
    P.dma("sp", rbc, rb.partition_broadcast(128), writes=[b_rb])
    bdt = P.alloc([32, 1024], F32)
    b_bd = P.buf()
    P.dma("sp", bdt, bdn, writes=[b_bd])
    bgt = P.alloc([128, 16, NE], F32)
    b_bg = P.buf()
    P.dma("sp", bgt, bguT, writes=[b_bg])
    ts(P, "dve", bgt[:, 8:16, :], bgt[:, 8:16, :], 1.0, None, ALU.add, None, [b_bg], [b_bg])
    ustr = P.alloc([128, 128], BF16)
    b_us = P.buf()
    P.dma("sp", ustr, io["ustrict"], writes=[b_us])
    ones = P.alloc([128, 128], BF16)
    b_ones = P.buf()
    memset(P, "dve", ones, 1.0, [b_ones])
    iotaC = P.alloc([128, NE], F32)
    b_io = P.buf()
    P.dma("sp", iotaC, io["iotaC"], writes=[b_io])
    G = P.alloc([128, 16, NE], F32)
    b_G = P.buf()
    GT = P.alloc([32, 2048], F32)
    b_GT = P.buf()
    destu = P.alloc([128, 64], F32).bitcast(U32)
    b_dest = P.bufs(16)
    gk = P.alloc([128, 16, 4], F32)
    b_gk = P.bufs(16)
    maccf = P.alloc([128, NE], F32)
    maccb = P.alloc([128, NE], BF16)
    b_macc = P.buf()
    memset(P, "dve", maccf, 0.0, [b_macc])
    memset(P, "dve", maccb, 0.0, [b_macc])
    b_xd = P.buf()
    b_sc = P.bufs(8)
    mark1 = P.top
    xt = [P.alloc([128, 1024], F32) for _ in range(2)]
    xb = P.bufs(2)
    hTt = [P.alloc([128, 8, 128], BF16) for _ in range(2)]
    hTb = P.bufs(2)
    lg = [P.alloc([128, NE], F32) for _ in range(2)]
    lgb = P.bufs(2)
    m8 = [P.alloc([128, 8], F32) for _ in range(2)]
    m8b = P.bufs(2)
    ex = [P.alloc([128, NE], F32) for _ in range(2)]
    exb = P.bufs(2)
    mk = [P.alloc([128, NE], BF16) for _ in range(2)]
    mkb = P.bufs(2)
    Dt = [P.alloc([128, NE], F32) for _ in range(2)]
    Dtb = P.bufs(2)
    ov = [P.alloc([128, NE], F32) for _ in range(2)]
    ovb = P.bufs(2)
    oh = [P.alloc([128, NE], F32) for _ in range(2)]
    ohb = P.bufs(2)
    tm = [P.alloc([128, NE], F32) for _ in range(2)]
    tmb = P.bufs(2)
    dsf = [P.alloc([128, 8], F32) for _ in range(2)]
    dsb = P.bufs(2)
    psx = [P.bank(0, 2), P.bank(2, 2)]
    psxb = P.bufs(2)
    psl = [P.bank(4), P.bank(5)]
    pslb = P.bufs(2)
    psr = [P.bank(6), P.bank(7)]
    psrb = P.bufs(2)
    for t in range(16):
        i = t % 2
        P.dma("sp", xt[i], x_dram[t * 128:(t + 1) * 128, :], writes=[xb[i]])
        for k in range(8):
            tr(P, psx[i][:, k * 128:(k + 1) * 128], xt[i][:, k * 128:(k + 1) * 128], C.ident, [xb[i], C.b_ident],
               [psxb[i]])
        for k in range(8):
            act(P, hTt[i][:, k, :], psx[i][:, k * 128:(k + 1) * 128], AF.Identity, [psxb[i], M.b_mod], [hTb[i]],
                bias=M.modT[:, k:k + 1], scale=M.modT[:, 8 + k:9 + k])
        for k in range(8):
            mm(P, psl[i][:, 0:NE], hTt[i][:, k, :], rwt[:, k, :], k == 0, k == 7, [hTb[i], b_rw], [pslb[i]])
        tt(P, "dve", lg[i], psl[i][:, 0:NE], rbc, ALU.add, [pslb[i], b_rb], [lgb[i]])
        P.op("dve", lambda e, i=i: e.max(out=m8[i], in_=lg[i]), [lgb[i]], [m8b[i]])
        ts(P, "dve", ex[i], lg[i], m8[i][:, 0:1], None, ALU.subtract, None, [lgb[i], m8b[i]], [exb[i]])
        act(P, ex[i], ex[i], AF.Exp, [exb[i]], [exb[i]])
        ts(P, "dve", mk[i], lg[i], m8[i][:, 3:4], None, ALU.is_ge, None, [lgb[i], m8b[i]], [mkb[i]])
        tt(P, "dve", ex[i], ex[i], mk[i], ALU.mult, [exb[i], mkb[i]], [exb[i]])
        P.op("dve", lambda e, i=i: e.reduce_sum(out=m8[i][:, 4:5], in_=ex[i], axis=AX.X), [exb[i]], [m8b[i]])
        P.op("dve", lambda e, i=i: e.reciprocal(out=m8[i][:, 5:6], in_=m8[i][:, 4:5]), [m8b[i]], [m8b[i]])
        ts(P, "dve", G[:, t, :], ex[i], m8[i][:, 5:6], None, ALU.mult, None, [exb[i], m8b[i]], [b_G])
        mm(P, psr[i][:, 0:NE], ustr, mk[i], True, False, [b_us, mkb[i]], [psrb[i]])
        mm(P, psr[i][:, 0:NE], ones, maccb, False, True, [b_ones, b_macc], [psrb[i]])
        ts(P, "dve", ov[i], psr[i][:, 0:NE], float(CAP), None, ALU.is_ge, None, [psrb[i]], [ovb[i]])
        tt(P, "dve", Dt[i], psr[i][:, 0:NE], iotaC, ALU.add, [psrb[i], b_io], [Dtb[i]])
        stt(P, "dve", Dt[i], ov[i], 1.0e9, Dt[i], ALU.mult, ALU.add, [ovb[i], Dtb[i]], [Dtb[i]])
        tt(P, "dve", maccf, maccf, mk[i], ALU.add, [b_macc, mkb[i]], [b_macc])
        cp(P, "dve", maccb, maccf, [b_macc], [b_macc])
        for k in range(4):
            ts(P, "dve", oh[i], lg[i], m8[i][:, k:k + 1], None, ALU.is_equal, None, [lgb[i], m8b[i]], [ohb[i]])
            tt(P, "dve", tm[i], oh[i], Dt[i], ALU.mult, [ohb[i], Dtb[i]], [tmb[i]])
            P.op("dve", lambda e, i=i, k=k: e.reduce_sum(out=dsf[i][:, k:k + 1], in_=tm[i], axis=AX.X), [tmb[i]],
                 [dsb[i]])
            tt(P, "dve", tm[i], oh[i], G[:, t, :], ALU.mult, [ohb[i], b_G], [tmb[i]])
            P.op("dve", lambda e, i=i, k=k: e.reduce_sum(out=dsf[i][:, 4 + k:5 + k], in_=tm[i], axis=AX.X), [tmb[i]],
                 [dsb[i]])
        ts(P, "dve", tm[i][:, 0:4], dsf[i][:, 0:4], float(BOUND), None, ALU.is_le, None, [dsb[i]], [tmb[i]])
        tt(P, "dve", gk[:, t, :], dsf[i][:, 4:8], tm[i][:, 0:4], ALU.mult, [dsb[i], tmb[i]], [b_gk[t]])
        cp(P, "dve", destu[:, t * 4:t * 4 + 4], dsf[i][:, 0:4], [dsb[i]], [b_dest[t]])
        for k in range(4):
            P._add("pool", lambda e, t=t, k=k, src=xt[i]: e.indirect_dma_start(
                out=xd, out_offset=bass.IndirectOffsetOnAxis(ap=destu[:, t * 4 + k:t * 4 + k + 1], axis=0), in_=src,
                in_offset=None, bounds_check=P.const_reg(e, BOUND), oob_is_err=False), [xb[i], b_dest[t]],
                [b_sc[(t * 4 + k) % 8]], True)
        tr(P, psl[i][:32, 128:256], G[:, t, :], C.ident, [b_G, C.b_ident], [pslb[i]])
        cp(P, "act", GT[:, t * 128:(t + 1) * 128], psl[i][:32, 128:256], [pslb[i]], [b_GT])
    P.barrier()
    P.top = mark1
    wguv = wgu.rearrange("e (k p) n -> e p k n", p=128)
    wdnv = wdn.rearrange("e (k p) n -> e p k n", p=128)
    wg = P.alloc([128, 8, 2048], BF16)
    wgb = P.bufs(2)
    wd = [P.alloc([128, 8, 1024], BF16) for _ in range(2)]
    wdb = P.bufs(2)
    xe = P.alloc([128, NCH, 1024], F32)
    xeb = P.buf()
    xeT = [P.alloc([128, 8, CAP], BF16) for _ in range(2)]
    xeTb = P.bufs(2)
    aT = P.alloc([128, 8, CAP], BF16)
    aTb = P.buf()
    gt = [P.alloc([128, 512], F32) for _ in range(2)]
    gtb = P.bufs(2)
    sg = [P.alloc([128, 512], F32) for _ in range(2)]
    sgb = P.bufs(2)
    up = [P.alloc([128, 512], F32) for _ in range(2)]
    upb = P.bufs(2)
    yo = [P.alloc([128, 1024], F32) for _ in range(2)]
    yob = P.bufs(2)
    b_yd = P.buf()
    pst = [P.bank(0, 2), P.bank(2, 2)]
    pstb = P.bufs(2)
    psg = [P.bank(4), P.bank(5)]
    psgb = P.bufs(2)
    psu = [P.bank(6), P.bank(7)]
    psub = P.bufs(2)
    blks = [(0, 512), (512, CAP)]
    cnt = {"it": 0, "nt": 0}

    def load_wg(e):
        for c in range(2):
            P.dma("pool", wg[:, :, c * 512:(c + 1) * 512], wguv[e][:, :, c * 512:(c + 1) * 512], writes=[wgb[c]])
            P.dma("pool", wg[:, :, 1024 + c * 512:1024 + (c + 1) * 512],
                  wguv[e][:, :, 1024 + c * 512:1024 + (c + 1) * 512], writes=[wgb[c]])

    def load_wd(e):
        for h2 in range(2):
            P.dma("pool", wd[e % 2][:, :, h2 * 512:(h2 + 1) * 512], wdnv[e][:, :, h2 * 512:(h2 + 1) * 512],
                  writes=[wdb[e % 2]])

    def load_xe(e):
        P.dma("sp", xe, xd[e * CAP:(e + 1) * CAP, :].rearrange("(c p) f -> p c f", p=128), reads=[b_xd],
              writes=[xeb])

    def transposes(e):
        for k in range(8):
            i = cnt["nt"] % 2
            cnt["nt"] += 1
            for ch in range(NCH):
                tr(P, pst[i][:, ch * 128:(ch + 1) * 128], xe[:, ch, k * 128:(k + 1) * 128], C.ident,
                   [xeb, C.b_ident], [pstb[i]])
            act(P, xeT[e % 2][:, k, :], pst[i][:, 0:CAP], AF.Identity, [pstb[i], M.b_mod], [xeTb[e % 2]],
                bias=M.modT[:, k:k + 1], scale=M.modT[:, 8 + k:9 + k])

    def gu(e):
        xT, xTb = xeT[e % 2], xeTb[e % 2]
        for ct in range(8):
            c = ct // 4
            for (b0, b1) in blks:
                i = cnt["it"] % 2
                cnt["it"] += 1
                w_ = b1 - b0
                for k in range(8):
                    mm(P, psg[i][:, 0:w_], wg[:, k, ct * 128:(ct + 1) * 128], xT[:, k, b0:b1], k == 0, k == 7,
                       [wgb[c], xTb], [psgb[i]])
                for k in range(8):
                    mm(P, psu[i][:, 0:w_], wg[:, k, 1024 + ct * 128:1024 + (ct + 1) * 128], xT[:, k, b0:b1], k == 0,
                       k == 7, [wgb[c], xTb], [psub[i]])
                ts(P, "dve", gt[i][:, 0:w_], psg[i][:, 0:w_], bgt[:, ct, e:e + 1], 7.0, ALU.add, ALU.min,
                   [psgb[i], b_bg], [gtb[i]])
                act(P, sg[i][:, 0:w_], gt[i][:, 0:w_], AF.Sigmoid, [gtb[i]], [sgb[i]], scale=1.702)
                ts(P, "dve", up[i][:, 0:w_], psu[i][:, 0:w_], bgt[:, 8 + ct, e:e + 1], -6.0, ALU.add, ALU.max,
                   [psub[i], b_bg], [upb[i]])
                tt(P, "dve", sg[i][:, 0:w_], sg[i][:, 0:w_], gt[i][:, 0:w_], ALU.mult, [sgb[i], gtb[i]], [sgb[i]])
                stt(P, "dve", aT[:, ct, b0:b1], up[i][:, 0:w_], 8.0, sg[i][:, 0:w_], ALU.min, ALU.mult,
                    [sgb[i], upb[i]], [aTb])

    def down(e):
        for ch in range(NCH):
            i = cnt["nt"] % 2
            cnt["nt"] += 1
            for cb in range(2):
                for k in range(8):
                    mm(P, pst[i][:, cb * 512:(cb + 1) * 512], aT[:, k, ch * 128:(ch + 1) * 128],
                       wd[e % 2][:, k, cb * 512:(cb + 1) * 512], k == 0, k == 7, [aTb, wdb[e % 2]], [pstb[i]])
            cp(P, "act", yo[i], pst[i], [pstb[i]], [yob[i]])
            P.dma("sp", yd[e * CAP + ch * 128:e * CAP + (ch + 1) * 128, :], yo[i], reads=[yob[i]], writes=[b_yd])

    load_wg(0)
    load_wd(0)
    load_xe(0)
    transposes(0)
    for e in range(NE):
        if e + 1 < NE:
            load_xe(e + 1)
        gu(e)
        if e + 1 < NE:
            load_wg(e + 1)
            load_wd(e + 1)
            transposes(e + 1)
        down(e)
    P.barrier()
    P.top = mark1
    yk = [[P.alloc([128, 1024], F32) for _ in range(4)] for _ in range(2)]
    ykb = [P.bufs(4) for _ in range(2)]
    for i in range(2):
        for k in range(4):
            memset(P, "pool", yk[i][k], 0.0, [ykb[i][k]])
    xt = [P.alloc([128, 1024], F32) for _ in range(2)]
    xb = P.bufs(2)
    acc = [P.alloc([128, 1024], F32) for _ in range(2)]
    accb = P.bufs(2)
    ot = [P.alloc([128, 1024], F32) for _ in range(2)]
    ob = P.bufs(2)
    sm = [P.alloc([128, 16], F32) for _ in range(2)]
    sb = P.bufs(2)
    psd = [P.bank(4, 2), P.bank(6, 2)]
    psdb = P.bufs(2)
    for t in range(16):
        i = t % 2
        P.dma("sp", xt[i], x_dram[t * 128:(t + 1) * 128, :], writes=[xb[i]])
        for k in range(4):
            P._add("pool", lambda e, t=t, k=k, dst=yk[i][k]: e.indirect_dma_start(
                out=dst, out_offset=None, in_=yd,
                in_offset=bass.IndirectOffsetOnAxis(ap=destu[:, t * 4 + k:t * 4 + k + 1], axis=0),
                bounds_check=P.const_reg(e, BOUND), oob_is_err=False), [b_yd, b_dest[t]], [ykb[i][k]], True)
        for cb in range(2):
            mm(P, psd[i][:, cb * 512:(cb + 1) * 512], GT[:, t * 128:(t + 1) * 128], bdt[:, cb * 512:(cb + 1) * 512],
               True, True, [b_GT, b_bd], [psdb[i]])
        stt(P, "dve", acc[i], yk[i][0], gk[:, t, 0:1], psd[i], ALU.mult, ALU.add, [ykb[i][0], b_gk[t], psdb[i]],
            [accb[i]])
        for k in range(1, 4):
            stt(P, "dve", acc[i], yk[i][k], gk[:, t, k:k + 1], acc[i], ALU.mult, ALU.add,
                [ykb[i][k], b_gk[t], accb[i]], [accb[i]])
        tt(P, "dve", acc[i], acc[i], M.gate_bc, ALU.mult, [accb[i], M.b_bc_buf], [accb[i]])
        stt(P, "dve", acc[i], xt[i], ALPHA, acc[i], ALU.mult, ALU.add, [xb[i], accb[i]], [accb[i]])
        ln_tile(P, M, acc[i], accb[i], ot[i], ob[i], sm[i], sb[i])
        P.dma("sp", out_dram[t * 128:(t + 1) * 128, :], ot[i], reads=[ob[i]], writes=[b_out])
    P.barrier()
    P.top = mark0
```
